# Optimizing a Trainium2 kernel written in Bass

```python
import math
import jax
import jax.numpy as jnp
from jax import lax
import numpy as np

D_MODEL = 1024
BATCH = 8
SEQ = 2048
DEPTH = 2

CHUNK = 64
N_BRANCH = 4
BRANCH_DIM = D_MODEL // 2
NORM_EPS = 1e-6

RWKV_HEAD_DIM = 64
RWKV_HEADS = BRANCH_DIM // RWKV_HEAD_DIM
RWKV_DIM = RWKV_HEADS * RWKV_HEAD_DIM
RWKV_DECAY_RANK = 64
RWKV_A_RANK = 64
RWKV_V_RANK = 32
RWKV_GATE_RANK = 160
RWKV_LN_EPS = 64e-5

SSM_HEAD_DIM = 64
SSM_HEADS = BRANCH_DIM // SSM_HEAD_DIM
SSM_DIM = SSM_HEADS * SSM_HEAD_DIM
SSM_GROUPS = 2
SSM_STATE = 128
SSM_CONV = 4
SSM_XBC = SSM_DIM + 2 * SSM_GROUPS * SSM_STATE

ATT_HEAD_DIM = 64
ATT_HEADS = BRANCH_DIM // ATT_HEAD_DIM
ATT_DIM = ATT_HEADS * ATT_HEAD_DIM
ATT_LEFT_CHUNKS = 8
ATT_BAND = (ATT_LEFT_CHUNKS + 1) * CHUNK
REL_CLIP = 2 * CHUNK

GLA_HEADS = 4
GLA_KEY_DIM = BRANCH_DIM // 2
GLA_VAL_DIM = BRANCH_DIM
GLA_GATE_RANK = 16
GLA_GATE_NORM = 16.0

RWKV_COLS = 3 * RWKV_DIM + RWKV_DECAY_RANK + RWKV_A_RANK + RWKV_GATE_RANK
SSM_COLS = SSM_DIM + SSM_XBC + SSM_HEADS
ATT_COLS = 3 * ATT_DIM
GLA_COLS = 2 * GLA_KEY_DIM + 2 * GLA_VAL_DIM + GLA_GATE_RANK
GATE_COLS = N_BRANCH * D_MODEL
IN_COLS = RWKV_COLS + SSM_COLS + ATT_COLS + GLA_COLS + GATE_COLS

FFN_DIM = 2816
N_EXPERTS = 8
TOP_K = 2
EXPERT_DIM = 3584
MOE_ROWS = 256
N_DENSE = (DEPTH + 1) // 2
N_MOE = DEPTH // 2
N_VRES = DEPTH - 1

kernel_name = 'hybrid_streaming_block'


def _split(z, sizes):
    return jnp.split(z, np.cumsum(sizes)[:-1].tolist(), axis=-1)


def _rms(x, g, eps=NORM_EPS):
    xf = x.astype(jnp.float32)
    y = xf * lax.rsqrt(jnp.mean(xf * xf, axis=-1, keepdims=True) + eps)
    return (y * g.astype(jnp.float32)).astype(x.dtype)


def _shift(z):
    return jnp.pad(z, ((0, 0), (1, 0), (0, 0)))[:, :-1]


def _chunks(t, *tail):
    return t.reshape(t.shape[0], t.shape[1] // CHUNK, CHUNK, *tail)


def _chunk_scan(contrib, decay):
    def step(state, inp):
        inc, dec = inp
        return state * dec + inc, state
    init = jnp.zeros_like(contrib[:, 0])
    _, entering = lax.scan(step, init, (jnp.moveaxis(contrib, 1, 0), jnp.moveaxis(decay, 1, 0)))
    return jnp.moveaxis(entering, 0, 1)


def _causal_dwconv(x, w, b):
    width = w.shape[0]
    y = lax.conv_general_dilated(x, w[:, None, :].astype(x.dtype), window_strides=(1,),
                                 padding=[(width - 1, 0)], dimension_numbers=('NWC', 'WIO', 'NWC'),
                                 feature_group_count=x.shape[-1])
    return y + b


def _rwkv7_scan(r, w, k, v, a, b):
    bsz, _, heads, n = r.shape
    seq_in = tuple(jnp.moveaxis(t.astype(jnp.float32), 1, 0) for t in (r, w, k, v, a, b))

    def step(state, inp):
        rt, wt, kt, vt, at, bt = inp
        sa = jnp.einsum('bhij,bhj->bhi', state, at)
        state = (state * wt[:, :, None, :] + vt[..., None] * kt[:, :, None, :]
                 + sa[..., None] * bt[:, :, None, :])
        return state, jnp.einsum('bhij,bhj->bhi', state, rt)

    s0 = jnp.zeros((bsz, heads, n, n), jnp.float32)
    _, y = lax.scan(step, s0, seq_in)
    return jnp.moveaxis(y, 0, 1)


def _rwkv7(za, mu, decay_up, w0, a_up, a0, gate_up, k_k, k_a, r_k, ln_w, ln_b, v_first, v_mix):
    bsz, seq, _ = za.shape
    f32 = jnp.float32
    za = za + (_shift(za) - za) * mu
    r, k, v, xw, xa, xg = _split(za, [RWKV_DIM, RWKV_DIM, RWKV_DIM, RWKV_DECAY_RANK, RWKV_A_RANK, RWKV_GATE_RANK])
    w_log = -jax.nn.softplus(-(w0 + jnp.tanh(xw) @ decay_up).astype(f32)) - 0.5
    decay = jnp.exp(-jnp.exp(w_log))
    a = jax.nn.sigmoid(a0 + xa @ a_up)
    g = jax.nn.sigmoid(xg) @ gate_up
    v_raw = v
    if v_mix is not None:
        v = v + (v_first - v) * v_mix
    kk = k * k_k
    k = k * (1.0 + (a - 1.0) * k_a)

    def heads(t):
        return t.astype(f32).reshape(bsz, seq, RWKV_HEADS, RWKV_HEAD_DIM)

    r, k, v, a, kk, decay = (heads(t) for t in (r, k, v, a, kk, decay))
    kk = kk / jnp.maximum(jnp.sqrt(jnp.sum(kk * kk, axis=-1, keepdims=True)), 1e-12)
    y = _rwkv7_scan(r, decay, k, v, -kk, kk * a)
    mean = jnp.mean(y, axis=-1, keepdims=True)
    var = jnp.mean(jnp.square(y - mean), axis=-1, keepdims=True)
    y = (y - mean) * lax.rsqrt(var + RWKV_LN_EPS)
    y = (y * ln_w.astype(f32).reshape(RWKV_HEADS, RWKV_HEAD_DIM)
         + ln_b.astype(f32).reshape(RWKV_HEADS, RWKV_HEAD_DIM))
    y = y + jnp.sum(r * k * r_k.astype(f32), axis=-1, keepdims=True) * v
    out = y.reshape(bsz, seq, RWKV_DIM) * g.astype(f32)
    return out.astype(za.dtype), v_raw


def _mamba2(zb, conv_w, conv_b, dt_bias, a_log, d_skip, norm_w):
    bsz, seq, _ = zb.shape
    f32 = jnp.float32
    e = SSM_HEADS // SSM_GROUPS
    gate, xbc, dt = _split(zb, [SSM_DIM, SSM_XBC, SSM_HEADS])
    xbc = jax.nn.silu(_causal_dwconv(xbc, conv_w, conv_b))
    xs, bm, cm = _split(xbc, [SSM_DIM, SSM_GROUPS * SSM_STATE, SSM_GROUPS * SSM_STATE])
    xs = _chunks(xs.astype(f32), SSM_GROUPS, e, SSM_HEAD_DIM)
    bm = _chunks(bm.astype(f32), SSM_GROUPS, SSM_STATE)
    cm = _chunks(cm.astype(f32), SSM_GROUPS, SSM_STATE)
    dt = _chunks(jax.nn.softplus(dt.astype(f32) + dt_bias.astype(f32)), SSM_GROUPS, e)
    a = -jnp.exp(a_log.astype(f32)).reshape(SSM_GROUPS, e)
    acs = jnp.cumsum(dt * a, axis=2)
    xdt = xs * dt[..., None]
    causal = jnp.tril(jnp.ones((CHUNK, CHUNK), bool))
    seg = acs[:, :, :, None] - acs[:, :, None, :]
    lmat = jnp.exp(jnp.where(causal[:, :, None, None], seg, -jnp.inf))
    cb = jnp.einsum('bclgn,bcsgn->bclsg', cm, bm)
    y = jnp.einsum('bclsg,bclsge,bcsgep->bclgep', cb, lmat, xdt)
    decay_to_end = jnp.exp(acs[:, :, -1:] - acs)
    states = jnp.einsum('bclgn,bclge,bclgep->bcgepn', bm, decay_to_end, xdt)
    entering = _chunk_scan(states, jnp.exp(acs[:, :, -1])[..., None, None])
    y = y + jnp.einsum('bclgn,bcgepn,bclge->bclgep', cm, entering, jnp.exp(acs))
    y = y + xs * d_skip.astype(f32).reshape(SSM_GROUPS, e, 1)
    y = y.reshape(bsz, seq, SSM_DIM) * jax.nn.silu(gate.astype(f32))
    y = _rms(y.reshape(bsz, seq, SSM_GROUPS, SSM_DIM // SSM_GROUPS), norm_w.reshape(SSM_GROUPS, -1))
    return y.reshape(bsz, seq, SSM_DIM).astype(zb.dtype)


def _band_attention(zc, q_gain, k_gain, rel_bias):
    bsz, seq, _ = zc.shape
    nc = seq // CHUNK
    q, k, v = _split(zc, [ATT_DIM, ATT_DIM, ATT_DIM])
    q = _chunks(_rms(q.reshape(bsz, seq, ATT_HEADS, ATT_HEAD_DIM), q_gain), ATT_HEADS, ATT_HEAD_DIM)
    k = _chunks(_rms(k.reshape(bsz, seq, ATT_HEADS, ATT_HEAD_DIM), k_gain), ATT_HEADS, ATT_HEAD_DIM)
    v = _chunks(v, ATT_HEADS, ATT_HEAD_DIM)
    pad = ((0, 0), (ATT_LEFT_CHUNKS, 0), (0, 0), (0, 0), (0, 0))
    kp = jnp.pad(k, pad)
    vp = jnp.pad(v, pad)
    k_band = jnp.concatenate([kp[:, i:i + nc] for i in range(ATT_LEFT_CHUNKS + 1)], axis=2)
    v_band = jnp.concatenate([vp[:, i:i + nc] for i in range(ATT_LEFT_CHUNKS + 1)], axis=2)
    scores = jnp.einsum('bclhd,bckhd->bhclk', q, k_band,
                        preferred_element_type=jnp.float32) * (ATT_HEAD_DIM ** -0.5)
    q_pos = ATT_LEFT_CHUNKS * CHUNK + jnp.arange(CHUNK)
    k_pos = jnp.arange(ATT_BAND)
    rel = jnp.clip(k_pos[None, :] - q_pos[:, None], -REL_CLIP, REL_CLIP) + REL_CLIP
    bias = rel_bias.astype(jnp.float32)[:, rel]
    key_chunk = jnp.arange(nc)[:, None] - ATT_LEFT_CHUNKS + k_pos[None, :] // CHUNK
    scores = jnp.where((key_chunk >= 0)[None, None, :, None, :], scores + bias[:, None], -jnp.inf)
    probs = jax.nn.softmax(scores, axis=-1)
    o = jnp.einsum('bhclk,bckhd->bclhd', probs.astype(v.dtype), v_band)
    return o.reshape(bsz, seq, ATT_DIM)


def _gla(zd, gate_up, gate_bias, norm_w):
    bsz, seq, _ = zd.shape
    f32 = jnp.float32
    dk = GLA_KEY_DIM // GLA_HEADS
    dv = GLA_VAL_DIM // GLA_HEADS
    q, k, v, xgk, g = _split(zd, [GLA_KEY_DIM, GLA_KEY_DIM, GLA_VAL_DIM, GLA_GATE_RANK, GLA_VAL_DIM])
    log_a = jax.nn.log_sigmoid((xgk @ gate_up + gate_bias).astype(f32)) / GLA_GATE_NORM
    q = _chunks(q.astype(f32) * (dk ** -0.5), GLA_HEADS, dk)
    k = _chunks(k.astype(f32), GLA_HEADS, dk)
    v = _chunks(v.astype(f32), GLA_HEADS, dv)
    bcum = jnp.cumsum(_chunks(log_a, GLA_HEADS, dk), axis=2)
    blast = bcum[:, :, -1:]
    qg = q * jnp.exp(bcum)
    kg = k * jnp.exp(-bcum)
    kd = k * jnp.exp(blast - bcum)
    causal = jnp.tril(jnp.ones((CHUNK, CHUNK), bool))
    att = jnp.where(causal, jnp.einsum('bclhk,bcshk->bchls', qg, kg), 0.0)
    o = jnp.einsum('bchls,bcshv->bclhv', att, v)
    contrib = jnp.einsum('bclhk,bclhv->bchkv', kd, v)
    entering = _chunk_scan(contrib, jnp.exp(blast[:, :, 0])[..., None])
    o = o + jnp.einsum('bclhk,bchkv->bclhv', qg, entering)
    o = _rms(o.reshape(bsz, seq, GLA_HEADS, dv), norm_w)
    o = o.reshape(bsz, seq, GLA_VAL_DIM) * jax.nn.silu(g.astype(f32))
    return o.astype(zd.dtype)


def _swiglu(x, w1, w3, w2):
    return (jax.nn.silu(x @ w1) * (x @ w3)) @ w2


def _moe(x, w_router, w1, w3, w2):
    bsz, seq, d = x.shape
    n_tok = bsz * seq
    n_pair = n_tok * TOP_K
    xf = x.reshape(n_tok, d)
    logits = (xf @ w_router).astype(jnp.float32)
    top_val, top_idx = lax.top_k(logits, TOP_K)
    gate = jax.nn.softmax(top_val, axis=-1)
    e_pair = top_idx.reshape(-1)
    t_pair = jnp.arange(n_pair) // TOP_K
    g_pair = gate.reshape(-1)
    order = jnp.argsort(e_pair)
    e_sorted = e_pair[order]
    counts = jnp.bincount(e_pair, length=N_EXPERTS)
    padded = (counts + MOE_ROWS - 1) // MOE_ROWS * MOE_ROWS
    start_sorted = jnp.cumsum(counts) - counts
    end_padded = jnp.cumsum(padded)
    start_padded = end_padded - padded
    dest = start_padded[e_sorted] + jnp.arange(n_pair) - start_sorted[e_sorted]
    n_groups = (n_pair + MOE_ROWS - 1) // MOE_ROWS + N_EXPERTS
    n_rows = n_groups * MOE_ROWS
    row_token = jnp.full((n_rows,), n_tok, jnp.int32).at[dest].set(t_pair[order].astype(jnp.int32))
    row_gate = jnp.zeros((n_rows,), jnp.float32).at[dest].set(g_pair[order])
    group_expert = jnp.minimum(jnp.searchsorted(end_padded, jnp.arange(n_groups) * MOE_ROWS, side='right'),
                               N_EXPERTS - 1)
    x_pad = jnp.concatenate([xf, jnp.zeros((1, d), xf.dtype)], axis=0)
    xg = x_pad[row_token].reshape(n_groups, MOE_ROWS, d)

    def expert_rows(args):
        rows, e = args
        return _swiglu(rows, w1[e], w3[e], w2[e])

    yg = lax.map(expert_rows, (xg, group_expert)).reshape(n_rows, d)
    out = jnp.zeros((n_tok + 1, d), jnp.float32).at[row_token].add(yg.astype(jnp.float32) * row_gate[:, None])
    return out[:n_tok].reshape(bsz, seq, d).astype(x.dtype)


def setup_inputs(seed: int = 0) -> dict:
    key = jax.random.key(seed)
    ks = iter(jax.random.split(key, 64))
    f32 = jnp.float32

    def nrm(shape, scale):
        return jax.random.normal(next(ks), shape, f32) * scale

    def uni(shape, lo, hi):
        return jax.random.uniform(next(ks), shape, f32, lo, hi)

    L = DEPTH
    dt0 = jnp.exp(uni((L, SSM_HEADS), math.log(1e-3), math.log(1e-1)))
    return {
        'x': nrm((BATCH, SEQ, D_MODEL), 1.0),
        'w_in': nrm((L, D_MODEL, IN_COLS), D_MODEL ** -0.5),
        'norm_mix': 1.0 + nrm((L, D_MODEL), 0.02),
        'rwkv_mu': uni((L, RWKV_COLS), 0.0, 1.0),
        'rwkv_decay_up': nrm((L, RWKV_DECAY_RANK, RWKV_DIM), RWKV_DECAY_RANK ** -0.5),
        'rwkv_w0': uni((L, RWKV_DIM), -6.0, 0.0),
        'rwkv_a_up': nrm((L, RWKV_A_RANK, RWKV_DIM), RWKV_A_RANK ** -0.5),
        'rwkv_a0': nrm((L, RWKV_DIM), 0.1),
        'rwkv_gate_up': nrm((L, RWKV_GATE_RANK, RWKV_DIM), RWKV_GATE_RANK ** -0.5),
        'rwkv_k_k': 0.85 + nrm((L, RWKV_DIM), 0.02),
        'rwkv_k_a': 1.0 + nrm((L, RWKV_DIM), 0.02),
        'rwkv_r_k': nrm((L, RWKV_HEADS, RWKV_HEAD_DIM), 0.1),
        'rwkv_ln_w': 1.0 + nrm((L, RWKV_DIM), 0.02),
        'rwkv_ln_b': nrm((L, RWKV_DIM), 0.02),
        'vres_down': nrm((N_VRES, D_MODEL, RWKV_V_RANK), D_MODEL ** -0.5),
        'vres_up': nrm((N_VRES, RWKV_V_RANK, RWKV_DIM), RWKV_V_RANK ** -0.5),
        'vres_v0': 1.0 + nrm((N_VRES, RWKV_DIM), 0.1),
        'ssm_conv_w': nrm((L, SSM_CONV, SSM_XBC), SSM_CONV ** -0.5),
        'ssm_conv_b': nrm((L, SSM_XBC), 0.02),
        'ssm_dt_bias': dt0 + jnp.log(-jnp.expm1(-dt0)),
        'ssm_a_log': jnp.log(uni((L, SSM_HEADS), 1.0, 16.0)),
        'ssm_d': 1.0 + nrm((L, SSM_HEADS), 0.1),
        'ssm_norm_w': 1.0 + nrm((L, SSM_DIM), 0.02),
        'att_q_gain': 1.0 + nrm((L, ATT_HEAD_DIM), 0.02),
        'att_k_gain': 1.0 + nrm((L, ATT_HEAD_DIM), 0.02),
        'att_rel_bias': nrm((ATT_HEADS, 2 * REL_CLIP + 1), 0.2),
        'gla_gate_up': nrm((L, GLA_GATE_RANK, GLA_KEY_DIM), GLA_GATE_RANK ** -0.5),
        'gla_gate_bias': nrm((L, GLA_KEY_DIM), 0.1),
        'gla_norm_w': 1.0 + nrm((L, GLA_VAL_DIM // GLA_HEADS), 0.02),
        'w_branch': nrm((L, N_BRANCH, BRANCH_DIM, D_MODEL), BRANCH_DIM ** -0.5),
        'w_out': nrm((L, D_MODEL, D_MODEL), D_MODEL ** -0.5),
        'norm_ffn': 1.0 + nrm((L, D_MODEL), 0.02),
        'ffn_w1': nrm((N_DENSE, D_MODEL, FFN_DIM), D_MODEL ** -0.5),
        'ffn_w3': nrm((N_DENSE, D_MODEL, FFN_DIM), D_MODEL ** -0.5),
        'ffn_w2': nrm((N_DENSE, FFN_DIM, D_MODEL), FFN_DIM ** -0.5),
        'moe_router': nrm((N_MOE, D_MODEL, N_EXPERTS), D_MODEL ** -0.5),
        'moe_w1': nrm((N_MOE, N_EXPERTS, D_MODEL, EXPERT_DIM), D_MODEL ** -0.5),
        'moe_w3': nrm((N_MOE, N_EXPERTS, D_MODEL, EXPERT_DIM), D_MODEL ** -0.5),
        'moe_w2': nrm((N_MOE, N_EXPERTS, EXPERT_DIM, D_MODEL), EXPERT_DIM ** -0.5),
    }


def reference(x, w_in, norm_mix, rwkv_mu, rwkv_decay_up, rwkv_w0, rwkv_a_up, rwkv_a0, rwkv_gate_up,
              rwkv_k_k, rwkv_k_a, rwkv_r_k, rwkv_ln_w, rwkv_ln_b, vres_down, vres_up, vres_v0,
              ssm_conv_w, ssm_conv_b, ssm_dt_bias, ssm_a_log, ssm_d, ssm_norm_w,
              att_q_gain, att_k_gain, att_rel_bias, gla_gate_up, gla_gate_bias, gla_norm_w,
              w_branch, w_out, norm_ffn, ffn_w1, ffn_w3, ffn_w2, moe_router, moe_w1, moe_w3, moe_w2):
    bsz, seq, _ = x.shape
    v_first = None
    for l in range(DEPTH):
        h = _rms(x, norm_mix[l])
        z = h @ w_in[l]
        za, zb, zc, zd, zg = _split(z, [RWKV_COLS, SSM_COLS, ATT_COLS, GLA_COLS, GATE_COLS])
        v_mix = None
        if l > 0:
            v_mix = jax.nn.sigmoid(vres_v0[l - 1] + (h @ vres_down[l - 1]) @ vres_up[l - 1])
        o_a, v_raw = _rwkv7(za, rwkv_mu[l], rwkv_decay_up[l], rwkv_w0[l], rwkv_a_up[l], rwkv_a0[l],
                            rwkv_gate_up[l], rwkv_k_k[l], rwkv_k_a[l], rwkv_r_k[l], rwkv_ln_w[l],
                            rwkv_ln_b[l], v_first, v_mix)
        if l == 0:
            v_first = v_raw
        o_b = _mamba2(zb, ssm_conv_w[l], ssm_conv_b[l], ssm_dt_bias[l], ssm_a_log[l], ssm_d[l], ssm_norm_w[l])
        o_c = _band_attention(zc, att_q_gain[l], att_k_gain[l], att_rel_bias)
        o_d = _gla(zd, gla_gate_up[l], gla_gate_bias[l], gla_norm_w[l])
        branches = jnp.stack([o_a, o_b, o_c, o_d], axis=2).astype(x.dtype)
        u = jnp.einsum('bsnc,ncd->bsnd', branches, w_branch[l])
        gates = jax.nn.sigmoid(zg.reshape(bsz, seq, N_BRANCH, D_MODEL))
        x = x + jnp.sum(gates * u, axis=2) @ w_out[l]
        hf = _rms(x, norm_ffn[l])
        if l % 2 == 0:
            x = x + _swiglu(hf, ffn_w1[l // 2], ffn_w3[l // 2], ffn_w2[l // 2])
        else:
            x = x + _moe(hf, moe_router[l // 2], moe_w1[l // 2], moe_w3[l // 2], moe_w2[l // 2])
    return x
```

```python
import contextlib
import numpy as np
import concourse.bass as bass
import concourse.mybir as mybir
from concourse.bass_utils import run_bass_kernel_spmd

F32 = mybir.dt.float32
BF16 = mybir.dt.bfloat16
AF = mybir.ActivationFunctionType
ALU = mybir.AluOpType
AX = mybir.AxisListType

ENGS = ['pe', 'act', 'dve', 'pool', 'sp']


def _prod(xs):
    r = 1
    for v in xs:
        r *= int(v)
    return r


class Prog:
    def __init__(self, nc, n_dma_sems=48, same_engine_sync=True, relax=False):
        self.nc = nc
        self.same_engine_sync = same_engine_sync
        self.relax_same_engine = relax
        self.relax_min = 512
        self.root = contextlib.ExitStack()
        self.stream = {e: [] for e in ENGS}
        self.cnt = {e: 0 for e in ENGS}
        self.known = {e: {} for e in ENGS}
        self.acc = {}
        self.esem = {e: self.root.enter_context(nc.semaphore('s_' + e)) for e in ENGS if e != 'sp'}
        self.dsem = [self.root.enter_context(nc.semaphore('d_%d' % i)) for i in range(n_dma_sems)]
        self.dval = [0] * n_dma_sems
        self.drr = {'sp': 0, 'pool': 0, 'act': 0}
        self.phases = []
        self.uid = 0
        self.ninst = 0

    def begin_phase(self):
        self.phases.append(contextlib.ExitStack())

    def end_phase(self):
        self.barrier()
        self.phases.pop().close()
        self.acc = {k: v for k, v in self.acc.items() if k.startswith('D:')}

    def _stk(self, persist):
        return self.root if (persist or not self.phases) else self.phases[-1]

    def sb(self, name, shape, dtype=F32, persist=False):
        self.uid += 1
        return self._stk(persist).enter_context(self.nc.sbuf_tensor('%s_%d' % (name, self.uid), list(shape), dtype))

    def ps(self, name, shape, dtype=F32, persist=False):
        self.uid += 1
        es = 2 if dtype == BF16 else 4
        n = _prod(shape[1:])
        nb = (n * es + 2047) // 2048
        t = self._stk(persist).enter_context(
            self.nc.psum_tensor('%s_%d' % (name, self.uid), [128, nb * 2048 // es], dtype))
        v = t[0:shape[0], 0:n]
        if len(shape) == 3:
            v = v.rearrange("p (a b) -> p a b", a=shape[1])
        elif len(shape) == 4:
            v = v.rearrange("p (a b c) -> p a b c", a=shape[1], b=shape[2])
        return v

    def dram(self, name, shape, dtype=F32, kind='Internal'):
        return self.nc.dram_tensor(name, list(shape), dtype, kind=kind)

    @staticmethod
    def _box(ap):
        t = ap.tensor
        nm = t.name
        tn = type(t).__name__
        if tn.startswith('DRam'):
            return [('D:' + nm, 0, 1, 0, 1)]
        if tn.startswith('PSum'):
            es = 2 if ap.dtype == BF16 else 4
            shape = list(t.shape)
            row = _prod(shape[1:])
            off = int(ap.offset)
            pairs = [(int(s), int(c)) for s, c in ap.ap]
            f0 = off % row
            ext = sum(abs(s) * (c - 1) for s, c in pairs[1:])
            b0 = (f0 * es) // 2048
            b1 = ((f0 + ext) * es) // 2048
            return [('PS:%s:%d' % (nm, b), 0, 128, 0, 1) for b in range(b0, b1 + 1)]
        shape = list(t.shape)
        row = _prod(shape[1:])
        off = int(ap.offset)
        pairs = [(int(s), int(c)) for s, c in ap.ap]
        p0 = off // row
        f0 = off % row
        ps_, pc = pairs[0]
        if ps_ == 0:
            pc = 1
        p1 = p0 + pc
        ext = sum(abs(s) * (c - 1) for s, c in pairs[1:])
        sig = (off, tuple(pairs))
        n = _prod([c for _, c in pairs[1:]])
        return [(nm, p0, p1, f0, f0 + ext + 1, sig, n)]

    @staticmethod
    def _ov(a, b):
        return a[1] < b[2] and b[1] < a[2] and a[3] < b[4] and b[3] < a[4]

    @staticmethod
    def _inside(a, b):
        return a[1] >= b[1] and a[2] <= b[2] and a[3] >= b[3] and a[4] <= b[4]

    def _deps(self, rb, wb, E=None):
        deps = set()
        relax = self.relax_same_engine and E in ('act', 'dve', 'pool')
        for b in rb:
            for (rbx, kind, tok) in self.acc.get(b[0], ()):
                if kind == 'w' and self._ov(b, rbx):
                    if (relax and tok[0] == 'e' and tok[1] == E and len(b) > 5 and len(rbx) > 5
                            and b[5] == rbx[5] and b[6] >= self.relax_min):
                        continue
                    deps.add(tok)
        for b in wb:
            sbuf = len(b) > 5
            for (rbx, kind, tok) in self.acc.get(b[0], ()):
                if self._ov(b, rbx):
                    if relax and sbuf and tok[0] == 'e' and tok[1] == E:
                        continue
                    deps.add(tok)
        return deps

    def _record(self, rb, wb, tok):
        for b in wb:
            lst = self.acc.setdefault(b[0], [])
            lst[:] = [r for r in lst if not self._inside(r[0], b)]
            lst.append((b, 'w', tok))
        for b in rb:
            lst = self.acc.setdefault(b[0], [])
            if tok[0] == 'e':
                lst[:] = [r for r in lst if not (r[1] == 'r' and r[2][0] == 'e' and r[2][1] == tok[1]
                                                 and self._inside(r[0], b))]
            lst.append((b, 'r', tok))

    def _emit_waits(self, E, deps):
        need = {}
        for tok in deps:
            if tok[0] == 'e':
                _, f, idx = tok
                if f == E and (E == 'pe' or not self.same_engine_sync):
                    continue
                key = ('e', f)
            else:
                _, si, idx = tok
                key = ('d', si)
            if idx > need.get(key, 0):
                need[key] = idx
        for key, idx in need.items():
            if self.known[E].get(key, 0) >= idx:
                continue
            self.known[E][key] = idx
            sem = self.esem[key[1]] if key[0] == 'e' else self.dsem[key[1]]
            self.stream[E].append(('w', sem, idx))

    def op(self, E, fn, reads=(), writes=()):
        rb = [b for a in reads for b in self._box(a)]
        wb = [b for a in writes for b in self._box(a)]
        wb += [b for b in rb if b[0].startswith('PS:')]
        deps = self._deps(rb, wb, E)
        self._emit_waits(E, deps)
        self.cnt[E] += 1
        tok = ('e', E, self.cnt[E])
        self.stream[E].append(('i', fn, self.esem[E], 1))
        self._record(rb, wb, tok)
        self.ninst += 1

    def dma(self, Q, out, in_, track_dram=True, **kw):
        rb = self._box(in_)
        wb = self._box(out)
        if not track_dram:
            rb = [b for b in rb if not b[0].startswith('D:')]
            wb = [b for b in wb if not b[0].startswith('D:')]
        deps = self._deps(rb, wb)
        self._emit_waits(Q, deps)
        half = len(self.dsem) // 2
        base = half if Q == 'pool' else 0
        si = base + self.drr[Q] % half
        self.drr[Q] += 1
        if self.dval[si] > 0 and self.known[Q].get(('d', si), 0) < self.dval[si]:
            self.known[Q][('d', si)] = self.dval[si]
            self.stream[Q].append(('w', self.dsem[si], self.dval[si]))
        self.dval[si] += 16
        tok = ('d', si, self.dval[si])
        self.stream[Q].append(('i', (lambda eng, o=out, i=in_, k=kw: eng.dma_start(out=o, in_=i, **k)),
                               self.dsem[si], 16))
        self._record(rb, wb, tok)
        self.ninst += 1

    def barrier(self):
        for E in ENGS:
            deps = set()
            for f in ENGS:
                if f != 'sp' and self.cnt[f] > 0:
                    deps.add(('e', f, self.cnt[f]))
            for si, v in enumerate(self.dval):
                if v > 0:
                    deps.add(('d', si, v))
            self._emit_waits(E, deps)

    def finish(self):
        self.barrier()

    def emit(self):
        nc = self.nc
        streams = self.stream

        def run(eng, items):
            for it in items:
                if it[0] == 'w':
                    eng.wait_ge(it[1], it[2])
                else:
                    try:
                        ins = it[1](eng)
                    except Exception:
                        d = it[1].__defaults__
                        print('FAILED INSTR defaults:', [(getattr(x, 'tensor', None) and x.tensor.name, getattr(x, 'shape', None)) for x in (d or [])])
                        raise
                    ins.then_inc(it[2], it[3])

        with nc.Block() as block:
            @block.tensor
            def _(eng):
                run(eng, streams['pe'])

            @block.scalar
            def _(eng):
                run(eng, streams['act'])

            @block.vector
            def _(eng):
                run(eng, streams['dve'])

            @block.gpsimd
            def _(eng):
                run(eng, streams['pool'])

            @block.sync
            def _(eng):
                run(eng, streams['sp'])
        self.root.close()

    def mm(self, out, lhsT, rhs, start=True, stop=True):
        self.op('pe', lambda e: e.matmul(out, lhsT, rhs, start=start, stop=stop),
                reads=[lhsT, rhs], writes=[out])

    def tr(self, out, in_, ident):
        self.op('pe', lambda e: e.transpose(out, in_, ident), reads=[in_, ident], writes=[out])

    def act(self, out, in_, func, bias=None, scale=None, accum_out=None):
        kw = {}
        reads = [in_]
        writes = [out]
        if bias is not None:
            kw['bias'] = bias
            if not isinstance(bias, (int, float)):
                reads.append(bias)
        if scale is not None:
            kw['scale'] = scale
            if not isinstance(scale, (int, float)):
                reads.append(scale)
        if accum_out is not None:
            kw['accum_out'] = accum_out
            writes.append(accum_out)
        self.op('act', lambda e: e.activation(out, in_, func, **kw), reads=reads, writes=writes)

    def tt(self, out, in0, in1, op, eng='dve'):
        self.op(eng, lambda e: e.tensor_tensor(out, in0, in1, op), reads=[in0, in1], writes=[out])

    def ts(self, out, in0, s1, op0, s2=None, op1=None, eng='dve', accum_out=None):
        reads = [in0]
        writes = [out]
        if not isinstance(s1, (int, float)):
            reads.append(s1)
        if s2 is not None and not isinstance(s2, (int, float)):
            reads.append(s2)
        kw = {}
        if accum_out is not None:
            kw['accum_out'] = accum_out
            writes.append(accum_out)
        o1 = op1 if op1 is not None else ALU.bypass
        self.op(eng, lambda e: e.tensor_scalar(out, in0, s1, s2, op0, o1, **kw), reads=reads, writes=writes)

    def stt(self, out, in0, scalar, in1, op0, op1, eng='dve'):
        reads = [in0, in1]
        if not isinstance(scalar, (int, float)):
            reads.append(scalar)
        self.op(eng, lambda e: e.scalar_tensor_tensor(out, in0, scalar, in1, op0, op1), reads=reads, writes=[out])

    def copy(self, out, in_, eng='dve'):
        if eng == 'act':
            self.op('act', lambda e: e.copy(out, in_), reads=[in_], writes=[out])
        else:
            self.op(eng, lambda e: e.tensor_copy(out, in_), reads=[in_], writes=[out])

    def memset(self, ap, val, eng='dve'):
        self.op(eng, lambda e: e.memset(ap, val), reads=[], writes=[ap])

    def recip(self, out, in_):
        self.op('dve', lambda e: e.reciprocal(out, in_), reads=[in_], writes=[out])

    def reduce(self, out, in_, op, axis=None):
        ax = axis if axis is not None else AX.X
        self.op('dve', lambda e: e.tensor_reduce(out, in_, ax, op), reads=[in_], writes=[out])

    def scan(self, out, d0, d1, init, op0, op1):
        reads = [d0, d1]
        if not isinstance(init, (int, float)):
            reads.append(init)
        self.op('dve', lambda e: e.tensor_tensor_scan(out, d0, d1, init, op0, op1), reads=reads, writes=[out])


D = 1024
S = 2048
NT = 16
NCH = 32
IN_COLS = 10552
C_RWKV, C_SSM, C_ATT, C_GLA, C_GATE = 0, 1824, 3368, 4904, 6456
EPS = 1e-6


class K:
    def __init__(self, nc, dbg=None, stop_after=None):
        self.nc = nc
        self.P = Prog(nc)
        self.dbg = dbg or []
        self.stop_after = stop_after
        self.inp = {}
        self.out = {}

    def din(self, name, shape, dtype=F32):
        if name in self.inp:
            return self.inp[name]
        t = self.nc.dram_tensor(name, list(shape), dtype, kind="ExternalInput").ap()
        self.inp[name] = t
        return t

    def dout(self, name, shape):
        t = self.nc.dram_tensor(name, list(shape), F32, kind="ExternalOutput").ap()
        self.out[name] = t
        return t

    def dscr(self, name, shape, dtype=F32):
        return self.nc.dram_tensor(name, list(shape), dtype, kind="Internal").ap()

    def consts(self):
        P = self.P
        self.ident = P.sb('ident', [128, 128], F32, persist=True)
        self.identb = P.sb('identb', [128, 128], BF16, persist=True)
        self.blktri = P.sb('blktri', [128, 128], F32, persist=True)
        self.blktrib = P.sb('blktrib', [128, 128], BF16, persist=True)
        self.blkones = P.sb('blkones', [128, 128], F32, persist=True)
        self.ones = P.sb('ones', [128, 128], F32, persist=True)
        c_ident = self.din('c_ident', [128, 128])
        c_blktri = self.din('c_blktri', [128, 128])
        c_blkones = self.din('c_blkones', [128, 128])
        P.dma('sp', self.ident[:], c_ident)
        P.dma('pool', self.identb[:], c_ident)
        P.dma('sp', self.blktri[:], c_blktri)
        P.dma('pool', self.blktrib[:], c_blktri)
        P.dma('sp', self.blkones[:], c_blkones)
        P.memset(self.ones[:], 1.0)
        self.ostage = [P.sb('ostage%d' % i, [128, 4, 128], BF16, persist=True) for i in range(2)]
        self.ostage_i = 0

    def load_w(self, dst, src, q='pool'):
        self.P.dma(q, dst, src.rearrange("(k p) c -> p k c", p=128))

    def norm_T(self, x_dram, g_row, hT, router=None, x_sb=None):
        P = self.P
        P.begin_phase()
        g_bc = P.sb('g_bc', [128, D])
        P.dma('sp', g_bc[:], g_row.partition_broadcast(128))
        if x_sb is None:
            x_sb = P.sb('xall', [128, NT, D])
            for tt in range(NT):
                P.dma('sp', x_sb[:, tt, :], x_dram[tt * 128:(tt + 1) * 128, :])
        junk = P.sb('junk', [128, D])
        hb = [P.sb('hb%d' % i, [128, D], BF16) for i in range(2)]
        ss = P.sb('ss', [128, NT])
        rstd = P.sb('rstd', [128, NT])
        ptb = [P.ps('ptb%d' % i, [128, 8, 128], BF16) for i in range(2)]
        if router is not None:
            wr_d, logits = router
            wr = P.sb('wr', [128, 8, 8])
            P.dma('sp', wr[:], wr_d.rearrange("(k p) c -> p k c", p=128))
            h32 = [P.sb('h32_%d' % i, [128, D]) for i in range(2)]
            h32T = [P.sb('h32T_%d' % i, [128, 8, 128]) for i in range(2)]
            pt32 = [P.ps('pt32_%d' % i, [128, 4, 128]) for i in range(2)]
            plog = P.ps('plog', [128, 8])
        for tt in range(NT):
            P.act(junk[:], x_sb[:, tt, :], AF.Square, accum_out=ss[:, tt:tt + 1])
        P.act(rstd[:], ss[:], AF.Sqrt, bias=EPS, scale=1.0 / D)
        P.recip(rstd[:], rstd[:])
        for tt in range(NT):
            b = tt % 2
            xcur = x_sb[:, tt, :]
            P.stt(hb[b][:], xcur, rstd[:, tt:tt + 1], g_bc[:], ALU.mult, ALU.mult)
            for kc in range(8):
                P.tr(ptb[b][:, kc, :], hb[b][:, kc * 128:(kc + 1) * 128], self.identb[:])
            if tt % 2 == 0:
                P.copy(hT[:, :, tt * 128:(tt + 1) * 128], ptb[b][:], eng='act')
            else:
                P.copy(hT[:, :, tt * 128:(tt + 1) * 128], ptb[b][:], eng='dve')
            if router is not None:
                P.stt(h32[b][:], xcur, rstd[:, tt:tt + 1], g_bc[:], ALU.mult, ALU.mult)
                for half in range(2):
                    for k4 in range(4):
                        kc = half * 4 + k4
                        P.tr(pt32[half][:, k4, :], h32[b][:, kc * 128:(kc + 1) * 128], self.ident[:])
                    P.copy(h32T[b][:, half * 4:(half + 1) * 4, :], pt32[half][:], eng='act' if half == 0 else 'dve')
                for kc in range(8):
                    P.mm(plog[:], h32T[b][:, kc, :], wr[:, kc, :], start=(kc == 0), stop=(kc == 7))
                P.copy(logits[:, tt, :], plog[:])
        P.end_phase()

    def to_T(self, o_bf, oT, tt, ptr, eng='act'):
        P = self.P
        for c in range(4):
            P.tr(ptr[:, c, :], o_bf[:, c * 128:(c + 1) * 128], self.identb[:])
        st = self.ostage[self.ostage_i % 2]
        self.ostage_i += 1
        P.copy(st[:], ptr, eng=eng)
        P.dma('sp', oT[tt], st[:], track_dram=False)

    def to_T_sb(self, o_bf, oT, tt, ptr, eng='act'):
        P = self.P
        for c in range(4):
            P.tr(ptr[:, c, :], o_bf[:, c * 128:(c + 1) * 128], self.identb[:])
        P.copy(oT[:, :, tt * 128:(tt + 1) * 128], ptr[:], eng=eng)

    def proj_tm(self, pout, hT, w, tt, ncols, c0=0):
        for kc in range(8):
            self.P.mm(pout, hT[:, kc, tt * 128:(tt + 1) * 128], w[:, kc, c0:c0 + ncols], start=(kc == 0), stop=(kc == 7))

    def proj_fm(self, pout, hT, w, tb, c0, m, ntok=512):
        for kc in range(8):
            self.P.mm(pout, w[:, kc, c0:c0 + m], hT[:, kc, tb * ntok:(tb + 1) * ntok], start=(kc == 0), stop=(kc == 7))

    def attention(self, l, hT, oT):
        P = self.P
        w_in = self.inp['w_in']
        P.begin_phase()
        qT = P.sb('qT', [128, 4, S], BF16)
        kT = P.sb('kT', [128, 4, S], BF16)
        vaug = P.sb('vaug', [128, NT, 8, 65], BF16)
        EBm = P.sb('EBm', [128, 8, 5, 128], BF16)
        P.begin_phase()
        EB = P.sb('EB', [128, 8, 5, 128])
        amask = P.sb('amask', [128, 5, 128])
        P.dma('sp', EB[:], self.inp['c_attbias'])
        P.dma('sp', amask[:], self.inp['c_attmask'])
        P.ts(amask[:], amask[:], 30000.0, ALU.mult, -30000.0, ALU.add)
        P.stt(EBm[:], EB[:], 8.0, amask[:].unsqueeze(1).broadcast_to([128, 8, 5, 128]), ALU.mult, ALU.add)
        P.end_phase()
        P.memset(vaug[:, :, :, 64:65], 1.0)
        P.begin_phase()
        w = P.sb('w_att', [128, 8, 1536], BF16)
        self.load_w(w[:], w_in[l, :, C_ATT:C_ATT + 1536])
        gq = P.sb('gq', [128, 64])
        gk = P.sb('gk', [128, 64])
        P.dma('sp', gq[:], self.inp['att_q_gain'][l].partition_broadcast(128))
        P.dma('sp', gk[:], self.inp['att_k_gain'][l].partition_broadcast(128))
        pq = [P.ps('pq%d' % i, [128, 512]) for i in range(3)]
        ptr = [P.ps('ptr%d' % i, [128, 4, 128], BF16) for i in range(2)]
        sq = [P.sb('sq%d' % i, [128, 8, 64]) for i in range(2)]
        ssq = [P.sb('ssq%d' % i, [128, 8]) for i in range(2)]
        tmp = [P.sb('tmp%d' % i, [128, 8, 64]) for i in range(2)]
        nb = [P.sb('nb%d' % i, [128, 512], BF16) for i in range(2)]
        qk = [(gq, qT), (gk, kT)]
        for tt in range(NT):
            for i in range(3):
                self.proj_tm(pq[i][:], hT, w, tt, 512, c0=i * 512)
            pv3 = [pq[i][:].rearrange("p (h d) -> p h d", h=8) for i in range(2)]
            for i in range(2):
                P.act(sq[i][:], pv3[i], AF.Square)
            for i in range(2):
                P.reduce(ssq[i][:], sq[i][:], ALU.add)
            for i in range(2):
                P.act(ssq[i][:], ssq[i][:], AF.Sqrt, bias=EPS, scale=1.0 / 64)
            for i in range(2):
                P.recip(ssq[i][:], ssq[i][:])
            for i in range(2):
                P.tt(tmp[i][:], pv3[i], ssq[i][:].unsqueeze(2).broadcast_to([128, 8, 64]), ALU.mult)
            for i in range(2):
                P.tt(nb[i][:].rearrange("p (h d) -> p h d", h=8), tmp[i][:], qk[i][0][:].unsqueeze(1).broadcast_to([128, 8, 64]), ALU.mult)
            for i in range(2):
                self.to_T_sb(nb[i], qk[i][1], tt, ptr[i], eng='act')
            P.copy(vaug[:, tt, :, 0:64], pq[2][:].rearrange("p (h d) -> p h d", h=8), eng='act')
        P.end_phase()
        P.begin_phase()
        pS0 = [P.ps('pS0_%d' % i, [128, 4, 128]) for i in range(2)]
        pS1 = [P.ps('pS1_%d' % i, [128, 128]) for i in range(2)]
        po = [P.ps('po%d' % i, [128, 4, 65]) for i in range(2)]
        ptr = P.ps('ptr', [128, 4, 128], BF16)
        Pm = [P.sb('Pm%d' % i, [128, 5, 128], BF16) for i in range(2)]
        rec = P.sb('rec', [128, 4, 1])
        obf = [P.sb('obf%d' % i, [128, 512], BF16) for i in range(2)]
        iters = [(qt, h) for qt in range(NT) for h in range(8)]

        def emit_S(it):
            qt, h = iters[it]
            hp, pb = h // 2, (h % 2) * 64
            b = it % 2
            for j in range(5):
                kt = qt - 4 + j
                if kt < 0:
                    continue
                dst = pS0[b][:, j, :] if j < 4 else pS1[b][:]
                P.mm(dst, kT[pb:pb + 64, hp, kt * 128:(kt + 1) * 128], qT[pb:pb + 64, hp, qt * 128:(qt + 1) * 128],
                     start=True, stop=False)
                P.mm(dst, self.identb[:], EBm[:, h, j, :], start=False, stop=True)

        emit_S(0)
        for it, (qt, h) in enumerate(iters):
            ob = obf[qt % 2]
            b = it % 2
            js = [j for j in range(5) if qt - 4 + j >= 0]
            j0 = js[0]
            if it + 1 < len(iters):
                emit_S(it + 1)
            if j0 < 4:
                P.act(Pm[b][:, j0:4, :], pS0[b][:, j0:4, :], AF.Exp, scale=0.125)
            P.act(Pm[b][:, 4, :], pS1[b][:], AF.Exp, scale=0.125)
            pob = po[(h // 4) % 2]
            for j in js:
                kt = qt - 4 + j
                P.mm(pob[:, h % 4, :], Pm[b][:, j, :], vaug[:, kt, h, :], start=(j == j0), stop=(j == 4))
            if h % 4 == 3:
                P.recip(rec[:], pob[:, :, 64:65])
                h0 = h - 3
                P.tt(ob[:, h0 * 64:(h0 + 4) * 64].rearrange("p (h d) -> p h d", h=4), pob[:, :, 0:64],
                     rec[:].broadcast_to([128, 4, 64]), ALU.mult)
            if h == 7:
                self.to_T(ob, oT, qt, ptr, eng='act')
        P.end_phase()
        P.end_phase()


def gla(self, l, hT, oT):
    P = self.P
    w_in = self.inp['w_in']
    P.begin_phase()
    w = P.sb('w_gla', [128, 8, 1552], BF16)
    self.load_w(w[:], w_in[l, :, C_GLA:C_GLA + 1552])
    gup = P.sb('gup', [16, 256], BF16)
    P.dma('pool', gup[:], self.inp['gla_gate_up'][l])
    gbias = P.sb('gbias', [1, 256], BF16)
    P.dma('pool', gbias[:], self.inp['gla_gate_bias'][l:l + 1, :])
    onesb = P.sb('onesb', [1, 128], BF16)
    P.memset(onesb[:], 1.0)
    nw = P.sb('nw', [128, 128])
    P.dma('sp', nw[:], self.inp['gla_norm_w'][l].partition_broadcast(128))
    B0 = P.ps('B0', [128, 512]); B1 = P.ps('B1', [128, 512]); B2 = P.ps('B2', [128, 512]); B3 = P.ps('B3', [128, 512])
    B4 = P.ps('B4', [128, 512]); B5 = P.ps('B5', [128, 512]); B6 = P.ps('B6', [128, 512])
    B7 = P.ps('B7', [128, 1024], BF16)
    pla = B0[:, 0:256]
    pxg = B0[0:16, 256:384]
    p64 = B1[0:64, :].rearrange("p (h t) -> p h t", h=4)
    pv = B2[:]
    pa = B3[:].rearrange("p (h t) -> p h t", h=4)
    pcA = B4[0:64, :].rearrange("p (h t) -> p h t", h=4)
    pcB = B5[0:64, :].rearrange("p (h t) -> p h t", h=4)
    po = B6[:].rearrange("p (h t) -> p h t", h=4)
    ptk = B7[:, 0:256].rearrange("p (h t) -> p h t", h=4)
    ptr = B7[:, 512:1024].rearrange("p (h t) -> p h t", h=4)
    xg = P.sb('xg', [16, 128], BF16)
    ee = P.sb('ee', [128, 256])
    la = P.sb('la', [128, 256])
    E1 = P.sb('E1', [64, 4, 128]); E2 = P.sb('E2', [64, 4, 128])
    ebl = P.sb('ebl', [64, 4, 2])
    qgA = P.sb('qgA', [64, 4, 128], BF16); qgB = P.sb('qgB', [64, 4, 128], BF16)
    qg = P.sb('qg', [64, 4, 128], BF16)
    kg = P.sb('kg', [64, 4, 128], BF16); kd = P.sb('kd', [64, 4, 128], BF16)
    kdt = P.sb('kdt', [128, 4, 64], BF16)
    vt = P.sb('vt', [128, 512], BF16)
    sg = P.sb('sg', [128, 512])
    att = P.sb('att', [128, 4, 128], BF16)
    Sm = P.sb('Sm', [64, 4, 128])
    SAb = P.sb('SAb', [64, 4, 128], BF16); SBb = P.sb('SBb', [64, 4, 128], BF16)
    sq = P.sb('sq', [128, 4, 128]); ssq = P.sb('ssq', [128, 4])
    o1 = P.sb('o1', [128, 4, 128]); obf = P.sb('obf', [128, 512], BF16)
    P.memset(qgA[:], 0.0); P.memset(qgB[:], 0.0); P.memset(Sm[:], 0.0)
    for tt in range(NT):
        ts_ = slice(tt * 128, (tt + 1) * 128)
        for kc in range(8):
            P.mm(pxg, w[:, kc, 1024:1040], hT[:, kc, ts_], start=(kc == 0), stop=(kc == 7))
        P.copy(xg[:], pxg, eng='act')
        self.proj_tm(pv, hT, w, tt, 512, c0=512)
        P.copy(vt[:], pv, eng='act')
        self.proj_tm(pv, hT, w, tt, 512, c0=1040)
        P.act(sg[:], pv, AF.Silu)
        P.mm(pla, xg[:], gup[:], start=True, stop=False)
        P.mm(pla, onesb[:], gbias[:], start=False, stop=True)
        P.act(ee[:], pla, AF.Exp, scale=-1.0)
        P.act(ee[:], ee[:], AF.Ln, bias=1.0)
        P.ts(la[:], ee[:], -1.0 / 16.0, ALU.mult)
        for h in range(4):
            P.mm(p64[:, h, :], la[:, h * 64:(h + 1) * 64], self.blktri[:])
        P.act(E1[:], p64, AF.Exp)
        P.act(E2[:], p64, AF.Exp, scale=-1.0)
        P.act(ebl[:], B1[0:64, :].rearrange("p (h c t) -> p h c t", h=4, c=2)[:, :, :, 63], AF.Exp)
        for h in range(4):
            for kc in range(8):
                P.mm(p64[:, h, :], w[:, kc, h * 64:(h + 1) * 64], hT[:, kc, ts_], start=(kc == 0), stop=(kc == 7))
        P.stt(qg[:], p64, 0.125, E1[:], ALU.mult, ALU.mult)
        P.copy(qgA[:, :, 0:64], qg[:, :, 0:64], eng='act')
        P.copy(qgB[:, :, 64:128], qg[:, :, 64:128], eng='act')
        for h in range(4):
            for kc in range(8):
                P.mm(p64[:, h, :], w[:, kc, 256 + h * 64:256 + (h + 1) * 64], hT[:, kc, ts_], start=(kc == 0), stop=(kc == 7))
        P.tt(kg[:], p64, E2[:], ALU.mult)
        P.tt(kd[:].rearrange("p h (c t) -> p h c t", c=2), kg[:].rearrange("p h (c t) -> p h c t", c=2),
             ebl[:].unsqueeze(3).broadcast_to([64, 4, 2, 64]), ALU.mult)
        for h in range(4):
            P.tr(ptk[:, h, :], kd[:, h, :], self.identb[0:64, 0:64])
        P.copy(kdt[:], ptk, eng='act')
        for h in range(4):
            P.mm(pa[:, h, :], kg[:, h, :], qg[:, h, :])
        P.tt(att[:], pa, self.blktri[:].unsqueeze(1).broadcast_to([128, 4, 128]), ALU.mult)
        for h in range(4):
            P.mm(pcA[:, h, :], kdt[0:64, h, :], vt[0:64, h * 128:(h + 1) * 128])
            P.mm(pcB[:, h, :], kdt[64:128, h, :], vt[64:128, h * 128:(h + 1) * 128])
        P.copy(SAb[:], Sm[:], eng='act')
        P.tt(Sm[:], Sm[:], ebl[:, :, 0:1].broadcast_to([64, 4, 128]), ALU.mult)
        P.tt(Sm[:], Sm[:], pcA, ALU.add)
        P.copy(SBb[:], Sm[:], eng='act')
        for h in range(4):
            P.mm(po[:, h, :], att[:, h, :], vt[:, h * 128:(h + 1) * 128], start=True, stop=False)
            P.mm(po[:, h, :], qgA[:, h, :], SAb[:, h, :], start=False, stop=False)
            P.mm(po[:, h, :], qgB[:, h, :], SBb[:, h, :], start=False, stop=True)
        P.tt(Sm[:], Sm[:], ebl[:, :, 1:2].broadcast_to([64, 4, 128]), ALU.mult)
        P.tt(Sm[:], Sm[:], pcB, ALU.add)
        P.act(sq[:], po, AF.Square)
        P.reduce(ssq[:], sq[:], ALU.add)
        P.act(ssq[:], ssq[:], AF.Sqrt, bias=EPS, scale=1.0 / 128)
        P.recip(ssq[:], ssq[:])
        P.tt(o1[:], po, ssq[:].unsqueeze(2).broadcast_to([128, 4, 128]), ALU.mult)
        P.tt(o1[:], o1[:], nw[:].unsqueeze(1).broadcast_to([128, 4, 128]), ALU.mult)
        P.tt(obf[:].rearrange("p (h t) -> p h t", h=4), o1[:], sg[:].rearrange("p (h t) -> p h t", h=4), ALU.mult)
        self.to_T(obf, oT, tt, ptr, eng='act')
    P.end_phase()


K.gla = gla


def mamba(self, l, hT, oT):
    P = self.P
    w_in = self.inp['w_in']
    P.begin_phase()
    w = P.sb('w_ssm', [128, 8, 1544], BF16)
    self.load_w(w[:], w_in[l, :, C_SSM:C_SSM + 1544])
    cw = P.sb('cw', [128, 8, 4])
    P.dma('sp', cw[:], self.inp['h_convw'][l])
    cbias = P.sb('cbias', [128, 8])
    P.dma('sp', cbias[:], self.inp['h_convb'][l])
    dtb = P.sb('dtb', [128, 8]); abc = P.sb('abc', [128, 8]); dsk = P.sb('dsk', [128, 8])
    P.dma('sp', dtb[:], self.inp['ssm_dt_bias'][l].partition_broadcast(128))
    P.dma('sp', abc[:], self.inp['ssm_a_log'][l].partition_broadcast(128))
    P.dma('sp', dsk[:], self.inp['ssm_d'][l].partition_broadcast(128))
    P.act(abc[:], abc[:], AF.Exp)
    P.ts(abc[:], abc[:], -1.0, ALU.mult)
    nw = P.sb('nw', [128, 512])
    P.dma('sp', nw[:], self.inp['ssm_norm_w'][l].partition_broadcast(128))
    negm = P.sb('negm', [128, 128])
    P.dma('sp', negm[:], self.inp['c_negmask'])
    BR = P.ps('BR', [128, 8, 128])
    Bs = P.ps('Bs', [128, 512])
    BstA = P.ps('BstA', [128, 8, 64]); BstB = P.ps('BstB', [128, 8, 64])
    By = P.ps('By', [128, 8, 64])
    Bg = P.ps('Bg', [128, 512])
    Bt = P.ps('Bt', [128, 1024], BF16)
    pdt = Bs[:, 0:8]; pacs = Bs[:, 8:16]; ptot = Bs[:, 16:24]
    pcb = Bs[:, 256:512].rearrange("p (g t) -> p g t", g=2)
    ptx = Bt[:, 0:512].rearrange("p (c t) -> p c t", c=4)
    ptb_ = Bt[:, 0:256].rearrange("p (c t) -> p c t", c=2)
    ptr = Bt[:, 0:512].rearrange("p (c t) -> p c t", c=4)
    raw = P.sb('raw', [128, 8, 131])
    acc = P.sb('acc', [128, 8, 128]); tmp = P.sb('tmp', [128, 8, 128])
    xsb = P.sb('xsb', [128, 4, 128], BF16); Bb = P.sb('Bb', [128, 2, 128], BF16); Cb = P.sb('Cb', [128, 2, 128], BF16)
    xs_tm = P.sb('xs_tm', [128, 8, 64], BF16)
    bm_tm = P.sb('bm_tm', [128, 2, 128], BF16)
    dt = P.sb('dt', [128, 8]); dA = P.sb('dA', [128, 8]); acs = P.sb('acs', [128, 8]); dte = P.sb('dte', [128, 8])
    xdt = P.sb('xdt', [128, 8, 64], BF16); xdtd = P.sb('xdtd', [128, 8, 64], BF16)
    Dg = P.sb('Dg', [128, 8, 128]); seg = P.sb('seg', [128, 8, 128]); Lm = P.sb('Lm', [128, 8, 128])
    eacs = P.sb('eacs', [128, 8, 128])
    MT = P.sb('MT', [128, 8, 128], BF16)
    cmhA = P.sb('cmhA', [128, 8, 128], BF16); cmhB = P.sb('cmhB', [128, 8, 128], BF16)
    Em = P.sb('Em', [128, 8, 64]); EAb = P.sb('EAb', [128, 8, 64], BF16); EBb = P.sb('EBb', [128, 8, 64], BF16)
    y2 = P.sb('y2', [128, 8, 64]); sgt = P.sb('sgt', [128, 512]); sq = P.sb('sq', [128, 2, 256]); ssq = P.sb('ssq', [128, 2])
    obf = P.sb('obf', [128, 512], BF16)
    P.memset(raw[:], 0.0); P.memset(cmhA[:], 0.0); P.memset(cmhB[:], 0.0); P.memset(Em[:], 0.0)
    for tt in range(NT):
        ts_ = slice(tt * 128, (tt + 1) * 128)
        if tt > 0:
            P.copy(raw[:, :, 0:3], raw[:, :, 128:131], eng='act')
        for j in range(8):
            for kc in range(8):
                P.mm(BR[:, j, :], w[:, kc, 512 + j * 128:512 + (j + 1) * 128], hT[:, kc, ts_], start=(kc == 0), stop=(kc == 7))
        P.copy(raw[:, 0:4, 3:131], BR[:, 0:4, :], eng='act')
        P.copy(raw[:, 4:8, 3:131], BR[:, 4:8, :], eng='act')
        P.tt(acc[:], raw[:, :, 3:131], cw[:, :, 3:4].broadcast_to([128, 8, 128]), ALU.mult)
        for i in range(3):
            P.tt(tmp[:], raw[:, :, i:i + 128], cw[:, :, i:i + 1].broadcast_to([128, 8, 128]), ALU.mult)
            P.tt(acc[:], acc[:], tmp[:], ALU.add)
        P.tt(acc[:], acc[:], cbias[:].unsqueeze(2).broadcast_to([128, 8, 128]), ALU.add)
        P.act(xsb[:], acc[:, 0:4, :], AF.Silu)
        P.act(Bb[:], acc[:, 4:6, :], AF.Silu)
        P.act(Cb[:], acc[:, 6:8, :], AF.Silu)
        for c in range(4):
            P.tr(ptx[:, c, :], xsb[:, c, :], self.identb[:])
        P.copy(xs_tm[:].rearrange("p h d -> p (h d)"), Bt[:, 0:512], eng='act')
        for g in range(2):
            P.tr(ptb_[:, g, :], Bb[:, g, :], self.identb[:])
        P.copy(bm_tm[:], ptb_, eng='act')
        self.proj_tm(pdt, hT, w, tt, 8, c0=1536)
        P.tt(dt[:], pdt, dtb[:], ALU.add)
        P.act(dt[:], dt[:], AF.Exp)
        P.act(dt[:], dt[:], AF.Ln, bias=1.0)
        P.tt(dA[:], dt[:], abc[:], ALU.mult)
        P.mm(pacs, self.blktri[:], dA[:])
        P.mm(ptot, self.blkones[:], dA[:])
        P.copy(acs[:], pacs)
        P.tt(dte[:], ptot, acs[:], ALU.subtract)
        P.act(dte[:], dte[:], AF.Exp)
        P.tt(xdt[:], xs_tm[:], dt[:].unsqueeze(2).broadcast_to([128, 8, 64]), ALU.mult)
        P.tt(xdtd[:], xdt[:], dte[:].unsqueeze(2).broadcast_to([128, 8, 64]), ALU.mult)
        P.tt(Dg[:], self.ident[:].unsqueeze(1).broadcast_to([128, 8, 128]), acs[:].unsqueeze(2).broadcast_to([128, 8, 128]), ALU.mult)
        P.mm(BR[:, 0:4, :], self.ones[:], Dg[:, 0:4, :])
        P.mm(BR[:, 4:8, :], self.ones[:], Dg[:, 4:8, :])
        P.tt(seg[:], BR[:], acs[:].unsqueeze(2).broadcast_to([128, 8, 128]), ALU.subtract)
        P.tt(seg[:], seg[:], negm[:].unsqueeze(1).broadcast_to([128, 8, 128]), ALU.add)
        P.act(Lm[:], seg[:], AF.Exp)
        P.act(eacs[:], BR[:], AF.Exp)
        for g in range(2):
            P.mm(pcb[:, g, :], Bb[:, g, :], Cb[:, g, :])
        P.tt(MT[:].rearrange("p (g e) t -> p g e t", g=2), Lm[:].rearrange("p (g e) t -> p g e t", g=2),
             pcb.unsqueeze(2).broadcast_to([128, 2, 4, 128]), ALU.mult)
        P.tt(cmhA[:, :, 0:64].rearrange("p (g e) t -> p g e t", g=2), eacs[:, :, 0:64].rearrange("p (g e) t -> p g e t", g=2),
             Cb[:, :, 0:64].unsqueeze(2).broadcast_to([128, 2, 4, 64]), ALU.mult)
        P.tt(cmhB[:, :, 64:128].rearrange("p (g e) t -> p g e t", g=2), eacs[:, :, 64:128].rearrange("p (g e) t -> p g e t", g=2),
             Cb[:, :, 64:128].unsqueeze(2).broadcast_to([128, 2, 4, 64]), ALU.mult)
        for g in range(2):
            P.mm(BstA[:, g * 4:(g + 1) * 4, :], bm_tm[0:64, g, :], xdtd[0:64, g * 4:(g + 1) * 4, :])
            P.mm(BstB[:, g * 4:(g + 1) * 4, :], bm_tm[64:128, g, :], xdtd[64:128, g * 4:(g + 1) * 4, :])
        P.copy(EAb[:], Em[:], eng='act')
        P.tt(Em[:], Em[:], eacs[:, :, 63:64].broadcast_to([128, 8, 64]), ALU.mult)
        P.tt(Em[:], Em[:], BstA[:], ALU.add)
        P.copy(EBb[:], Em[:], eng='act')
        for h in range(8):
            P.mm(By[:, h, :], MT[:, h, :], xdt[:, h, :], start=True, stop=False)
            P.mm(By[:, h, :], cmhA[:, h, :], EAb[:, h, :], start=False, stop=False)
            P.mm(By[:, h, :], cmhB[:, h, :], EBb[:, h, :], start=False, stop=True)
        P.tt(Em[:], Em[:], eacs[:, :, 127:128].broadcast_to([128, 8, 64]), ALU.mult)
        P.tt(Em[:], Em[:], BstB[:], ALU.add)
        P.tt(y2[:], xs_tm[:], dsk[:].unsqueeze(2).broadcast_to([128, 8, 64]), ALU.mult)
        P.tt(y2[:], y2[:], By[:], ALU.add)
        self.proj_tm(Bg[:], hT, w, tt, 512, c0=0)
        P.act(sgt[:], Bg[:], AF.Silu)
        y2f = y2[:].rearrange("p h d -> p (h d)")
        P.tt(y2f, y2f, sgt[:], ALU.mult)
        y2g = y2[:].rearrange("p (g e) d -> p g (e d)", g=2)
        P.act(sq[:], y2g, AF.Square)
        P.reduce(ssq[:], sq[:], ALU.add)
        P.act(ssq[:], ssq[:], AF.Sqrt, bias=EPS, scale=1.0 / 256)
        P.recip(ssq[:], ssq[:])
        P.tt(sq[:], y2g, ssq[:].unsqueeze(2).broadcast_to([128, 2, 256]), ALU.mult)
        P.tt(obf[:], sq[:].rearrange("p g t -> p (g t)"), nw[:], ALU.mult)
        self.to_T(obf, oT, tt, ptr, eng='act')
    P.end_phase()


K.mamba = mamba


def mamba2(self, l, hT, oT):
    P = self.P
    w_in = self.inp['w_in']
    P.begin_phase()
    w = P.sb('w_ssm', [128, 8, 1544], BF16)
    self.load_w(w[:], w_in[l, :, C_SSM:C_SSM + 1544])
    cw = P.sb('cw', [128, 8, 4])
    P.dma('sp', cw[:], self.inp['h_convw'][l])
    cbias = P.sb('cbias', [128, 8])
    P.dma('sp', cbias[:], self.inp['h_convb'][l])
    dtb = P.sb('dtb', [128, 8]); abc = P.sb('abc', [128, 8]); dsk = P.sb('dsk', [128, 8])
    P.dma('sp', dtb[:], self.inp['ssm_dt_bias'][l].partition_broadcast(128))
    P.dma('sp', abc[:], self.inp['ssm_a_log'][l].partition_broadcast(128))
    P.dma('sp', dsk[:], self.inp['ssm_d'][l].partition_broadcast(128))
    P.act(abc[:], abc[:], AF.Exp)
    P.ts(abc[:], abc[:], -1.0, ALU.mult)
    nw = P.sb('nw', [128, 512])
    P.dma('sp', nw[:], self.inp['ssm_norm_w'][l].partition_broadcast(128))
    negm = P.sb('negm', [128, 128])
    P.dma('sp', negm[:], self.inp['c_negmask'])
    BR = P.ps('BR', [128, 8, 128])
    Bs = P.ps('Bs', [128, 512])
    BstA = P.ps('BstA', [128, 8, 64]); BstB = P.ps('BstB', [128, 8, 64])
    By = P.ps('By', [128, 8, 64])
    Bc = P.ps('Bc', [128, 4, 128])
    Bt = P.ps('Bt', [128, 1024], BF16)
    pdt = Bs[:, 0:8]; pacs = Bs[:, 8:16]; ptot = Bs[:, 16:24]
    pcb = Bs[:, 256:512].rearrange("p (g t) -> p g t", g=2)
    ptx = Bt[:, 0:512].rearrange("p (c t) -> p c t", c=4)
    ptb_ = Bt[:, 0:256].rearrange("p (c t) -> p c t", c=2)
    ptr = Bt[:, 0:512].rearrange("p (c t) -> p c t", c=4)
    raw = P.sb('raw', [128, 8, 131])
    acc = P.sb('acc', [128, 8, 128]); tmp = P.sb('tmp', [128, 8, 128])
    xsb = P.sb('xsb', [128, 4, 128], BF16); Bb = P.sb('Bb', [128, 2, 128], BF16); Cb = P.sb('Cb', [128, 2, 128], BF16)
    xs_tm = P.sb('xs_tm', [128, 8, 64], BF16)
    bm_tm = P.sb('bm_tm', [128, 2, 128], BF16)
    dt = P.sb('dt', [128, 8]); dA = P.sb('dA', [128, 8]); acs = P.sb('acs', [128, 8]); dte = P.sb('dte', [128, 8])
    xdt = P.sb('xdt', [128, 8, 64], BF16); xdtd = P.sb('xdtd', [128, 8, 64], BF16)
    Dg = P.sb('Dg', [128, 8, 128]); seg = P.sb('seg', [128, 8, 128]); Lm = P.sb('Lm', [128, 8, 128])
    eacs = P.sb('eacs', [128, 8, 128])
    MT = P.sb('MT', [128, 8, 128], BF16)
    cmhA = P.sb('cmhA', [128, 8, 128], BF16); cmhB = P.sb('cmhB', [128, 8, 128], BF16)
    Em = P.sb('Em', [128, 8, 64]); EAb = P.sb('EAb', [128, 8, 64], BF16); EBb = P.sb('EBb', [128, 8, 64], BF16)
    y2 = P.sb('y2', [128, 8, 64]); sgt = P.sb('sgt', [128, 512]); sq = P.sb('sq', [128, 2, 256]); ssq = P.sb('ssq', [128, 2])
    obf = P.sb('obf', [128, 512], BF16)
    P.memset(raw[:], 0.0); P.memset(cmhA[:], 0.0); P.memset(cmhB[:], 0.0); P.memset(Em[:], 0.0)
    def emit_proj(tq):
        tsq = slice(tq * 128, (tq + 1) * 128)
        if tq > 0:
            P.copy(raw[:, :, 0:3], raw[:, :, 128:131], eng='act')
        for j in range(8):
            for kc in range(8):
                P.mm(BR[:, j, :], w[:, kc, 512 + j * 128:512 + (j + 1) * 128], hT[:, kc, tsq], start=(kc == 0), stop=(kc == 7))
        P.copy(raw[:, 0:4, 3:131], BR[:, 0:4, :], eng='act')
        P.copy(raw[:, 4:8, 3:131], BR[:, 4:8, :], eng='act')

    for tt in range(NT):
        ts_ = slice(tt * 128, (tt + 1) * 128)
        if tt == 0:
            emit_proj(0)
        Bg = By.rearrange("p h d -> p (h d)")
        self.proj_tm(Bg, hT, w, tt, 512, c0=0)
        P.act(sgt[:], Bg, AF.Silu)
        P.tt(acc[:], raw[:, :, 3:131], cw[:, :, 3:4].broadcast_to([128, 8, 128]), ALU.mult)
        for i in range(3):
            P.tt(tmp[:], raw[:, :, i:i + 128], cw[:, :, i:i + 1].broadcast_to([128, 8, 128]), ALU.mult)
            P.tt(acc[:], acc[:], tmp[:], ALU.add)
        P.tt(acc[:], acc[:], cbias[:].unsqueeze(2).broadcast_to([128, 8, 128]), ALU.add)
        P.act(xsb[:], acc[:, 0:4, :], AF.Silu)
        P.act(Bb[:], acc[:, 4:6, :], AF.Silu)
        P.act(Cb[:], acc[:, 6:8, :], AF.Silu)
        if tt + 1 < NT:
            emit_proj(tt + 1)
        for c in range(4):
            P.tr(ptx[:, c, :], xsb[:, c, :], self.identb[:])
        P.copy(xs_tm[:].rearrange("p h d -> p (h d)"), Bt[:, 0:512], eng='act')
        for g in range(2):
            P.tr(ptb_[:, g, :], Bb[:, g, :], self.identb[:])
        P.copy(bm_tm[:], ptb_, eng='act')
        self.proj_tm(pdt, hT, w, tt, 8, c0=1536)
        P.tt(dt[:], pdt, dtb[:], ALU.add)
        P.act(dt[:], dt[:], AF.Exp)
        P.act(dt[:], dt[:], AF.Ln, bias=1.0)
        P.tt(dA[:], dt[:], abc[:], ALU.mult)
        P.mm(pacs, self.blktri[:], dA[:])
        P.mm(ptot, self.blkones[:], dA[:])
        P.copy(acs[:], pacs)
        P.tt(dte[:], ptot, acs[:], ALU.subtract)
        P.act(dte[:], dte[:], AF.Exp)
        P.tt(xdt[:], xs_tm[:], dt[:].unsqueeze(2).broadcast_to([128, 8, 64]), ALU.mult)
        P.tt(xdtd[:], xdt[:], dte[:].unsqueeze(2).broadcast_to([128, 8, 64]), ALU.mult)
        P.tt(Dg[:], self.ident[:].unsqueeze(1).broadcast_to([128, 8, 128]), acs[:].unsqueeze(2).broadcast_to([128, 8, 128]), ALU.mult)
        for hh in range(2):
            hs = slice(hh * 4, (hh + 1) * 4)
            P.mm(Bc, self.ones[:], Dg[:, hs, :])
            P.tt(seg[:, hs, :], Bc, acs[:, hs].unsqueeze(2).broadcast_to([128, 4, 128]), ALU.subtract)
            P.act(eacs[:, hs, :], Bc, AF.Exp)
        P.tt(seg[:], seg[:], negm[:].unsqueeze(1).broadcast_to([128, 8, 128]), ALU.add)
        P.act(Lm[:], seg[:], AF.Exp)
        for g in range(2):
            P.mm(pcb[:, g, :], Bb[:, g, :], Cb[:, g, :])
        P.tt(MT[:].rearrange("p (g e) t -> p g e t", g=2), Lm[:].rearrange("p (g e) t -> p g e t", g=2),
             pcb.unsqueeze(2).broadcast_to([128, 2, 4, 128]), ALU.mult)
        P.tt(cmhA[:, :, 0:64].rearrange("p (g e) t -> p g e t", g=2), eacs[:, :, 0:64].rearrange("p (g e) t -> p g e t", g=2),
             Cb[:, :, 0:64].unsqueeze(2).broadcast_to([128, 2, 4, 64]), ALU.mult)
        P.tt(cmhB[:, :, 64:128].rearrange("p (g e) t -> p g e t", g=2), eacs[:, :, 64:128].rearrange("p (g e) t -> p g e t", g=2),
             Cb[:, :, 64:128].unsqueeze(2).broadcast_to([128, 2, 4, 64]), ALU.mult)
        for g in range(2):
            P.mm(BstA[:, g * 4:(g + 1) * 4, :], bm_tm[0:64, g, :], xdtd[0:64, g * 4:(g + 1) * 4, :])
            P.mm(BstB[:, g * 4:(g + 1) * 4, :], bm_tm[64:128, g, :], xdtd[64:128, g * 4:(g + 1) * 4, :])
        P.copy(EAb[:], Em[:], eng='act')
        P.tt(Em[:], Em[:], eacs[:, :, 63:64].broadcast_to([128, 8, 64]), ALU.mult)
        P.tt(Em[:], Em[:], BstA[:], ALU.add)
        P.copy(EBb[:], Em[:], eng='act')
        for h in range(8):
            P.mm(By[:, h, :], MT[:, h, :], xdt[:, h, :], start=True, stop=False)
            P.mm(By[:, h, :], cmhA[:, h, :], EAb[:, h, :], start=False, stop=False)
            P.mm(By[:, h, :], cmhB[:, h, :], EBb[:, h, :], start=False, stop=True)
        P.tt(Em[:], Em[:], eacs[:, :, 127:128].broadcast_to([128, 8, 64]), ALU.mult)
        P.tt(Em[:], Em[:], BstB[:], ALU.add)
        P.tt(y2[:], xs_tm[:], dsk[:].unsqueeze(2).broadcast_to([128, 8, 64]), ALU.mult)
        P.tt(y2[:], y2[:], By[:], ALU.add)
        y2f = y2[:].rearrange("p h d -> p (h d)")
        P.tt(y2f, y2f, sgt[:], ALU.mult)
        y2g = y2[:].rearrange("p (g e) d -> p g (e d)", g=2)
        P.act(sq[:], y2g, AF.Square)
        P.reduce(ssq[:], sq[:], ALU.add)
        P.act(ssq[:], ssq[:], AF.Sqrt, bias=EPS, scale=1.0 / 256)
        P.recip(ssq[:], ssq[:])
        P.tt(sq[:], y2g, ssq[:].unsqueeze(2).broadcast_to([128, 2, 256]), ALU.mult)
        P.tt(obf[:], sq[:].rearrange("p g t -> p (g t)"), nw[:], ALU.mult)
        self.to_T(obf, oT, tt, ptr, eng='act')
    P.end_phase()


K.mamba = mamba2


def rwkv(self, l, hT, oT, vfirst):
    P = self.P
    I = self.inp
    w_in = I['w_in']
    P.begin_phase()
    w = P.sb('w_rwkv', [128, 8, 1824], BF16)
    self.load_w(w[:], w_in[l, :, 0:1824])
    dup = P.sb('dup', [64, 512], BF16); aup = P.sb('aup', [64, 512], BF16)
    P.dma('pool', dup[:], I['rwkv_decay_up'][l]); P.dma('pool', aup[:], I['rwkv_a_up'][l])
    gu1 = P.sb('gu1', [128, 512], BF16); gu2 = P.sb('gu2', [32, 512], BF16)
    P.dma('pool', gu1[:], I['rwkv_gate_up'][l, 0:128, :]); P.dma('pool', gu2[:], I['rwkv_gate_up'][l, 128:160, :])
    mu = P.sb('mu', [64, 26]); mug = P.sb('mug', [128, 2])
    P.dma('sp', mu[:], I['h_mu64'][l]); P.dma('sp', mug[:], I['h_mug'][l])
    rw = P.sb('rw', [64, 6, 8])
    P.dma('sp', rw[:], I['h_rw'][l])
    w0, a0, kkw, kaw, rkw, v0w = [rw[:, i, :] for i in range(6)]
    lnw = P.sb('lnw', [64, 512]); lnb = P.sb('lnb', [64, 512])
    P.dma('sp', lnw[:], I['rwkv_ln_w'][l].partition_broadcast(64)); P.dma('sp', lnb[:], I['rwkv_ln_b'][l].partition_broadcast(64))
    m1 = P.sb('m1', [64, 128]); SL = P.sb('SL', [64, 64]); smask = P.sb('smask', [64, 1024])
    P.dma('sp', m1[:], I['c_m1']); P.dma('sp', SL[:], I['c_sl']); P.dma('sp', smask[:], I['c_scanmask'])
    if l > 0:
        wdown = P.sb('wdown', [128, 8, 32], BF16); wup = P.sb('wup', [32, 512], BF16)
        self.load_w(wdown[:], I['vres_down'][l - 1]); P.dma('pool', wup[:], I['vres_up'][l - 1])
        tb = P.sb('tb', [32, 128], BF16)
    I64 = self.ident[0:64, 0:64]
    T01 = P.ps('T01', [128, 1024]); T23 = P.ps('T23', [128, 1024])
    B4 = P.ps('B4', [128, 512]); B5 = P.ps('B5', [128, 512]); B6 = P.ps('B6', [128, 512]); B7 = P.ps('B7', [128, 512])
    def v8(t, c0=0, n=128):
        return t[0:64, c0:c0 + 8 * n].rearrange("p (h t) -> p h t", h=8)
    raw = P.sb('raw', [64, 26, 129]); rawg = P.sb('rawg', [128, 2, 129])
    z = P.sb('z', [64, 26, 128]); zg = P.sb('zg', [128, 2, 128])
    txw = P.sb('txw', [64, 128], BF16); xab = P.sb('xab', [64, 128], BF16); sxg = P.sb('sxg', [128, 2, 128], BF16)
    Ta = P.sb('Ta', [64, 8, 128]); Tb = P.sb('Tb', [64, 8, 128]); Tc = P.sb('Tc', [64, 8, 128]); Td = P.sb('Td', [64, 8, 128])
    Te = P.sb('Te', [64, 8, 128]); Tf = P.sb('Tf', [64, 8, 128]); Tg = P.sb('Tg', [64, 8, 128]); Th = P.sb('Th', [64, 8, 128])
    AR = P.sb('AR', [64, 8, 2, 2, 64]); gT = P.sb('gT', [64, 8, 2])
    Vc = P.sb('Vc', [64, 2, 8, 64])
    M1 = P.sb('M1', [64, 8, 128]); M2 = P.sb('M2', [64, 8, 128])
    Q = P.sb('Q', [64, 8, 64]); XT = P.sb('XT', [64, 8, 64]); X = P.sb('X', [64, 8, 64])
    Wsb = P.sb('Wsb', [64, 8, 64]); Usb = P.sb('Usb', [64, 8, 64])
    KT = P.sb('KT', [64, 8, 64]); BT = P.sb('BT', [64, 8, 64])
    S0T = P.sb('S0T', [64, 8, 64])
    Y1 = P.sb('Y1', [64, 8, 64]); Y2 = P.sb('Y2', [64, 8, 64]); obf = P.sb('obf', [64, 512])
    st8 = P.sb('st8', [64, 8]); st8b = P.sb('st8b', [64, 8]); sbon = P.sb('sbon', [64, 8])
    P.memset(raw[:], 0.0); P.memset(rawg[:], 0.0); P.memset(S0T[:], 0.0)
    r_ = z[:, 0:8, :]; k_ = z[:, 8:16, :]; vT = z[:, 16:24, :]
    Kh = z[:, 8:16, :]
    Bh = z[:, 0:8, :]
    b8 = lambda a: a.unsqueeze(2).broadcast_to([64, 8, 128])
    for tt in range(NT):
        ts_ = slice(tt * 128, (tt + 1) * 128)
        if tt > 0:
            P.copy(raw[:, :, 0:1], raw[:, :, 128:129], eng='act')
            P.copy(rawg[:, :, 0:1], rawg[:, :, 128:129], eng='act')
        for grp, (ps_t, c0) in enumerate([(T01, 0), (T23, 512), (T01, 1024)]):
            for h in range(8):
                for kc in range(8):
                    P.mm(ps_t[0:64, h * 128:(h + 1) * 128], w[:, kc, c0 + h * 64:c0 + (h + 1) * 64], hT[:, kc, ts_],
                         start=(kc == 0), stop=(kc == 7))
            P.copy(raw[:, grp * 8:(grp + 1) * 8, 1:129], v8(ps_t), eng='act' if grp != 1 else 'dve')
        for i in range(2):
            for kc in range(8):
                P.mm(B4[0:64, i * 128:(i + 1) * 128], w[:, kc, 1536 + i * 64:1536 + (i + 1) * 64], hT[:, kc, ts_],
                     start=(kc == 0), stop=(kc == 7))
        for kc in range(8):
            P.mm(B4[:, 256:384], w[:, kc, 1664:1792], hT[:, kc, ts_], start=(kc == 0), stop=(kc == 7))
        for kc in range(8):
            P.mm(B5[0:32, 0:128], w[:, kc, 1792:1824], hT[:, kc, ts_], start=(kc == 0), stop=(kc == 7))
        P.copy(raw[:, 24:26, 1:129], B4[0:64, 0:256].rearrange("p (c t) -> p c t", c=2), eng='act')
        P.copy(rawg[:, 0, 1:129], B4[:, 256:384], eng='act')
        P.copy(rawg[0:32, 1, 1:129], B5[0:32, 0:128], eng='act')
        P.tt(z[:], raw[:, :, 0:128], raw[:, :, 1:129], ALU.subtract)
        P.tt(z[:], z[:], mu[:].unsqueeze(2).broadcast_to([64, 26, 128]), ALU.mult)
        P.tt(z[:], z[:], raw[:, :, 1:129], ALU.add)
        P.tt(zg[:], rawg[:, :, 0:128], rawg[:, :, 1:129], ALU.subtract)
        P.tt(zg[:], zg[:], mug[:].unsqueeze(2).broadcast_to([128, 2, 128]), ALU.mult)
        P.tt(zg[:], zg[:], rawg[:, :, 1:129], ALU.add)
        P.act(txw[:], z[:, 24, :], AF.Tanh)
        P.copy(xab[:], z[:, 25, :], eng='act')
        P.act(sxg[:], zg[:], AF.Sigmoid)
        for h in range(8):
            P.mm(T01[0:64, h * 128:(h + 1) * 128], dup[:, h * 64:(h + 1) * 64], txw[:])
            P.mm(T23[0:64, h * 128:(h + 1) * 128], aup[:, h * 64:(h + 1) * 64], xab[:])
        P.tt(Ta[:], v8(T01), b8(w0), ALU.add)
        P.act(Ta[:], Ta[:], AF.Sigmoid)
        P.ts(Ta[:], Ta[:], -0.6065306597126334, ALU.mult)
        P.tt(Tb[:], v8(T23), b8(a0), ALU.add)
        P.act(Tb[:], Tb[:], AF.Sigmoid)
        P.scan(Tc[:].rearrange("p h t -> p (h t)"), smask[:], Ta[:].rearrange("p h t -> p (h t)"), 0.0, ALU.mult, ALU.add)
        P.act(Td[:], Tc[:], AF.Exp)
        P.act(Te[:], Tc[:], AF.Exp, scale=-1.0)
        P.tt(Ta[:], Tc[:], Ta[:], ALU.subtract)
        P.act(Ta[:], Ta[:], AF.Exp)
        P.copy(gT[:], Td[:].rearrange("p h (c t) -> p h c t", c=2)[:, :, :, 63], eng='act')
        P.tt(Tf[:], k_, b8(kkw), ALU.mult)
        P.act(Tg[:], Tf[:], AF.Square)
        P.mm(T01[0:64, 0:512], self.ones[0:64, 0:64], Tg[:, 0:4, :])
        P.mm(T01[0:64, 512:1024], self.ones[0:64, 0:64], Tg[:, 4:8, :])
        P.act(Tg[:], v8(T01), AF.Sqrt)
        P.ts(Tg[:], Tg[:], 1e-12, ALU.max)
        P.recip(Tg[:], Tg[:])
        P.tt(Tf[:], Tf[:], Tg[:], ALU.mult)
        P.stt(Tg[:], Tb[:], -1.0, b8(kaw), ALU.add, ALU.mult)
        P.stt(Tg[:], Tg[:], 1.0, k_, ALU.add, ALU.mult)
        P.tt(Th[:], r_, Tg[:], ALU.mult)
        P.tt(Th[:], Th[:], b8(rkw), ALU.mult)
        r4 = lambda a: a.rearrange("p h (c t) -> p h c t", c=2)
        P.tt(AR[:, :, :, 1, :], r4(r_), r4(Td[:]), ALU.mult)
        P.stt(AR[:, :, :, 0, :], r4(Tf[:]), -1.0, r4(Ta[:]), ALU.mult, ALU.mult)
        P.tt(Kh, Tg[:], Te[:], ALU.mult)
        P.tt(Tf[:], Tf[:], Tb[:], ALU.mult)
        P.tt(Bh, Tf[:], Te[:], ALU.mult)
        if l == 0:
            P.dma('sp', vfirst[tt], vT, track_dram=False)
        else:
            vf = M2
            vm = M1
            P.dma('sp', vf[:], vfirst[tt], track_dram=False)
            for kc in range(8):
                P.mm(B5[0:32, 0:128], wdown[:, kc, :], hT[:, kc, ts_], start=(kc == 0), stop=(kc == 7))
            P.copy(tb[:], B5[0:32, 0:128], eng='act')
            for h in range(8):
                P.mm(T23[0:64, h * 128:(h + 1) * 128], wup[:, h * 64:(h + 1) * 64], tb[:])
            P.tt(vm[:], v8(T23), b8(v0w), ALU.add)
            P.act(vm[:], vm[:], AF.Sigmoid)
            P.tt(vf[:], vf[:], vT, ALU.subtract)
            P.tt(vf[:], vf[:], vm[:], ALU.mult)
            P.tt(vT, vT, vf[:], ALU.add)
        for Xc in range(2):
            for h in range(8):
                P.tr(T23[0:64, (Xc * 8 + h) * 64:(Xc * 8 + h + 1) * 64], vT[:, h, Xc * 64:(Xc + 1) * 64], I64)
        P.copy(Vc[:].rearrange("p c h i -> p (c h i)"), T23[0:64, :], eng='act')
        for Xc in range(2):
            cs = slice(Xc * 64, (Xc + 1) * 64)
            pS1 = v8(T01); pS2 = v8(T23)
            pQ = v8(B4, 0, 64); pP = v8(B5, 0, 64); pX1 = v8(B6, 0, 64); pX2 = v8(B7, 0, 64)
            for h in range(8):
                ar = AR[:, h, Xc, :, :].rearrange("p a t -> p (a t)")
                P.mm(pS1[:, h, :], Bh[:, h, cs], ar)
                P.mm(pS2[:, h, :], Kh[:, h, cs], ar)
                P.mm(pQ[:, h, :], AR[:, h, Xc, 0, :], Bh[:, h, cs])
            P.tt(M1[:], pS1, m1[:].unsqueeze(1).broadcast_to([64, 8, 128]), ALU.mult)
            P.tt(M2[:], pS2, m1[:].unsqueeze(1).broadcast_to([64, 8, 128]), ALU.mult)
            P.tt(Q[:], pQ, SL[:].unsqueeze(1).broadcast_to([64, 8, 64]), ALU.mult)
            Pm = M1[:, :, 0:64]
            P.tt(XT[:], Pm, I64.unsqueeze(1).broadcast_to([64, 8, 64]), ALU.add)
            P.tt(X[:], Q[:], I64.unsqueeze(1).broadcast_to([64, 8, 64]), ALU.add)
            for kq in range(1, 6):
                last = (kq == 5)
                for h in range(8):
                    P.mm(pP[:, h, :], Q[:, h, :], Pm[:, h, :])
                if not last:
                    for h in range(8):
                        P.mm(pQ[:, h, :], Pm[:, h, :], Q[:, h, :])
                P.copy(Pm, pP, eng='act')
                if not last:
                    P.copy(Q[:], pQ, eng='dve')
                for h in range(8):
                    P.mm(pX1[:, h, :], X[:, h, :], Pm[:, h, :])
                if not last:
                    for h in range(8):
                        P.mm(pX2[:, h, :], Pm[:, h, :], X[:, h, :])
                P.tt(XT[:], XT[:], pX1, ALU.add)
                if not last:
                    P.tt(X[:], X[:], pX2, ALU.add)
            pW = v8(T01, 0, 64); pU = v8(T01, 512, 64); pY = v8(T23, 0, 64); pSt = v8(T23, 512, 64)
            pKT = v8(B4, 0, 64); pBT = v8(B6, 0, 64)
            for h in range(8):
                P.mm(pW[:, h, :], M2[:, h, 0:64], Vc[:, Xc, h, :], start=True, stop=False)
                P.mm(pW[:, h, :], AR[:, h, Xc, 0, :], S0T[:, h, :], start=False, stop=True)
            P.copy(Wsb[:], pW, eng='act')
            for h in range(8):
                P.mm(pU[:, h, :], XT[:, h, :], Wsb[:, h, :])
            P.copy(Usb[:], pU, eng='act')
            for h in range(8):
                P.mm(pY[:, h, :], AR[:, h, Xc, 1, :], S0T[:, h, :], start=True, stop=False)
                P.mm(pY[:, h, :], M2[:, h, 64:128], Vc[:, Xc, h, :], start=False, stop=False)
                P.mm(pY[:, h, :], M1[:, h, 64:128], Usb[:, h, :], start=False, stop=True)
            for h in range(8):
                P.tr(pKT[:, h, :], Kh[:, h, cs], I64)
                P.tr(pBT[:, h, :], Bh[:, h, cs], I64)
            P.copy(KT[:], pKT, eng='act')
            P.copy(BT[:], pBT, eng='dve')
            for h in range(8):
                P.mm(pSt[:, h, :], KT[:, h, :], Vc[:, Xc, h, :], start=True, stop=False)
                P.mm(pSt[:, h, :], BT[:, h, :], Usb[:, h, :], start=False, stop=True)
            P.reduce(st8[:], pY, ALU.add)
            P.ts(st8[:], st8[:], -1.0 / 64, ALU.mult)
            P.tt(Y1[:], pY, st8[:].unsqueeze(2).broadcast_to([64, 8, 64]), ALU.add)
            P.act(Y2[:], Y1[:], AF.Square)
            P.reduce(st8b[:], Y2[:], ALU.add)
            P.act(st8b[:], st8b[:], AF.Sqrt, bias=64e-5, scale=1.0 / 64)
            P.recip(st8b[:], st8b[:])
            P.tt(Y1[:], Y1[:], st8b[:].unsqueeze(2).broadcast_to([64, 8, 64]), ALU.mult)
            Y1f = Y1[:].rearrange("p h i -> p (h i)")
            P.tt(Y1f, Y1f, lnw[:], ALU.mult)
            P.tt(Y1f, Y1f, lnb[:], ALU.add)
            P.tt(S0T[:], S0T[:], pSt, ALU.add)
            P.tt(S0T[:], S0T[:], gT[:, :, Xc:Xc + 1].broadcast_to([64, 8, 64]), ALU.mult)
            for h in range(8):
                P.mm(B5[0:64, h:h + 1], Th[:, h, cs], self.ones[0:64, 0:1])
            P.copy(sbon[:], B5[0:64, 0:8], eng='act')
            P.tt(Y2[:], Vc[:, Xc, :, :], sbon[:].unsqueeze(2).broadcast_to([64, 8, 64]), ALU.mult)
            P.tt(Y1[:], Y1[:], Y2[:], ALU.add)
            P.mm(B7[0:64, :], sxg[:, 0, cs], gu1[:], start=True, stop=False)
            P.mm(B7[0:64, :], sxg[0:32, 1, cs], gu2[:], start=False, stop=True)
            P.tt(obf[:], Y1f, B7[0:64, :], ALU.mult)
            for c in range(4):
                P.tr(B5[:, 256 + c * 64:256 + (c + 1) * 64], obf[:, c * 128:(c + 1) * 128], I64)
            st = self.ostage[self.ostage_i % 2]
            P.copy(st[:, :, cs], B5[:, 256:512].rearrange("p (c t) -> p c t", c=4), eng='act')
            if Xc == 1:
                self.ostage_i += 1
                P.dma('sp', oT[tt], st[:], track_dram=False)
    P.end_phase()


K.rwkv = rwkv


def rwkv2(self, l, hT, oT, vfirst):
    P = self.P
    I = self.inp
    w_in = I['w_in']
    P.begin_phase()
    w = P.sb('w_rwkv', [128, 8, 1824], BF16)
    self.load_w(w[:], w_in[l, :, 0:1824])
    dup = P.sb('dup', [64, 512], BF16); aup = P.sb('aup', [64, 512], BF16)
    P.dma('pool', dup[:], I['rwkv_decay_up'][l]); P.dma('pool', aup[:], I['rwkv_a_up'][l])
    gu1 = P.sb('gu1', [128, 512], BF16); gu2 = P.sb('gu2', [32, 512], BF16)
    P.dma('pool', gu1[:], I['rwkv_gate_up'][l, 0:128, :]); P.dma('pool', gu2[:], I['rwkv_gate_up'][l, 128:160, :])
    mu = P.sb('mu', [64, 26]); mug = P.sb('mug', [128, 2])
    P.dma('sp', mu[:], I['h_mu64'][l]); P.dma('sp', mug[:], I['h_mug'][l])
    rw = P.sb('rw', [64, 6, 8])
    P.dma('sp', rw[:], I['h_rw'][l])
    w0, a0, kkw, kaw, rkw, v0w = [rw[:, i, :] for i in range(6)]
    lnw = P.sb('lnw', [64, 512]); lnb = P.sb('lnb', [64, 512])
    P.dma('sp', lnw[:], I['rwkv_ln_w'][l].partition_broadcast(64)); P.dma('sp', lnb[:], I['rwkv_ln_b'][l].partition_broadcast(64))
    m1 = P.sb('m1', [64, 128]); SL = P.sb('SL', [64, 64]); smask = P.sb('smask', [64, 1024])
    P.dma('sp', m1[:], I['c_m1']); P.dma('sp', SL[:], I['c_sl']); P.dma('sp', smask[:], I['c_scanmask'])
    if l > 0:
        wdown = P.sb('wdown', [128, 8, 32], BF16); wup = P.sb('wup', [32, 512], BF16)
        self.load_w(wdown[:], I['vres_down'][l - 1]); P.dma('pool', wup[:], I['vres_up'][l - 1])
        tb = P.sb('tb', [32, 128], BF16)
    I64 = self.ident[0:64, 0:64]
    T01 = P.ps('T01', [128, 1024]); T23 = P.ps('T23', [128, 1024])
    B4 = P.ps('B4', [128, 512]); B5 = P.ps('B5', [128, 512]); B6 = P.ps('B6', [128, 512]); B7 = P.ps('B7', [128, 512])
    def v8(t, c0=0, n=128):
        return t[0:64, c0:c0 + 8 * n].rearrange("p (h t) -> p h t", h=8)
    raw = P.sb('raw', [64, 26, 129]); rawg = P.sb('rawg', [128, 2, 129])
    z = P.sb('z', [64, 26, 128]); zg = P.sb('zg', [128, 2, 128])
    txw = P.sb('txw', [64, 128], BF16); xab = P.sb('xab', [64, 128], BF16); sxg = P.sb('sxg', [128, 2, 128], BF16)
    Ta = P.sb('Ta', [64, 8, 128]); Tb = P.sb('Tb', [64, 8, 128]); Tc = P.sb('Tc', [64, 8, 128]); Td = P.sb('Td', [64, 8, 128])
    Te = P.sb('Te', [64, 8, 128]); Tf = P.sb('Tf', [64, 8, 128]); Tg = P.sb('Tg', [64, 8, 128]); Th = P.sb('Th', [64, 8, 128])
    AR = P.sb('AR', [64, 8, 2, 2, 64], BF16); gT = P.sb('gT', [64, 8, 2])
    Vc = P.sb('Vc', [64, 2, 8, 64]); Vcb = P.sb('Vcb', [64, 2, 8, 64], BF16)
    Khb = P.sb('Khb', [64, 8, 128], BF16); Bhb = P.sb('Bhb', [64, 8, 128], BF16)
    KTb = P.sb('KTb', [64, 2, 8, 64], BF16); BTb = P.sb('BTb', [64, 2, 8, 64], BF16)
    M1s = [P.sb('M1_%d' % i, [64, 8, 128], BF16) for i in range(2)]
    M2s = [P.sb('M2_%d' % i, [64, 8, 128], BF16) for i in range(2)]
    Qs = [P.sb('Q_%d' % i, [64, 8, 64], BF16) for i in range(2)]
    Xbs = [P.sb('Xb_%d' % i, [64, 8, 64], BF16) for i in range(2)]
    XTs = [P.sb('XT_%d' % i, [64, 8, 64]) for i in range(2)]
    XTb = [P.sb('XTb_%d' % i, [64, 8, 64], BF16) for i in range(2)]
    Wsb = P.sb('Wsb', [64, 8, 64], BF16); Usb = P.sb('Usb', [64, 8, 64], BF16)
    S0T = P.sb('S0T', [64, 8, 64]); S0Tb = P.sb('S0Tb', [64, 8, 64], BF16)
    vtmp = [P.sb('vtmp%d' % i, [64, 8, 128]) for i in range(2)]
    Y1 = P.sb('Y1', [64, 8, 64]); Y2 = P.sb('Y2', [64, 8, 64]); obf = P.sb('obf', [64, 512])
    st8 = P.sb('st8', [64, 8]); st8b = P.sb('st8b', [64, 8]); sbon = P.sb('sbon', [64, 8])
    P.memset(raw[:], 0.0); P.memset(rawg[:], 0.0); P.memset(S0T[:], 0.0); P.memset(S0Tb[:], 0.0)
    r_ = z[:, 0:8, :]; k_ = z[:, 8:16, :]; vT = z[:, 16:24, :]
    Kh = z[:, 8:16, :]
    Bh = z[:, 0:8, :]
    b8 = lambda a: a.unsqueeze(2).broadcast_to([64, 8, 128])
    for tt in range(NT):
        ts_ = slice(tt * 128, (tt + 1) * 128)
        if tt > 0:
            P.copy(raw[:, :, 0:1], raw[:, :, 128:129], eng='act')
            P.copy(rawg[:, :, 0:1], rawg[:, :, 128:129], eng='act')
        for grp, (ps_t, c0) in enumerate([(T01, 0), (T23, 512), (T01, 1024)]):
            for h in range(8):
                for kc in range(8):
                    P.mm(ps_t[0:64, h * 128:(h + 1) * 128], w[:, kc, c0 + h * 64:c0 + (h + 1) * 64], hT[:, kc, ts_],
                         start=(kc == 0), stop=(kc == 7))
            P.copy(raw[:, grp * 8:(grp + 1) * 8, 1:129], v8(ps_t), eng='act' if grp != 1 else 'dve')
        for i in range(2):
            for kc in range(8):
                P.mm(B4[0:64, i * 128:(i + 1) * 128], w[:, kc, 1536 + i * 64:1536 + (i + 1) * 64], hT[:, kc, ts_],
                     start=(kc == 0), stop=(kc == 7))
        for kc in range(8):
            P.mm(B4[:, 256:384], w[:, kc, 1664:1792], hT[:, kc, ts_], start=(kc == 0), stop=(kc == 7))
        for kc in range(8):
            P.mm(B5[0:32, 0:128], w[:, kc, 1792:1824], hT[:, kc, ts_], start=(kc == 0), stop=(kc == 7))
        P.copy(raw[:, 24:26, 1:129], B4[0:64, 0:256].rearrange("p (c t) -> p c t", c=2), eng='act')
        P.copy(rawg[:, 0, 1:129], B4[:, 256:384], eng='act')
        P.copy(rawg[0:32, 1, 1:129], B5[0:32, 0:128], eng='act')
        P.tt(z[:], raw[:, :, 0:128], raw[:, :, 1:129], ALU.subtract)
        P.tt(z[:], z[:], mu[:].unsqueeze(2).broadcast_to([64, 26, 128]), ALU.mult)
        P.tt(z[:], z[:], raw[:, :, 1:129], ALU.add)
        P.tt(zg[:], rawg[:, :, 0:128], rawg[:, :, 1:129], ALU.subtract)
        P.tt(zg[:], zg[:], mug[:].unsqueeze(2).broadcast_to([128, 2, 128]), ALU.mult)
        P.tt(zg[:], zg[:], rawg[:, :, 1:129], ALU.add)
        P.act(txw[:], z[:, 24, :], AF.Tanh)
        P.copy(xab[:], z[:, 25, :], eng='act')
        P.act(sxg[:], zg[:], AF.Sigmoid)
        for h in range(8):
            P.mm(T01[0:64, h * 128:(h + 1) * 128], dup[:, h * 64:(h + 1) * 64], txw[:])
            P.mm(T23[0:64, h * 128:(h + 1) * 128], aup[:, h * 64:(h + 1) * 64], xab[:])
        P.tt(Ta[:], v8(T01), b8(w0), ALU.add)
        P.act(Ta[:], Ta[:], AF.Sigmoid)
        P.ts(Ta[:], Ta[:], -0.6065306597126334, ALU.mult)
        P.tt(Tb[:], v8(T23), b8(a0), ALU.add)
        P.act(Tb[:], Tb[:], AF.Sigmoid)
        P.scan(Tc[:].rearrange("p h t -> p (h t)"), smask[:], Ta[:].rearrange("p h t -> p (h t)"), 0.0, ALU.mult, ALU.add)
        P.act(Td[:], Tc[:], AF.Exp)
        P.act(Te[:], Tc[:], AF.Exp, scale=-1.0)
        P.tt(Ta[:], Tc[:], Ta[:], ALU.subtract)
        P.act(Ta[:], Ta[:], AF.Exp)
        P.copy(gT[:], Td[:].rearrange("p h (c t) -> p h c t", c=2)[:, :, :, 63], eng='act')
        P.tt(Tf[:], k_, b8(kkw), ALU.mult)
        P.act(Tg[:], Tf[:], AF.Square)
        P.mm(T01[0:64, 0:512], self.ones[0:64, 0:64], Tg[:, 0:4, :])
        P.mm(T01[0:64, 512:1024], self.ones[0:64, 0:64], Tg[:, 4:8, :])
        P.act(Tg[:], v8(T01), AF.Sqrt)
        P.ts(Tg[:], Tg[:], 1e-12, ALU.max)
        P.recip(Tg[:], Tg[:])
        P.tt(Tf[:], Tf[:], Tg[:], ALU.mult)
        P.stt(Tg[:], Tb[:], -1.0, b8(kaw), ALU.add, ALU.mult)
        P.stt(Tg[:], Tg[:], 1.0, k_, ALU.add, ALU.mult)
        P.tt(Th[:], r_, Tg[:], ALU.mult)
        P.tt(Th[:], Th[:], b8(rkw), ALU.mult)
        r4 = lambda a: a.rearrange("p h (c t) -> p h c t", c=2)
        P.tt(AR[:, :, :, 1, :], r4(r_), r4(Td[:]), ALU.mult)
        P.stt(AR[:, :, :, 0, :], r4(Tf[:]), -1.0, r4(Ta[:]), ALU.mult, ALU.mult)
        P.tt(Kh, Tg[:], Te[:], ALU.mult)
        P.tt(Tf[:], Tf[:], Tb[:], ALU.mult)
        P.tt(Bh, Tf[:], Te[:], ALU.mult)
        if l == 0:
            P.dma('sp', vfirst[tt], vT, track_dram=False)
        else:
            vf = vtmp[0]
            vm = vtmp[1]
            P.dma('sp', vf[:], vfirst[tt], track_dram=False)
            for kc in range(8):
                P.mm(B5[0:32, 0:128], wdown[:, kc, :], hT[:, kc, ts_], start=(kc == 0), stop=(kc == 7))
            P.copy(tb[:], B5[0:32, 0:128], eng='act')
            for h in range(8):
                P.mm(T23[0:64, h * 128:(h + 1) * 128], wup[:, h * 64:(h + 1) * 64], tb[:])
            P.tt(vm[:], v8(T23), b8(v0w), ALU.add)
            P.act(vm[:], vm[:], AF.Sigmoid)
            P.tt(vf[:], vf[:], vT, ALU.subtract)
            P.tt(vf[:], vf[:], vm[:], ALU.mult)
            P.tt(vT, vT, vf[:], ALU.add)
        for Xc in range(2):
            for h in range(8):
                P.tr(T23[0:64, (Xc * 8 + h) * 64:(Xc * 8 + h + 1) * 64], vT[:, h, Xc * 64:(Xc + 1) * 64], I64)
        P.copy(Vc[:].rearrange("p c h i -> p (c h i)"), T23[0:64, :], eng='act')
        P.copy(Vcb[:].rearrange("p c h i -> p (c h i)"), T23[0:64, :], eng='dve')
        P.copy(Khb[:], Kh, eng='pool')
        P.copy(Bhb[:], Bh, eng='pool')
        for Xc in range(2):
            for h in range(8):
                P.tr(T01[0:64, (Xc * 8 + h) * 64:(Xc * 8 + h + 1) * 64], Kh[:, h, Xc * 64:(Xc + 1) * 64], I64)
        P.copy(KTb[:].rearrange("p c h i -> p (c h i)"), T01[0:64, :], eng='act')
        for Xc in range(2):
            for h in range(8):
                P.tr(T23[0:64, (Xc * 8 + h) * 64:(Xc * 8 + h + 1) * 64], Bh[:, h, Xc * 64:(Xc + 1) * 64], I64)
        P.copy(BTb[:].rearrange("p c h i -> p (c h i)"), T23[0:64, :], eng='dve')
        I8 = I64.unsqueeze(1).broadcast_to([64, 8, 64])
        for Xc in range(2):
            cs = slice(Xc * 64, (Xc + 1) * 64)
            pS1 = v8(T01); pS2 = v8(T23); pQ0 = v8(B4, 0, 64)
            for h in range(8):
                ar = AR[:, h, Xc, :, :].rearrange("p a t -> p (a t)")
                P.mm(pS1[:, h, :], Bhb[:, h, cs], ar)
                P.mm(pS2[:, h, :], Khb[:, h, cs], ar)
                P.mm(pQ0[:, h, :], AR[:, h, Xc, 0, :], Bhb[:, h, cs])
            P.tt(M1s[Xc][:], pS1, m1[:].unsqueeze(1).broadcast_to([64, 8, 128]), ALU.mult)
            P.tt(M2s[Xc][:], pS2, m1[:].unsqueeze(1).broadcast_to([64, 8, 128]), ALU.mult)
            P.tt(Qs[Xc][:], pQ0, SL[:].unsqueeze(1).broadcast_to([64, 8, 64]), ALU.mult)
            P.tt(XTs[Xc][:], M1s[Xc][:, :, 0:64], I8, ALU.add)
            P.tt(Xbs[Xc][:], Qs[Xc][:], I8, ALU.add)
        bankP = [v8(B4, 0, 64), v8(T01, 0, 64)]
        bankQ = [v8(B5, 0, 64), v8(T01, 512, 64)]
        bankX1 = [v8(B6, 0, 64), v8(T23, 0, 64)]
        bankX2 = [v8(B7, 0, 64), v8(T23, 512, 64)]
        for kq in range(1, 6):
            last = (kq == 5)
            for Xc in range(2):
                Pm = M1s[Xc][:, :, 0:64]
                for h in range(8):
                    P.mm(bankP[Xc][:, h, :], Qs[Xc][:, h, :], Pm[:, h, :])
                if not last:
                    for h in range(8):
                        P.mm(bankQ[Xc][:, h, :], Pm[:, h, :], Qs[Xc][:, h, :])
            for Xc in range(2):
                Pm = M1s[Xc][:, :, 0:64]
                P.copy(Pm, bankP[Xc], eng='act')
                if not last:
                    P.copy(Qs[Xc][:], bankQ[Xc], eng='dve')
            for Xc in range(2):
                Pm = M1s[Xc][:, :, 0:64]
                for h in range(8):
                    P.mm(bankX1[Xc][:, h, :], Xbs[Xc][:, h, :], Pm[:, h, :])
                if not last:
                    for h in range(8):
                        P.mm(bankX2[Xc][:, h, :], Pm[:, h, :], Xbs[Xc][:, h, :])
            for Xc in range(2):
                P.tt(XTs[Xc][:], XTs[Xc][:], bankX1[Xc], ALU.add)
                if not last:
                    P.tt(Xbs[Xc][:], Xbs[Xc][:], bankX2[Xc], ALU.add)
        for Xc in range(2):
            P.copy(XTb[Xc][:], XTs[Xc][:], eng='act')
        for Xc in range(2):
            cs = slice(Xc * 64, (Xc + 1) * 64)
            M1 = M1s[Xc]; M2 = M2s[Xc]
            if Xc == 0:
                pW = v8(B4, 0, 64); pU = v8(B5, 0, 64); pY = v8(B6, 0, 64); pSt = v8(B7, 0, 64)
                gbank = B4; obank = B5
            else:
                pW = v8(T01, 0, 64); pU = v8(T01, 512, 64); pY = v8(T23, 0, 64); pSt = v8(T23, 512, 64)
                gbank = T01[:, 0:512]; obank = T01[:, 512:1024]
            for h in range(8):
                P.mm(pW[:, h, :], M2[:, h, 0:64], Vcb[:, Xc, h, :], start=True, stop=False)
                P.mm(pW[:, h, :], AR[:, h, Xc, 0, :], S0Tb[:, h, :], start=False, stop=True)
            P.copy(Wsb[:], pW, eng='act')
            for h in range(8):
                P.mm(pU[:, h, :], XTb[Xc][:, h, :], Wsb[:, h, :])
            P.copy(Usb[:], pU, eng='act')
            for h in range(8):
                P.mm(pY[:, h, :], AR[:, h, Xc, 1, :], S0Tb[:, h, :], start=True, stop=False)
                P.mm(pY[:, h, :], M2[:, h, 64:128], Vcb[:, Xc, h, :], start=False, stop=False)
                P.mm(pY[:, h, :], M1[:, h, 64:128], Usb[:, h, :], start=False, stop=True)
            for h in range(8):
                P.mm(pSt[:, h, :], KTb[:, Xc, h, :], Vcb[:, Xc, h, :], start=True, stop=False)
                P.mm(pSt[:, h, :], BTb[:, Xc, h, :], Usb[:, h, :], start=False, stop=True)
            P.reduce(st8[:], pY, ALU.add)
            P.ts(st8[:], st8[:], -1.0 / 64, ALU.mult)
            P.tt(Y1[:], pY, st8[:].unsqueeze(2).broadcast_to([64, 8, 64]), ALU.add)
            P.act(Y2[:], Y1[:], AF.Square)
            P.reduce(st8b[:], Y2[:], ALU.add)
            P.act(st8b[:], st8b[:], AF.Sqrt, bias=64e-5, scale=1.0 / 64)
            P.recip(st8b[:], st8b[:])
            P.tt(Y1[:], Y1[:], st8b[:].unsqueeze(2).broadcast_to([64, 8, 64]), ALU.mult)
            Y1f = Y1[:].rearrange("p h i -> p (h i)")
            P.tt(Y1f, Y1f, lnw[:], ALU.mult)
            P.tt(Y1f, Y1f, lnb[:], ALU.add)
            P.tt(S0T[:], S0T[:], pSt, ALU.add)
            P.tt(S0T[:], S0T[:], gT[:, :, Xc:Xc + 1].broadcast_to([64, 8, 64]), ALU.mult)
            P.copy(S0Tb[:], S0T[:], eng='act')
            for h in range(8):
                P.mm(obank[0:64, h:h + 1], Th[:, h, cs], self.ones[0:64, 0:1])
            P.copy(sbon[:], obank[0:64, 0:8], eng='act')
            P.tt(Y2[:], Vc[:, Xc, :, :], sbon[:].unsqueeze(2).broadcast_to([64, 8, 64]), ALU.mult)
            P.tt(Y1[:], Y1[:], Y2[:], ALU.add)
            P.mm(gbank[0:64, :], sxg[:, 0, cs], gu1[:], start=True, stop=False)
            P.mm(gbank[0:64, :], sxg[0:32, 1, cs], gu2[:], start=False, stop=True)
            P.tt(obf[:], Y1f, gbank[0:64, :], ALU.mult)
            for c in range(4):
                P.tr(obank[:, 256 + c * 64:256 + (c + 1) * 64], obf[:, c * 128:(c + 1) * 128], I64)
            st = self.ostage[self.ostage_i % 2]
            P.copy(st[:, :, cs], obank[:, 256:512].rearrange("p (c t) -> p c t", c=4), eng='act')
            if Xc == 1:
                self.ostage_i += 1
                P.dma('sp', oT[tt], st[:], track_dram=False)
    P.end_phase()


K.rwkv = rwkv2


def rwkv3(self, l, hT, oT, vfirst):
    P = self.P
    I = self.inp
    w_in = I['w_in']
    P.begin_phase()
    wa = P.sb('wa_rwkv', [128, 8, 1824], BF16)
    wb = P.sb('wb_rwkv', [128, 8, 1824], BF16)
    P.begin_phase()
    w = P.sb('w_rwkv', [128, 8, 1824], BF16)
    self.load_w(w[:], w_in[l, :, 0:1824])
    mub = P.sb('mub', [128, 1824]); omub = P.sb('omub', [128, 1824])
    P.dma('sp', mub[:], I['rwkv_mu'][l].partition_broadcast(128))
    P.ts(omub[:], mub[:], -1.0, ALU.mult, 1.0, ALU.add)
    for kc in range(8):
        P.tt(wa[:, kc, :], w[:, kc, :], omub[:], ALU.mult, eng='dve')
        P.tt(wb[:, kc, :], w[:, kc, :], mub[:], ALU.mult, eng='dve')
    P.end_phase()
    dup = P.sb('dup', [64, 512], BF16); aup = P.sb('aup', [64, 512], BF16)
    P.dma('pool', dup[:], I['rwkv_decay_up'][l]); P.dma('pool', aup[:], I['rwkv_a_up'][l])
    gu1 = P.sb('gu1', [128, 512], BF16); gu2 = P.sb('gu2', [32, 512], BF16)
    P.dma('pool', gu1[:], I['rwkv_gate_up'][l, 0:128, :]); P.dma('pool', gu2[:], I['rwkv_gate_up'][l, 128:160, :])
    rw = P.sb('rw', [64, 6, 8])
    P.dma('sp', rw[:], I['h_rw'][l])
    w0, a0, kkw, kaw, rkw, v0w = [rw[:, i, :] for i in range(6)]
    lnw = P.sb('lnw', [64, 512]); lnb = P.sb('lnb', [64, 512])
    P.dma('sp', lnw[:], I['rwkv_ln_w'][l].partition_broadcast(64)); P.dma('sp', lnb[:], I['rwkv_ln_b'][l].partition_broadcast(64))
    m1 = P.sb('m1', [64, 128]); SL = P.sb('SL', [64, 64]); smask = P.sb('smask', [64, 1024])
    P.dma('sp', m1[:], I['c_m1']); P.dma('sp', SL[:], I['c_sl']); P.dma('sp', smask[:], I['c_scanmask'])
    if l > 0:
        wdown = P.sb('wdown', [128, 8, 32], BF16); wup = P.sb('wup', [32, 512], BF16)
        self.load_w(wdown[:], I['vres_down'][l - 1]); P.dma('pool', wup[:], I['vres_up'][l - 1])
        tb = P.sb('tb', [32, 128], BF16)
    I64 = self.ident[0:64, 0:64]
    T01 = P.ps('T01', [128, 1024]); T23 = P.ps('T23', [128, 1024])
    B4 = P.ps('B4', [128, 512]); B5 = P.ps('B5', [128, 512]); B6 = P.ps('B6', [128, 512]); B7 = P.ps('B7', [128, 512])
    def v8(t, c0=0, n=128):
        return t[0:64, c0:c0 + 8 * n].rearrange("p (h t) -> p h t", h=8)
    z = P.sb('z', [64, 26, 128]); zg = P.sb('zg', [128, 2, 128])
    txw = P.sb('txw', [64, 128], BF16); xab = P.sb('xab', [64, 128], BF16); sxg = P.sb('sxg', [128, 2, 128], BF16)
    Ta = P.sb('Ta', [64, 8, 128]); Tb = P.sb('Tb', [64, 8, 128]); Tc = P.sb('Tc', [64, 8, 128]); Td = P.sb('Td', [64, 8, 128])
    Te = P.sb('Te', [64, 8, 128]); Tf = P.sb('Tf', [64, 8, 128]); Tg = P.sb('Tg', [64, 8, 128]); Th = P.sb('Th', [64, 8, 128])
    AR = P.sb('AR', [64, 8, 2, 2, 64], BF16); gT = P.sb('gT', [64, 8, 2])
    Vc = P.sb('Vc', [64, 2, 8, 64]); Vcb = P.sb('Vcb', [64, 2, 8, 64], BF16)
    Khb = P.sb('Khb', [64, 8, 128], BF16); Bhb = P.sb('Bhb', [64, 8, 128], BF16)
    KTb = P.sb('KTb', [64, 2, 8, 64], BF16); BTb = P.sb('BTb', [64, 2, 8, 64], BF16)
    M1s = [P.sb('M1_%d' % i, [64, 8, 128], BF16) for i in range(2)]
    M2s = [P.sb('M2_%d' % i, [64, 8, 128], BF16) for i in range(2)]
    Qs = [P.sb('Q_%d' % i, [64, 8, 64], BF16) for i in range(2)]
    Xbs = [P.sb('Xb_%d' % i, [64, 8, 64], BF16) for i in range(2)]
    XTs = [P.sb('XT_%d' % i, [64, 8, 64]) for i in range(2)]
    XTb = [P.sb('XTb_%d' % i, [64, 8, 64], BF16) for i in range(2)]
    Wsb = P.sb('Wsb', [64, 8, 64], BF16); Usb = P.sb('Usb', [64, 8, 64], BF16)
    S0T = P.sb('S0T', [64, 8, 64]); S0Tb = P.sb('S0Tb', [64, 8, 64], BF16)
    Y1 = P.sb('Y1', [64, 8, 64]); Y2 = P.sb('Y2', [64, 8, 64]); obf = P.sb('obf', [64, 512])
    st8 = P.sb('st8', [64, 8]); st8b = P.sb('st8b', [64, 8]); sbon = P.sb('sbon', [64, 8])
    P.memset(zg[:], 0.0); P.memset(S0T[:], 0.0); P.memset(S0Tb[:], 0.0)
    r_ = z[:, 0:8, :]; k_ = z[:, 8:16, :]; vT = z[:, 16:24, :]
    Kh = z[:, 8:16, :]
    Bh = z[:, 0:8, :]
    b8 = lambda a: a.unsqueeze(2).broadcast_to([64, 8, 128])
    for tt in range(NT):
        ts_ = slice(tt * 128, (tt + 1) * 128)
        def proj(out, c0, m):
            for kc in range(8):
                P.mm(out, wa[:, kc, c0:c0 + m], hT[:, kc, ts_], start=(kc == 0), stop=False)
            if tt == 0:
                for kc in range(8):
                    P.mm(out[:, 1:128], wb[:, kc, c0:c0 + m], hT[:, kc, 0:127], start=False, stop=(kc == 7))
            else:
                for kc in range(8):
                    P.mm(out, wb[:, kc, c0:c0 + m], hT[:, kc, tt * 128 - 1:(tt + 1) * 128 - 1], start=False, stop=(kc == 7))
        for grp, (ps_t, c0) in enumerate([(T01, 0), (T23, 512), (T01, 1024)]):
            for h in range(8):
                proj(ps_t[0:64, h * 128:(h + 1) * 128], c0 + h * 64, 64)
            P.copy(z[:, grp * 8:(grp + 1) * 8, :], v8(ps_t), eng='act' if grp != 1 else 'dve')
        for i in range(2):
            proj(B4[0:64, i * 128:(i + 1) * 128], 1536 + i * 64, 64)
        proj(B4[:, 256:384], 1664, 128)
        proj(B5[0:32, 0:128], 1792, 32)
        P.copy(z[:, 24:26, :], B4[0:64, 0:256].rearrange("p (c t) -> p c t", c=2), eng='act')
        P.copy(zg[:, 0, :], B4[:, 256:384], eng='act')
        P.copy(zg[0:32, 1, :], B5[0:32, 0:128], eng='act')
        P.act(txw[:], z[:, 24, :], AF.Tanh)
        P.copy(xab[:], z[:, 25, :], eng='act')
        P.act(sxg[:], zg[:], AF.Sigmoid)
        for h in range(8):
            P.mm(T01[0:64, h * 128:(h + 1) * 128], dup[:, h * 64:(h + 1) * 64], txw[:])
            P.mm(T23[0:64, h * 128:(h + 1) * 128], aup[:, h * 64:(h + 1) * 64], xab[:])
        P.tt(Ta[:], v8(T01), b8(w0), ALU.add)
        P.act(Ta[:], Ta[:], AF.Sigmoid)
        P.tt(Tb[:], v8(T23), b8(a0), ALU.add)
        P.act(Tb[:], Tb[:], AF.Sigmoid)
        P.scan(Tc[:].rearrange("p h t -> p (h t)"), smask[:], Ta[:].rearrange("p h t -> p (h t)"), 0.0, ALU.mult, ALU.add)
        P.act(Td[:], Tc[:], AF.Exp, scale=-0.6065306597126334)
        P.act(Te[:], Tc[:], AF.Exp, scale=0.6065306597126334)
        P.tt(Ta[:], Tc[:], Ta[:], ALU.subtract)
        P.act(Ta[:], Ta[:], AF.Exp, scale=-0.6065306597126334)
        P.copy(gT[:], Td[:].rearrange("p h (c t) -> p h c t", c=2)[:, :, :, 63], eng='act')
        P.tt(Tf[:], k_, b8(kkw), ALU.mult)
        P.act(Tg[:], Tf[:], AF.Square)
        P.mm(T01[0:64, 0:512], self.ones[0:64, 0:64], Tg[:, 0:4, :])
        P.mm(T01[0:64, 512:1024], self.ones[0:64, 0:64], Tg[:, 4:8, :])
        P.act(Tg[:], v8(T01), AF.Sqrt)
        P.ts(Tg[:], Tg[:], 1e-12, ALU.max)
        P.recip(Tg[:], Tg[:])
        P.tt(Tf[:], Tf[:], Tg[:], ALU.mult)
        P.stt(Tg[:], Tb[:], -1.0, b8(kaw), ALU.add, ALU.mult)
        P.stt(Tg[:], Tg[:], 1.0, k_, ALU.add, ALU.mult)
        P.tt(Th[:], r_, Tg[:], ALU.mult)
        P.tt(Th[:], Th[:], b8(rkw), ALU.mult)
        r4 = lambda a: a.rearrange("p h (c t) -> p h c t", c=2)
        P.tt(AR[:, :, :, 1, :], r4(r_), r4(Td[:]), ALU.mult)
        P.stt(AR[:, :, :, 0, :], r4(Tf[:]), -1.0, r4(Ta[:]), ALU.mult, ALU.mult)
        P.tt(Kh, Tg[:], Te[:], ALU.mult)
        P.tt(Tf[:], Tf[:], Tb[:], ALU.mult)
        P.tt(Bh, Tf[:], Te[:], ALU.mult)
        if l == 0:
            P.dma('sp', vfirst[tt], vT, track_dram=False)
        else:
            vf = Ta
            vm = Tb
            P.dma('sp', vf[:], vfirst[tt], track_dram=False)
            for kc in range(8):
                P.mm(B5[0:32, 0:128], wdown[:, kc, :], hT[:, kc, ts_], start=(kc == 0), stop=(kc == 7))
            P.copy(tb[:], B5[0:32, 0:128], eng='act')
            for h in range(8):
                P.mm(T23[0:64, h * 128:(h + 1) * 128], wup[:, h * 64:(h + 1) * 64], tb[:])
            P.tt(vm[:], v8(T23), b8(v0w), ALU.add)
            P.act(vm[:], vm[:], AF.Sigmoid)
            P.tt(vf[:], vf[:], vT, ALU.subtract)
            P.tt(vf[:], vf[:], vm[:], ALU.mult)
            P.tt(vT, vT, vf[:], ALU.add)
        for Xc in range(2):
            for h in range(8):
                P.tr(T23[0:64, (Xc * 8 + h) * 64:(Xc * 8 + h + 1) * 64], vT[:, h, Xc * 64:(Xc + 1) * 64], I64)
        P.copy(Vc[:].rearrange("p c h i -> p (c h i)"), T23[0:64, :], eng='act')
        P.copy(Vcb[:].rearrange("p c h i -> p (c h i)"), T23[0:64, :], eng='dve')
        P.copy(Khb[:], Kh, eng='act')
        P.copy(Bhb[:], Bh, eng='act')
        for Xc in range(2):
            for h in range(8):
                P.tr(T01[0:64, (Xc * 8 + h) * 64:(Xc * 8 + h + 1) * 64], Kh[:, h, Xc * 64:(Xc + 1) * 64], I64)
        P.copy(KTb[:].rearrange("p c h i -> p (c h i)"), T01[0:64, :], eng='act')
        for Xc in range(2):
            for h in range(8):
                P.tr(T23[0:64, (Xc * 8 + h) * 64:(Xc * 8 + h + 1) * 64], Bh[:, h, Xc * 64:(Xc + 1) * 64], I64)
        P.copy(BTb[:].rearrange("p c h i -> p (c h i)"), T23[0:64, :], eng='dve')
        I8 = I64.unsqueeze(1).broadcast_to([64, 8, 64])
        for Xc in range(2):
            cs = slice(Xc * 64, (Xc + 1) * 64)
            pS1 = v8(T01); pS2 = v8(T23); pQ0 = v8(B4, 0, 64)
            for h in range(8):
                ar = AR[:, h, Xc, :, :].rearrange("p a t -> p (a t)")
                P.mm(pS1[:, h, :], Bhb[:, h, cs], ar)
                P.mm(pS2[:, h, :], Khb[:, h, cs], ar)
                P.mm(pQ0[:, h, :], AR[:, h, Xc, 0, :], Bhb[:, h, cs])
            P.tt(M1s[Xc][:], pS1, m1[:].unsqueeze(1).broadcast_to([64, 8, 128]), ALU.mult)
            P.tt(M2s[Xc][:], pS2, m1[:].unsqueeze(1).broadcast_to([64, 8, 128]), ALU.mult)
            P.tt(Qs[Xc][:], pQ0, SL[:].unsqueeze(1).broadcast_to([64, 8, 64]), ALU.mult)
            P.tt(XTs[Xc][:], M1s[Xc][:, :, 0:64], I8, ALU.add)
            P.tt(Xbs[Xc][:], Qs[Xc][:], I8, ALU.add)
        bankP = [v8(B4, 0, 64), v8(T01, 0, 64)]
        bankQ = [v8(B5, 0, 64), v8(T01, 512, 64)]
        bankX1 = [v8(B6, 0, 64), v8(T23, 0, 64)]
        bankX2 = [v8(B7, 0, 64), v8(T23, 512, 64)]
        for kq in range(1, 6):
            last = (kq == 5)
            for Xc in range(2):
                Pm = M1s[Xc][:, :, 0:64]
                for h in range(8):
                    P.mm(bankP[Xc][:, h, :], Qs[Xc][:, h, :], Pm[:, h, :])
                if not last:
                    for h in range(8):
                        P.mm(bankQ[Xc][:, h, :], Pm[:, h, :], Qs[Xc][:, h, :])
            for Xc in range(2):
                Pm = M1s[Xc][:, :, 0:64]
                P.copy(Pm, bankP[Xc], eng='act')
                if not last:
                    P.copy(Qs[Xc][:], bankQ[Xc], eng='act')
            for Xc in range(2):
                Pm = M1s[Xc][:, :, 0:64]
                for h in range(8):
                    P.mm(bankX1[Xc][:, h, :], Xbs[Xc][:, h, :], Pm[:, h, :])
                if not last:
                    for h in range(8):
                        P.mm(bankX2[Xc][:, h, :], Pm[:, h, :], Xbs[Xc][:, h, :])
            for Xc in range(2):
                P.tt(XTs[Xc][:], XTs[Xc][:], bankX1[Xc], ALU.add)
                if not last:
                    P.tt(Xbs[Xc][:], Xbs[Xc][:], bankX2[Xc], ALU.add)
        for Xc in range(2):
            P.copy(XTb[Xc][:], XTs[Xc][:], eng='act')
        for Xc in range(2):
            cs = slice(Xc * 64, (Xc + 1) * 64)
            M1 = M1s[Xc]; M2 = M2s[Xc]
            if Xc == 0:
                pW = v8(B4, 0, 64); pU = v8(B5, 0, 64); pY = v8(B6, 0, 64); pSt = v8(B7, 0, 64)
                gbank = B4; obank = B5
            else:
                pW = v8(T01, 0, 64); pU = v8(T01, 512, 64); pY = v8(T23, 0, 64); pSt = v8(T23, 512, 64)
                gbank = T01[:, 0:512]; obank = T01[:, 512:1024]
            for h in range(8):
                P.mm(pW[:, h, :], M2[:, h, 0:64], Vcb[:, Xc, h, :], start=True, stop=False)
                P.mm(pW[:, h, :], AR[:, h, Xc, 0, :], S0Tb[:, h, :], start=False, stop=True)
            P.copy(Wsb[:], pW, eng='act')
            for h in range(8):
                P.mm(pU[:, h, :], XTb[Xc][:, h, :], Wsb[:, h, :])
            P.copy(Usb[:], pU, eng='act')
            for h in range(8):
                P.mm(pY[:, h, :], AR[:, h, Xc, 1, :], S0Tb[:, h, :], start=True, stop=False)
                P.mm(pY[:, h, :], M2[:, h, 64:128], Vcb[:, Xc, h, :], start=False, stop=False)
                P.mm(pY[:, h, :], M1[:, h, 64:128], Usb[:, h, :], start=False, stop=True)
            for h in range(8):
                P.mm(pSt[:, h, :], KTb[:, Xc, h, :], Vcb[:, Xc, h, :], start=True, stop=False)
                P.mm(pSt[:, h, :], BTb[:, Xc, h, :], Usb[:, h, :], start=False, stop=True)
            P.reduce(st8[:], pY, ALU.add)
            P.ts(st8[:], st8[:], -1.0 / 64, ALU.mult)
            P.tt(Y1[:], pY, st8[:].unsqueeze(2).broadcast_to([64, 8, 64]), ALU.add)
            P.act(Y2[:], Y1[:], AF.Square)
            P.reduce(st8b[:], Y2[:], ALU.add)
            P.act(st8b[:], st8b[:], AF.Sqrt, bias=64e-5, scale=1.0 / 64)
            P.recip(st8b[:], st8b[:])
            P.tt(Y1[:], Y1[:], st8b[:].unsqueeze(2).broadcast_to([64, 8, 64]), ALU.mult)
            Y1f = Y1[:].rearrange("p h i -> p (h i)")
            P.tt(Y1f, Y1f, lnw[:], ALU.mult)
            P.tt(Y1f, Y1f, lnb[:], ALU.add)
            P.tt(S0T[:], S0T[:], pSt, ALU.add)
            P.tt(S0T[:], S0T[:], gT[:, :, Xc:Xc + 1].broadcast_to([64, 8, 64]), ALU.mult)
            P.copy(S0Tb[:], S0T[:], eng='act')
            for h in range(8):
                P.mm(obank[0:64, h:h + 1], Th[:, h, cs], self.ones[0:64, 0:1])
            P.copy(sbon[:], obank[0:64, 0:8], eng='act')
            P.tt(Y2[:], Vc[:, Xc, :, :], sbon[:].unsqueeze(2).broadcast_to([64, 8, 64]), ALU.mult)
            P.tt(Y1[:], Y1[:], Y2[:], ALU.add)
            P.mm(gbank[0:64, :], sxg[:, 0, cs], gu1[:], start=True, stop=False)
            P.mm(gbank[0:64, :], sxg[0:32, 1, cs], gu2[:], start=False, stop=True)
            P.tt(obf[:], Y1f, gbank[0:64, :], ALU.mult)
            for c in range(4):
                P.tr(obank[:, 256 + c * 64:256 + (c + 1) * 64], obf[:, c * 128:(c + 1) * 128], I64)
            st = self.ostage[self.ostage_i % 2]
            P.copy(st[:, :, cs], obank[:, 256:512].rearrange("p (c t) -> p c t", c=4), eng='act')
            if Xc == 1:
                self.ostage_i += 1
                P.dma('sp', oT[tt], st[:], track_dram=False)
    P.end_phase()


K.rwkv = rwkv3


def rwkv4(self, l, hT, oT, vfirst):
    P = self.P
    I = self.inp
    w_in = I['w_in']
    P.begin_phase()
    wa = P.sb('wa_rwkv', [128, 8, 1824], BF16)
    wb = P.sb('wb_rwkv', [128, 8, 1824], BF16)
    P.begin_phase()
    w = P.sb('w_rwkv', [128, 8, 1824], BF16)
    self.load_w(w[:], w_in[l, :, 0:1824])
    mub = P.sb('mub', [128, 1824]); omub = P.sb('omub', [128, 1824])
    P.dma('sp', mub[:], I['rwkv_mu'][l].partition_broadcast(128))
    P.ts(omub[:], mub[:], -1.0, ALU.mult, 1.0, ALU.add)
    for kc in range(8):
        P.tt(wa[:, kc, :], w[:, kc, :], omub[:], ALU.mult, eng='dve')
        P.tt(wb[:, kc, :], w[:, kc, :], mub[:], ALU.mult, eng='dve')
    P.end_phase()
    dup = P.sb('dup', [64, 512], BF16); aup = P.sb('aup', [64, 512], BF16)
    P.dma('pool', dup[:], I['rwkv_decay_up'][l]); P.dma('pool', aup[:], I['rwkv_a_up'][l])
    gu1 = P.sb('gu1', [128, 512], BF16); gu2 = P.sb('gu2', [32, 512], BF16)
    P.dma('pool', gu1[:], I['rwkv_gate_up'][l, 0:128, :]); P.dma('pool', gu2[:], I['rwkv_gate_up'][l, 128:160, :])
    rw = P.sb('rw', [64, 6, 8])
    P.dma('sp', rw[:], I['h_rw'][l])
    w0, a0, kkw, kaw, rkw, v0w = [rw[:, i, :] for i in range(6)]
    lnw = P.sb('lnw', [64, 512]); lnb = P.sb('lnb', [64, 512])
    P.dma('sp', lnw[:], I['rwkv_ln_w'][l].partition_broadcast(64)); P.dma('sp', lnb[:], I['rwkv_ln_b'][l].partition_broadcast(64))
    m1 = P.sb('m1', [64, 128]); SL = P.sb('SL', [64, 64]); smask = P.sb('smask', [64, 1024])
    P.dma('sp', m1[:], I['c_m1']); P.dma('sp', SL[:], I['c_sl']); P.dma('sp', smask[:], I['c_scanmask'])
    if l > 0:
        wdown = P.sb('wdown', [128, 8, 32], BF16); wup = P.sb('wup', [32, 512], BF16)
        self.load_w(wdown[:], I['vres_down'][l - 1]); P.dma('pool', wup[:], I['vres_up'][l - 1])
        tb = P.sb('tb', [32, 128], BF16)
    I64 = self.ident[0:64, 0:64]
    T01 = P.ps('T01', [128, 1024]); T23 = P.ps('T23', [128, 1024])
    B4 = P.ps('B4', [128, 512]); B5 = P.ps('B5', [128, 512]); B6 = P.ps('B6', [128, 512]); B7 = P.ps('B7', [128, 512])
    def v8(t, c0=0, n=128):
        return t[0:64, c0:c0 + 8 * n].rearrange("p (h t) -> p h t", h=8)
    z = P.sb('z', [64, 26, 128]); zg = P.sb('zg', [128, 2, 128])
    txw = P.sb('txw', [64, 128], BF16); xab = P.sb('xab', [64, 128], BF16); sxg = P.sb('sxg', [128, 2, 128], BF16)
    Ta = P.sb('Ta', [64, 8, 128]); Tb = P.sb('Tb', [64, 8, 128]); Tc = P.sb('Tc', [64, 8, 128]); Td = P.sb('Td', [64, 8, 128])
    Te = P.sb('Te', [64, 8, 128]); Tf = P.sb('Tf', [64, 8, 128]); Tg = P.sb('Tg', [64, 8, 128]); Th = P.sb('Th', [64, 8, 128])
    AR = P.sb('AR', [64, 8, 2, 2, 64], BF16); gT = P.sb('gT', [64, 8, 2])
    Vc = P.sb('Vc', [64, 2, 8, 64]); Vcb = P.sb('Vcb', [64, 2, 8, 64], BF16)
    Khb = P.sb('Khb', [64, 8, 128], BF16); Bhb = P.sb('Bhb', [64, 8, 128], BF16)
    KTb = P.sb('KTb', [64, 2, 8, 64], BF16); BTb = P.sb('BTb', [64, 2, 8, 64], BF16)
    M1s = [P.sb('M1_%d' % i, [64, 8, 128], BF16) for i in range(2)]
    M2s = [P.sb('M2_%d' % i, [64, 8, 128], BF16) for i in range(2)]
    Qs = [P.sb('Q_%d' % i, [64, 8, 64], BF16) for i in range(2)]
    Xbs = [P.sb('Xb_%d' % i, [64, 8, 64], BF16) for i in range(2)]
    XTs = [P.sb('XT_%d' % i, [64, 8, 64]) for i in range(2)]
    XTb = [P.sb('XTb_%d' % i, [64, 8, 64], BF16) for i in range(2)]
    Wsb = P.sb('Wsb', [64, 8, 64], BF16); Usb = P.sb('Usb', [64, 8, 64], BF16)
    S0T = P.sb('S0T', [64, 8, 64]); S0Tb = P.sb('S0Tb', [64, 8, 64], BF16)
    Y1 = P.sb('Y1', [64, 8, 64]); Y2 = P.sb('Y2', [64, 8, 64]); obf = P.sb('obf', [64, 512])
    st8 = P.sb('st8', [64, 8]); st8b = P.sb('st8b', [64, 8]); sbon = P.sb('sbon', [64, 8])
    P.memset(zg[:], 0.0); P.memset(S0T[:], 0.0); P.memset(S0Tb[:], 0.0)
    r_ = z[:, 0:8, :]; k_ = z[:, 8:16, :]; vT = z[:, 16:24, :]
    Kh = z[:, 8:16, :]
    Bh = z[:, 0:8, :]
    b8 = lambda a: a.unsqueeze(2).broadcast_to([64, 8, 128])
    def v4(t):
        return t[0:64, 0:512].rearrange("p (h t) -> p h t", h=4)

    def emit_proj(tt):
        ts_ = slice(tt * 128, (tt + 1) * 128)

        def proj(out, c0, m):
            for kc in range(8):
                P.mm(out, wa[:, kc, c0:c0 + m], hT[:, kc, ts_], start=(kc == 0), stop=False)
            if tt == 0:
                for kc in range(8):
                    P.mm(out[:, 1:128], wb[:, kc, c0:c0 + m], hT[:, kc, 0:127], start=False, stop=(kc == 7))
            else:
                for kc in range(8):
                    P.mm(out, wb[:, kc, c0:c0 + m], hT[:, kc, tt * 128 - 1:(tt + 1) * 128 - 1], start=False, stop=(kc == 7))
        for grp, (pa_, pb_, c0) in enumerate([(B4, B5, 0), (B6, B7, 512), (B4, B5, 1024)]):
            for h in range(8):
                bank = pa_ if h < 4 else pb_
                proj(bank[0:64, (h % 4) * 128:(h % 4 + 1) * 128], c0 + h * 64, 64)
            P.copy(z[:, grp * 8:grp * 8 + 4, :], v4(pa_), eng='act')
            P.copy(z[:, grp * 8 + 4:grp * 8 + 8, :], v4(pb_), eng='act')
        for i in range(2):
            proj(B6[0:64, i * 128:(i + 1) * 128], 1536 + i * 64, 64)
        proj(B6[:, 256:384], 1664, 128)
        proj(B7[0:32, 0:128], 1792, 32)
        P.copy(z[:, 24:26, :], B6[0:64, 0:256].rearrange("p (c t) -> p c t", c=2), eng='act')
        P.copy(zg[:, 0, :], B6[:, 256:384], eng='act')
        P.copy(zg[0:32, 1, :], B7[0:32, 0:128], eng='act')

    emit_proj(0)
    for tt in range(NT):
        ts_ = slice(tt * 128, (tt + 1) * 128)
        P.act(txw[:], z[:, 24, :], AF.Tanh)
        P.copy(xab[:], z[:, 25, :], eng='act')
        P.act(sxg[:], zg[:], AF.Sigmoid)
        for h in range(8):
            P.mm(T01[0:64, h * 128:(h + 1) * 128], dup[:, h * 64:(h + 1) * 64], txw[:])
            P.mm(T23[0:64, h * 128:(h + 1) * 128], aup[:, h * 64:(h + 1) * 64], xab[:])
        P.tt(Ta[:], v8(T01), b8(w0), ALU.add)
        P.act(Ta[:], Ta[:], AF.Sigmoid)
        P.tt(Tb[:], v8(T23), b8(a0), ALU.add)
        P.act(Tb[:], Tb[:], AF.Sigmoid)
        P.scan(Tc[:].rearrange("p h t -> p (h t)"), smask[:], Ta[:].rearrange("p h t -> p (h t)"), 0.0, ALU.mult, ALU.add)
        P.act(Td[:], Tc[:], AF.Exp, scale=-0.6065306597126334)
        P.act(Te[:], Tc[:], AF.Exp, scale=0.6065306597126334)
        P.tt(Ta[:], Tc[:], Ta[:], ALU.subtract)
        P.act(Ta[:], Ta[:], AF.Exp, scale=-0.6065306597126334)
        P.copy(gT[:], Td[:].rearrange("p h (c t) -> p h c t", c=2)[:, :, :, 63], eng='act')
        P.tt(Tf[:], k_, b8(kkw), ALU.mult)
        P.act(Tg[:], Tf[:], AF.Square)
        P.mm(T01[0:64, 0:512], self.ones[0:64, 0:64], Tg[:, 0:4, :])
        P.mm(T01[0:64, 512:1024], self.ones[0:64, 0:64], Tg[:, 4:8, :])
        P.act(Tg[:], v8(T01), AF.Sqrt)
        P.ts(Tg[:], Tg[:], 1e-12, ALU.max)
        P.recip(Tg[:], Tg[:])
        P.tt(Tf[:], Tf[:], Tg[:], ALU.mult)
        P.stt(Tg[:], Tb[:], -1.0, b8(kaw), ALU.add, ALU.mult)
        P.stt(Tg[:], Tg[:], 1.0, k_, ALU.add, ALU.mult)
        P.tt(Th[:], r_, Tg[:], ALU.mult)
        P.tt(Th[:], Th[:], b8(rkw), ALU.mult)
        r4 = lambda a: a.rearrange("p h (c t) -> p h c t", c=2)
        P.tt(AR[:, :, :, 1, :], r4(r_), r4(Td[:]), ALU.mult)
        P.stt(AR[:, :, :, 0, :], r4(Tf[:]), -1.0, r4(Ta[:]), ALU.mult, ALU.mult)
        P.tt(Kh, Tg[:], Te[:], ALU.mult)
        P.tt(Tf[:], Tf[:], Tb[:], ALU.mult)
        P.tt(Bh, Tf[:], Te[:], ALU.mult)
        if l == 0:
            P.dma('sp', vfirst[tt], vT, track_dram=False)
        else:
            vf = Ta
            vm = Tb
            P.dma('sp', vf[:], vfirst[tt], track_dram=False)
            for kc in range(8):
                P.mm(B5[0:32, 0:128], wdown[:, kc, :], hT[:, kc, ts_], start=(kc == 0), stop=(kc == 7))
            P.copy(tb[:], B5[0:32, 0:128], eng='act')
            for h in range(8):
                P.mm(T23[0:64, h * 128:(h + 1) * 128], wup[:, h * 64:(h + 1) * 64], tb[:])
            P.tt(vm[:], v8(T23), b8(v0w), ALU.add)
            P.act(vm[:], vm[:], AF.Sigmoid)
            P.tt(vf[:], vf[:], vT, ALU.subtract)
            P.tt(vf[:], vf[:], vm[:], ALU.mult)
            P.tt(vT, vT, vf[:], ALU.add)
        for Xc in range(2):
            for h in range(8):
                P.tr(T23[0:64, (Xc * 8 + h) * 64:(Xc * 8 + h + 1) * 64], vT[:, h, Xc * 64:(Xc + 1) * 64], I64)
        P.copy(Vc[:].rearrange("p c h i -> p (c h i)"), T23[0:64, :], eng='act')
        P.copy(Vcb[:].rearrange("p c h i -> p (c h i)"), T23[0:64, :], eng='dve')
        P.copy(Khb[:], Kh, eng='act')
        P.copy(Bhb[:], Bh, eng='act')
        for Xc in range(2):
            for h in range(8):
                P.tr(T01[0:64, (Xc * 8 + h) * 64:(Xc * 8 + h + 1) * 64], Kh[:, h, Xc * 64:(Xc + 1) * 64], I64)
        P.copy(KTb[:].rearrange("p c h i -> p (c h i)"), T01[0:64, :], eng='act')
        for Xc in range(2):
            for h in range(8):
                P.tr(T23[0:64, (Xc * 8 + h) * 64:(Xc * 8 + h + 1) * 64], Bh[:, h, Xc * 64:(Xc + 1) * 64], I64)
        P.copy(BTb[:].rearrange("p c h i -> p (c h i)"), T23[0:64, :], eng='dve')
        I8 = I64.unsqueeze(1).broadcast_to([64, 8, 64])
        for Xc in range(2):
            cs = slice(Xc * 64, (Xc + 1) * 64)
            pS1 = v8(T01); pS2 = v8(T23); pQ0 = v8(B4, 0, 64)
            for h in range(8):
                ar = AR[:, h, Xc, :, :].rearrange("p a t -> p (a t)")
                P.mm(pS1[:, h, :], Bhb[:, h, cs], ar)
                P.mm(pS2[:, h, :], Khb[:, h, cs], ar)
                P.mm(pQ0[:, h, :], AR[:, h, Xc, 0, :], Bhb[:, h, cs])
            P.tt(M1s[Xc][:], pS1, m1[:].unsqueeze(1).broadcast_to([64, 8, 128]), ALU.mult)
            P.tt(M2s[Xc][:], pS2, m1[:].unsqueeze(1).broadcast_to([64, 8, 128]), ALU.mult)
            P.tt(Qs[Xc][:], pQ0, SL[:].unsqueeze(1).broadcast_to([64, 8, 64]), ALU.mult)
            P.tt(XTs[Xc][:], M1s[Xc][:, :, 0:64], I8, ALU.add)
            P.tt(Xbs[Xc][:], Qs[Xc][:], I8, ALU.add)
        bankP = [v8(B4, 0, 64), v8(T01, 0, 64)]
        bankQ = [v8(B5, 0, 64), v8(T01, 512, 64)]
        bankX1 = [v8(B6, 0, 64), v8(T23, 0, 64)]
        bankX2 = [v8(B7, 0, 64), v8(T23, 512, 64)]
        for kq in range(1, 6):
            last = (kq == 5)
            for Xc in range(2):
                Pm = M1s[Xc][:, :, 0:64]
                for h in range(8):
                    P.mm(bankP[Xc][:, h, :], Qs[Xc][:, h, :], Pm[:, h, :])
                if not last:
                    for h in range(8):
                        P.mm(bankQ[Xc][:, h, :], Pm[:, h, :], Qs[Xc][:, h, :])
            for Xc in range(2):
                Pm = M1s[Xc][:, :, 0:64]
                P.copy(Pm, bankP[Xc], eng='act')
                if not last:
                    P.copy(Qs[Xc][:], bankQ[Xc], eng='act')
            for Xc in range(2):
                Pm = M1s[Xc][:, :, 0:64]
                for h in range(8):
                    P.mm(bankX1[Xc][:, h, :], Xbs[Xc][:, h, :], Pm[:, h, :])
                if not last:
                    for h in range(8):
                        P.mm(bankX2[Xc][:, h, :], Pm[:, h, :], Xbs[Xc][:, h, :])
            for Xc in range(2):
                P.tt(XTs[Xc][:], XTs[Xc][:], bankX1[Xc], ALU.add)
                if not last:
                    P.tt(Xbs[Xc][:], Xbs[Xc][:], bankX2[Xc], ALU.add)
        for Xc in range(2):
            P.copy(XTb[Xc][:], XTs[Xc][:], eng='act')
        for Xc in range(2):
            cs = slice(Xc * 64, (Xc + 1) * 64)
            M1 = M1s[Xc]; M2 = M2s[Xc]
            if Xc == 0:
                pW = v8(B4, 0, 64); pU = v8(B5, 0, 64); pY = v8(B6, 0, 64); pSt = v8(B7, 0, 64)
                gbank = B4; obank = B5
            else:
                pW = v8(T01, 0, 64); pU = v8(T01, 512, 64); pY = v8(T23, 0, 64); pSt = v8(T23, 512, 64)
                gbank = T01[:, 0:512]; obank = T01[:, 512:1024]
            for h in range(8):
                P.mm(pW[:, h, :], M2[:, h, 0:64], Vcb[:, Xc, h, :], start=True, stop=False)
                P.mm(pW[:, h, :], AR[:, h, Xc, 0, :], S0Tb[:, h, :], start=False, stop=True)
            P.copy(Wsb[:], pW, eng='act')
            for h in range(8):
                P.mm(pU[:, h, :], XTb[Xc][:, h, :], Wsb[:, h, :])
            P.copy(Usb[:], pU, eng='act')
            for h in range(8):
                P.mm(pY[:, h, :], AR[:, h, Xc, 1, :], S0Tb[:, h, :], start=True, stop=False)
                P.mm(pY[:, h, :], M2[:, h, 64:128], Vcb[:, Xc, h, :], start=False, stop=False)
                P.mm(pY[:, h, :], M1[:, h, 64:128], Usb[:, h, :], start=False, stop=True)
            for h in range(8):
                P.mm(pSt[:, h, :], KTb[:, Xc, h, :], Vcb[:, Xc, h, :], start=True, stop=False)
                P.mm(pSt[:, h, :], BTb[:, Xc, h, :], Usb[:, h, :], start=False, stop=True)
            P.reduce(st8[:], pY, ALU.add)
            P.ts(st8[:], st8[:], -1.0 / 64, ALU.mult)
            P.tt(Y1[:], pY, st8[:].unsqueeze(2).broadcast_to([64, 8, 64]), ALU.add)
            P.act(Y2[:], Y1[:], AF.Square)
            P.reduce(st8b[:], Y2[:], ALU.add)
            P.act(st8b[:], st8b[:], AF.Sqrt, bias=64e-5, scale=1.0 / 64)
            P.recip(st8b[:], st8b[:])
            P.tt(Y1[:], Y1[:], st8b[:].unsqueeze(2).broadcast_to([64, 8, 64]), ALU.mult)
            Y1f = Y1[:].rearrange("p h i -> p (h i)")
            P.tt(Y1f, Y1f, lnw[:], ALU.mult)
            P.tt(Y1f, Y1f, lnb[:], ALU.add)
            P.tt(S0T[:], S0T[:], pSt, ALU.add)
            P.tt(S0T[:], S0T[:], gT[:, :, Xc:Xc + 1].broadcast_to([64, 8, 64]), ALU.mult)
            P.copy(S0Tb[:], S0T[:], eng='act')
            for h in range(8):
                P.mm(obank[0:64, h:h + 1], Th[:, h, cs], self.ones[0:64, 0:1])
            P.copy(sbon[:], obank[0:64, 0:8], eng='act')
            P.tt(Y2[:], Vc[:, Xc, :, :], sbon[:].unsqueeze(2).broadcast_to([64, 8, 64]), ALU.mult)
            P.tt(Y1[:], Y1[:], Y2[:], ALU.add)
            P.mm(gbank[0:64, :], sxg[:, 0, cs], gu1[:], start=True, stop=False)
            P.mm(gbank[0:64, :], sxg[0:32, 1, cs], gu2[:], start=False, stop=True)
            P.tt(obf[:], Y1f, gbank[0:64, :], ALU.mult)
            if Xc == 1 and tt + 1 < NT:
                emit_proj(tt + 1)
            for c in range(4):
                P.tr(obank[:, 256 + c * 64:256 + (c + 1) * 64], obf[:, c * 128:(c + 1) * 128], I64)
            st = self.ostage[self.ostage_i % 2]
            P.copy(st[:, :, cs], obank[:, 256:512].rearrange("p (c t) -> p c t", c=4), eng='act')
            if Xc == 1:
                self.ostage_i += 1
                P.dma('sp', oT[tt], st[:], track_dram=False)
    P.end_phase()


K.rwkv = rwkv4


def rwkv6(self, l, hT, oT, vfirst):
    P = self.P
    I = self.inp
    w_in = I['w_in']
    P.begin_phase()
    wa = P.sb('wa_rwkv', [128, 8, 1824], BF16)
    wb = P.sb('wb_rwkv', [128, 8, 1824], BF16)
    P.begin_phase()
    w = P.sb('w_rwkv', [128, 8, 1824], BF16)
    self.load_w(w[:], w_in[l, :, 0:1824])
    mub = P.sb('mub', [128, 1824]); omub = P.sb('omub', [128, 1824])
    P.dma('sp', mub[:], I['rwkv_mu'][l].partition_broadcast(128))
    P.ts(omub[:], mub[:], -1.0, ALU.mult, 1.0, ALU.add)
    for kc in range(8):
        P.tt(wa[:, kc, :], w[:, kc, :], omub[:], ALU.mult, eng='dve')
        P.tt(wb[:, kc, :], w[:, kc, :], mub[:], ALU.mult, eng='dve')
    P.end_phase()
    dup = P.sb('dup', [64, 512], BF16); aup = P.sb('aup', [64, 512], BF16)
    P.dma('pool', dup[:], I['rwkv_decay_up'][l]); P.dma('pool', aup[:], I['rwkv_a_up'][l])
    gu1 = P.sb('gu1', [128, 512], BF16); gu2 = P.sb('gu2', [32, 512], BF16)
    P.dma('pool', gu1[:], I['rwkv_gate_up'][l, 0:128, :]); P.dma('pool', gu2[:], I['rwkv_gate_up'][l, 128:160, :])
    rw = P.sb('rw', [64, 6, 8])
    P.dma('sp', rw[:], I['h_rw'][l])
    w0, a0, kkw, kaw, rkw, v0w = [rw[:, i, :] for i in range(6)]
    lnw = P.sb('lnw', [64, 512]); lnb = P.sb('lnb', [64, 512])
    P.dma('sp', lnw[:], I['rwkv_ln_w'][l].partition_broadcast(64)); P.dma('sp', lnb[:], I['rwkv_ln_b'][l].partition_broadcast(64))
    m1 = P.sb('m1', [64, 128]); SL = P.sb('SL', [64, 64]); smask = P.sb('smask', [64, 1024])
    P.dma('sp', m1[:], I['c_m1']); P.dma('sp', SL[:], I['c_sl']); P.dma('sp', smask[:], I['c_scanmask'])
    if l > 0:
        wdown = P.sb('wdown', [128, 8, 32], BF16); wup = P.sb('wup', [32, 512], BF16)
        self.load_w(wdown[:], I['vres_down'][l - 1]); P.dma('pool', wup[:], I['vres_up'][l - 1])
        tb = P.sb('tb', [32, 128], BF16)
    I64 = self.ident[0:64, 0:64]
    T01 = P.ps('T01', [128, 1024]); T23 = P.ps('T23', [128, 1024])
    B4 = P.ps('B4', [128, 512]); B5 = P.ps('B5', [128, 512]); B6 = P.ps('B6', [128, 512]); B7 = P.ps('B7', [128, 512])
    def v8(t, c0=0, n=128):
        return t[0:64, c0:c0 + 8 * n].rearrange("p (h t) -> p h t", h=8)
    z = P.sb('z', [64, 26, 128]); zg = P.sb('zg', [128, 2, 128])
    txw = P.sb('txw', [64, 128], BF16); xab = P.sb('xab', [64, 128], BF16); sxg = P.sb('sxg', [128, 2, 128], BF16)
    Ta = P.sb('Ta', [64, 8, 128]); Tb = P.sb('Tb', [64, 8, 128]); Tc = P.sb('Tc', [64, 8, 128]); Td = P.sb('Td', [64, 8, 128])
    Te = P.sb('Te', [64, 8, 128]); Tf = P.sb('Tf', [64, 8, 128]); Tg = P.sb('Tg', [64, 8, 128]); Th = P.sb('Th', [64, 8, 128])
    AR = P.sb('AR', [64, 8, 2, 2, 64], BF16); gT = P.sb('gT', [64, 8, 2])
    Vc = P.sb('Vc', [64, 2, 8, 64]); Vcb = P.sb('Vcb', [64, 2, 8, 64], BF16)
    Khb = P.sb('Khb', [64, 8, 128], BF16); Bhb = P.sb('Bhb', [64, 8, 128], BF16)
    KTb = P.sb('KTb', [64, 2, 8, 64], BF16); BTb = P.sb('BTb', [64, 2, 8, 64], BF16)
    M1s = [P.sb('M1_%d' % i, [64, 8, 128], BF16) for i in range(2)]
    M2s = [P.sb('M2_%d' % i, [64, 8, 128], BF16) for i in range(2)]
    Qs = [P.sb('Q_%d' % i, [64, 8, 64], BF16) for i in range(2)]
    Xbs = [P.sb('Xb_%d' % i, [64, 8, 64], BF16) for i in range(2)]
    XTs = [P.sb('XT_%d' % i, [64, 8, 64]) for i in range(2)]
    XTb = [P.sb('XTb_%d' % i, [64, 8, 64], BF16) for i in range(2)]
    Wsb = P.sb('Wsb', [64, 8, 64], BF16); Usb = P.sb('Usb', [64, 8, 64], BF16)
    S0T = P.sb('S0T', [64, 8, 64]); S0Tb = P.sb('S0Tb', [64, 8, 64], BF16)
    Y1 = P.sb('Y1', [64, 8, 64]); Y2 = P.sb('Y2', [64, 8, 64]); obf = P.sb('obf', [64, 512])
    st8 = P.sb('st8', [64, 8]); st8b = P.sb('st8b', [64, 8]); sbon = P.sb('sbon', [64, 8])
    P.memset(zg[:], 0.0); P.memset(S0T[:], 0.0); P.memset(S0Tb[:], 0.0)
    r_ = z[:, 0:8, :]; k_ = z[:, 8:16, :]; vT = z[:, 16:24, :]
    Kh = z[:, 8:16, :]
    Bh = z[:, 0:8, :]
    b8 = lambda a: a.unsqueeze(2).broadcast_to([64, 8, 128])
    def v4(t):
        return t[0:64, 0:512].rearrange("p (h t) -> p h t", h=4)

    def emit_proj(tt, groups=(0, 1, 2, 3)):
        ts_ = slice(tt * 128, (tt + 1) * 128)

        def proj(out, c0, m):
            for kc in range(8):
                P.mm(out, wa[:, kc, c0:c0 + m], hT[:, kc, ts_], start=(kc == 0), stop=False)
            if tt == 0:
                for kc in range(8):
                    P.mm(out[:, 1:128], wb[:, kc, c0:c0 + m], hT[:, kc, 0:127], start=False, stop=(kc == 7))
            else:
                for kc in range(8):
                    P.mm(out, wb[:, kc, c0:c0 + m], hT[:, kc, tt * 128 - 1:(tt + 1) * 128 - 1], start=False, stop=(kc == 7))
        for grp, (pa_, pb_, c0) in enumerate([(B4, B5, 0), (B6, B7, 512), (B4, B5, 1024)]):
            if grp not in groups:
                continue
            for h in range(8):
                bank = pa_ if h < 4 else pb_
                proj(bank[0:64, (h % 4) * 128:(h % 4 + 1) * 128], c0 + h * 64, 64)
            P.copy(z[:, grp * 8:grp * 8 + 4, :], v4(pa_), eng='act')
            P.copy(z[:, grp * 8 + 4:grp * 8 + 8, :], v4(pb_), eng='act')
        if 3 in groups:
            for i in range(2):
                proj(B6[0:64, i * 128:(i + 1) * 128], 1536 + i * 64, 64)
            proj(B6[:, 256:384], 1664, 128)
            proj(B7[0:32, 0:128], 1792, 32)
            P.copy(z[:, 24:26, :], B6[0:64, 0:256].rearrange("p (c t) -> p c t", c=2), eng='act')
            P.copy(zg[:, 0, :], B6[:, 256:384], eng='act')
            P.copy(zg[0:32, 1, :], B7[0:32, 0:128], eng='act')

    emit_proj(0)
    for tt in range(NT):
        ts_ = slice(tt * 128, (tt + 1) * 128)
        P.act(txw[:], z[:, 24, :], AF.Tanh)
        P.copy(xab[:], z[:, 25, :], eng='act')
        P.act(sxg[:], zg[:], AF.Sigmoid)
        for h in range(8):
            P.mm(T01[0:64, h * 128:(h + 1) * 128], dup[:, h * 64:(h + 1) * 64], txw[:])
            P.mm(T23[0:64, h * 128:(h + 1) * 128], aup[:, h * 64:(h + 1) * 64], xab[:])
        P.tt(Ta[:], v8(T01), b8(w0), ALU.add)
        P.act(Ta[:], Ta[:], AF.Sigmoid)
        P.tt(Tb[:], v8(T23), b8(a0), ALU.add)
        P.act(Tb[:], Tb[:], AF.Sigmoid)
        P.scan(Tc[:].rearrange("p h t -> p (h t)"), smask[:], Ta[:].rearrange("p h t -> p (h t)"), 0.0, ALU.mult, ALU.add)
        P.act(Td[:], Tc[:], AF.Exp, scale=-0.6065306597126334)
        P.act(Te[:], Tc[:], AF.Exp, scale=0.6065306597126334)
        P.tt(Ta[:], Tc[:], Ta[:], ALU.subtract)
        P.act(Ta[:], Ta[:], AF.Exp, scale=-0.6065306597126334)
        P.copy(gT[:], Td[:].rearrange("p h (c t) -> p h c t", c=2)[:, :, :, 63], eng='act')
        P.tt(Tf[:], k_, b8(kkw), ALU.mult)
        P.act(Tg[:], Tf[:], AF.Square)
        P.mm(T01[0:64, 0:512], self.ones[0:64, 0:64], Tg[:, 0:4, :])
        P.mm(T01[0:64, 512:1024], self.ones[0:64, 0:64], Tg[:, 4:8, :])
        P.act(Tg[:], v8(T01), AF.Sqrt)
        P.ts(Tg[:], Tg[:], 1e-12, ALU.max)
        P.recip(Tg[:], Tg[:])
        P.tt(Tf[:], Tf[:], Tg[:], ALU.mult)
        P.stt(Tg[:], Tb[:], -1.0, b8(kaw), ALU.add, ALU.mult)
        P.stt(Tg[:], Tg[:], 1.0, k_, ALU.add, ALU.mult)
        P.tt(Th[:], r_, Tg[:], ALU.mult)
        P.tt(Th[:], Th[:], b8(rkw), ALU.mult)
        r4 = lambda a: a.rearrange("p h (c t) -> p h c t", c=2)
        P.tt(AR[:, :, :, 1, :], r4(r_), r4(Td[:]), ALU.mult)
        P.stt(AR[:, :, :, 0, :], r4(Tf[:]), -1.0, r4(Ta[:]), ALU.mult, ALU.mult)
        P.tt(Kh, Tg[:], Te[:], ALU.mult)
        P.tt(Tf[:], Tf[:], Tb[:], ALU.mult)
        P.tt(Bh, Tf[:], Te[:], ALU.mult)
        if l == 0:
            P.dma('sp', vfirst[tt], vT, track_dram=False)
        else:
            vf = Ta
            vm = Tb
            P.dma('sp', vf[:], vfirst[tt], track_dram=False)
            for kc in range(8):
                P.mm(B5[0:32, 0:128], wdown[:, kc, :], hT[:, kc, ts_], start=(kc == 0), stop=(kc == 7))
            P.copy(tb[:], B5[0:32, 0:128], eng='act')
            for h in range(8):
                P.mm(T23[0:64, h * 128:(h + 1) * 128], wup[:, h * 64:(h + 1) * 64], tb[:])
            P.tt(vm[:], v8(T23), b8(v0w), ALU.add)
            P.act(vm[:], vm[:], AF.Sigmoid)
            P.tt(vf[:], vf[:], vT, ALU.subtract)
            P.tt(vf[:], vf[:], vm[:], ALU.mult)
            P.tt(vT, vT, vf[:], ALU.add)
        for Xc in range(2):
            for h in range(8):
                P.tr(T23[0:64, (Xc * 8 + h) * 64:(Xc * 8 + h + 1) * 64], vT[:, h, Xc * 64:(Xc + 1) * 64], I64)
        P.copy(Vc[:].rearrange("p c h i -> p (c h i)"), T23[0:64, :], eng='act')
        P.copy(Vcb[:].rearrange("p c h i -> p (c h i)"), T23[0:64, :], eng='dve')
        P.copy(Khb[:], Kh, eng='act')
        P.copy(Bhb[:], Bh, eng='act')
        for Xc in range(2):
            for h in range(8):
                P.tr(T01[0:64, (Xc * 8 + h) * 64:(Xc * 8 + h + 1) * 64], Kh[:, h, Xc * 64:(Xc + 1) * 64], I64)
        P.copy(KTb[:].rearrange("p c h i -> p (c h i)"), T01[0:64, :], eng='act')
        for Xc in range(2):
            for h in range(8):
                P.tr(T23[0:64, (Xc * 8 + h) * 64:(Xc * 8 + h + 1) * 64], Bh[:, h, Xc * 64:(Xc + 1) * 64], I64)
        P.copy(BTb[:].rearrange("p c h i -> p (c h i)"), T23[0:64, :], eng='dve')
        I8 = I64.unsqueeze(1).broadcast_to([64, 8, 64])
        for Xc in range(2):
            cs = slice(Xc * 64, (Xc + 1) * 64)
            pS1 = v8(T01); pS2 = v8(T23); pQ0 = v8(B4, 0, 64)
            for h in range(8):
                ar = AR[:, h, Xc, :, :].rearrange("p a t -> p (a t)")
                P.mm(pS1[:, h, :], Bhb[:, h, cs], ar)
                P.mm(pS2[:, h, :], Khb[:, h, cs], ar)
                P.mm(pQ0[:, h, :], AR[:, h, Xc, 0, :], Bhb[:, h, cs])
            P.tt(M1s[Xc][:], pS1, m1[:].unsqueeze(1).broadcast_to([64, 8, 128]), ALU.mult)
            P.tt(M2s[Xc][:], pS2, m1[:].unsqueeze(1).broadcast_to([64, 8, 128]), ALU.mult)
            P.tt(Qs[Xc][:], pQ0, SL[:].unsqueeze(1).broadcast_to([64, 8, 64]), ALU.mult)
            P.tt(XTs[Xc][:], M1s[Xc][:, :, 0:64], I8, ALU.add)
            P.tt(Xbs[Xc][:], Qs[Xc][:], I8, ALU.add)
        bankP = [v8(B4, 0, 64), v8(T01, 0, 64)]
        bankQ = [v8(B5, 0, 64), v8(T01, 512, 64)]
        bankX1 = [v8(B6, 0, 64), v8(T23, 0, 64)]
        bankX2 = [v8(B7, 0, 64), v8(T23, 512, 64)]
        for kq in range(1, 6):
            last = (kq == 5)
            for Xc in range(2):
                Pm = M1s[Xc][:, :, 0:64]
                for h in range(8):
                    P.mm(bankP[Xc][:, h, :], Qs[Xc][:, h, :], Pm[:, h, :])
                if not last:
                    for h in range(8):
                        P.mm(bankQ[Xc][:, h, :], Pm[:, h, :], Qs[Xc][:, h, :])
            for Xc in range(2):
                Pm = M1s[Xc][:, :, 0:64]
                P.copy(Pm, bankP[Xc], eng='act')
                if not last:
                    P.copy(Qs[Xc][:], bankQ[Xc], eng='act')
            for Xc in range(2):
                Pm = M1s[Xc][:, :, 0:64]
                for h in range(8):
                    P.mm(bankX1[Xc][:, h, :], Xbs[Xc][:, h, :], Pm[:, h, :])
                if not last:
                    for h in range(8):
                        P.mm(bankX2[Xc][:, h, :], Pm[:, h, :], Xbs[Xc][:, h, :])
            for Xc in range(2):
                P.tt(XTs[Xc][:], XTs[Xc][:], bankX1[Xc], ALU.add)
                if not last:
                    P.tt(Xbs[Xc][:], Xbs[Xc][:], bankX2[Xc], ALU.add)
        for Xc in range(2):
            P.copy(XTb[Xc][:], XTs[Xc][:], eng='act')
        for Xc in range(2):
            cs = slice(Xc * 64, (Xc + 1) * 64)
            M1 = M1s[Xc]; M2 = M2s[Xc]
            if Xc == 0:
                pW = v8(B4, 0, 64); pU = v8(B5, 0, 64); pY = v8(B6, 0, 64); pSt = v8(B7, 0, 64)
                gbank = B4; obank = B5
            else:
                pW = v8(T01, 0, 64); pU = v8(T01, 512, 64); pY = v8(T23, 0, 64); pSt = v8(T23, 512, 64)
                gbank = T01[:, 0:512]; obank = T01[:, 512:1024]
            for h in range(8):
                P.mm(pW[:, h, :], M2[:, h, 0:64], Vcb[:, Xc, h, :], start=True, stop=False)
                P.mm(pW[:, h, :], AR[:, h, Xc, 0, :], S0Tb[:, h, :], start=False, stop=True)
            P.copy(Wsb[:], pW, eng='act')
            for h in range(8):
                P.mm(pU[:, h, :], XTb[Xc][:, h, :], Wsb[:, h, :])
            P.copy(Usb[:], pU, eng='act')
            for h in range(8):
                P.mm(pY[:, h, :], AR[:, h, Xc, 1, :], S0Tb[:, h, :], start=True, stop=False)
                P.mm(pY[:, h, :], M2[:, h, 64:128], Vcb[:, Xc, h, :], start=False, stop=False)
                P.mm(pY[:, h, :], M1[:, h, 64:128], Usb[:, h, :], start=False, stop=True)
            for h in range(8):
                P.mm(pSt[:, h, :], KTb[:, Xc, h, :], Vcb[:, Xc, h, :], start=True, stop=False)
                P.mm(pSt[:, h, :], BTb[:, Xc, h, :], Usb[:, h, :], start=False, stop=True)
            P.reduce(st8[:], pY, ALU.add)
            P.ts(st8[:], st8[:], -1.0 / 64, ALU.mult)
            P.tt(Y1[:], pY, st8[:].unsqueeze(2).broadcast_to([64, 8, 64]), ALU.add)
            P.act(Y2[:], Y1[:], AF.Square)
            P.reduce(st8b[:], Y2[:], ALU.add)
            P.act(st8b[:], st8b[:], AF.Sqrt, bias=64e-5, scale=1.0 / 64)
            P.recip(st8b[:], st8b[:])
            P.tt(Y1[:], Y1[:], st8b[:].unsqueeze(2).broadcast_to([64, 8, 64]), ALU.mult)
            Y1f = Y1[:].rearrange("p h i -> p (h i)")
            P.tt(Y1f, Y1f, lnw[:], ALU.mult)
            P.tt(Y1f, Y1f, lnb[:], ALU.add)
            P.tt(S0T[:], S0T[:], pSt, ALU.add)
            P.tt(S0T[:], S0T[:], gT[:, :, Xc:Xc + 1].broadcast_to([64, 8, 64]), ALU.mult)
            P.copy(S0Tb[:], S0T[:], eng='act')
            for h in range(8):
                P.mm(obank[0:64, h:h + 1], Th[:, h, cs], self.ones[0:64, 0:1])
            P.copy(sbon[:], obank[0:64, 0:8], eng='act')
            P.tt(Y2[:], Vc[:, Xc, :, :], sbon[:].unsqueeze(2).broadcast_to([64, 8, 64]), ALU.mult)
            P.tt(Y1[:], Y1[:], Y2[:], ALU.add)
            P.mm(gbank[0:64, :], sxg[:, 0, cs], gu1[:], start=True, stop=False)
            P.mm(gbank[0:64, :], sxg[0:32, 1, cs], gu2[:], start=False, stop=True)
            P.tt(obf[:], Y1f, gbank[0:64, :], ALU.mult)
            if tt + 1 < NT:
                emit_proj(tt + 1, groups=(0, 1) if Xc == 0 else (2, 3))
            for c in range(4):
                P.tr(obank[:, 256 + c * 64:256 + (c + 1) * 64], obf[:, c * 128:(c + 1) * 128], I64)
            st = self.ostage[self.ostage_i % 2]
            P.copy(st[:, :, cs], obank[:, 256:512].rearrange("p (c t) -> p c t", c=4), eng='act')
            if Xc == 1:
                self.ostage_i += 1
                P.dma('sp', oT[tt], st[:], track_dram=False)
    P.end_phase()


K.rwkv = rwkv6


def rwkv5(self, l, hT, oT, vfirst):
    P = self.P
    I = self.inp
    w_in = I['w_in']
    P.begin_phase()
    wa = P.sb('wa_rwkv', [128, 8, 1824], BF16)
    wb = P.sb('wb_rwkv', [128, 8, 1824], BF16)
    P.begin_phase()
    w = P.sb('w_rwkv', [128, 8, 1824], BF16)
    self.load_w(w[:], w_in[l, :, 0:1824])
    mub = P.sb('mub', [128, 1824]); omub = P.sb('omub', [128, 1824])
    P.dma('sp', mub[:], I['rwkv_mu'][l].partition_broadcast(128))
    P.ts(omub[:], mub[:], -1.0, ALU.mult, 1.0, ALU.add)
    for kc in range(8):
        P.tt(wa[:, kc, :], w[:, kc, :], omub[:], ALU.mult, eng='dve')
        P.tt(wb[:, kc, :], w[:, kc, :], mub[:], ALU.mult, eng='dve')
    P.end_phase()
    dup = P.sb('dup', [64, 512], BF16); aup = P.sb('aup', [64, 512], BF16)
    P.dma('pool', dup[:], I['rwkv_decay_up'][l]); P.dma('pool', aup[:], I['rwkv_a_up'][l])
    gu1 = P.sb('gu1', [128, 512], BF16); gu2 = P.sb('gu2', [32, 512], BF16)
    P.dma('pool', gu1[:], I['rwkv_gate_up'][l, 0:128, :]); P.dma('pool', gu2[:], I['rwkv_gate_up'][l, 128:160, :])
    rw = P.sb('rw', [64, 6, 8])
    P.dma('sp', rw[:], I['h_rw'][l])
    w0, a0, kkw, kaw, rkw, v0w = [rw[:, i, :] for i in range(6)]
    lnw = P.sb('lnw', [64, 512]); lnb = P.sb('lnb', [64, 512])
    P.dma('sp', lnw[:], I['rwkv_ln_w'][l].partition_broadcast(64)); P.dma('sp', lnb[:], I['rwkv_ln_b'][l].partition_broadcast(64))
    m1 = P.sb('m1', [64, 128]); SL = P.sb('SL', [64, 64]); smask = P.sb('smask', [64, 1024])
    P.dma('sp', m1[:], I['c_m1']); P.dma('sp', SL[:], I['c_sl']); P.dma('sp', smask[:], I['c_scanmask'])
    if l > 0:
        wdown = P.sb('wdown', [128, 8, 32], BF16); wup = P.sb('wup', [32, 512], BF16)
        self.load_w(wdown[:], I['vres_down'][l - 1]); P.dma('pool', wup[:], I['vres_up'][l - 1])
        tb = P.sb('tb', [32, 128], BF16)
    I64 = self.ident[0:64, 0:64]
    T01 = P.ps('T01', [128, 1024]); T23 = P.ps('T23', [128, 1024])
    B4 = P.ps('B4', [128, 512]); B5 = P.ps('B5', [128, 512]); B6 = P.ps('B6', [128, 512]); B7 = P.ps('B7', [128, 512])
    def v8(t, c0=0, n=128):
        return t[0:64, c0:c0 + 8 * n].rearrange("p (h t) -> p h t", h=8)
    zbuf = [P.sb('z%d' % i, [64, 26, 128]) for i in range(2)]
    zgbuf = [P.sb('zg%d' % i, [128, 2, 128]) for i in range(2)]
    txw = P.sb('txw', [64, 128], BF16); xab = P.sb('xab', [64, 128], BF16); sxg = P.sb('sxg', [128, 2, 128], BF16)
    Ta = P.sb('Ta', [64, 8, 128]); Tb = P.sb('Tb', [64, 8, 128]); Tc = P.sb('Tc', [64, 8, 128]); Td = P.sb('Td', [64, 8, 128])
    Te = P.sb('Te', [64, 8, 128]); Tf = P.sb('Tf', [64, 8, 128]); Tg = P.sb('Tg', [64, 8, 128]); Th = P.sb('Th', [64, 8, 128])
    AR = P.sb('AR', [64, 8, 2, 2, 64], BF16); gT = P.sb('gT', [64, 8, 2])
    Vc = P.sb('Vc', [64, 2, 8, 64]); Vcb = P.sb('Vcb', [64, 2, 8, 64], BF16)
    Khb = P.sb('Khb', [64, 8, 128], BF16); Bhb = P.sb('Bhb', [64, 8, 128], BF16)
    KTb = P.sb('KTb', [64, 2, 8, 64], BF16); BTb = P.sb('BTb', [64, 2, 8, 64], BF16)
    M1s = [P.sb('M1_%d' % i, [64, 8, 128], BF16) for i in range(2)]
    M2s = [P.sb('M2_%d' % i, [64, 8, 128], BF16) for i in range(2)]
    Qs = [P.sb('Q_%d' % i, [64, 8, 64], BF16) for i in range(2)]
    Xbs = [P.sb('Xb_%d' % i, [64, 8, 64], BF16) for i in range(2)]
    XTb = [P.sb('XTb_%d' % i, [64, 8, 64], BF16) for i in range(2)]
    Wsb = P.sb('Wsb', [64, 8, 64], BF16); Usb = P.sb('Usb', [64, 8, 64], BF16)
    S0T = P.sb('S0T', [64, 8, 64]); S0Tb = P.sb('S0Tb', [64, 8, 64], BF16)
    _fl = lambda t: t[:].rearrange("p h t -> p (h t)")[:, 0:512]
    XTs = [_fl(Td).rearrange("p (h i) -> p h i", h=8), _fl(Te).rearrange("p (h i) -> p h i", h=8)]
    Y1 = _fl(Ta).rearrange("p (h i) -> p h i", h=8); Y2 = _fl(Tb).rearrange("p (h i) -> p h i", h=8); obf = _fl(Tc)
    st8 = P.sb('st8', [64, 8]); st8b = P.sb('st8b', [64, 8]); sbon = P.sb('sbon', [64, 8])
    P.memset(zgbuf[0][:], 0.0); P.memset(zgbuf[1][:], 0.0); P.memset(S0T[:], 0.0); P.memset(S0Tb[:], 0.0)
    b8 = lambda a: a.unsqueeze(2).broadcast_to([64, 8, 128])
    def v4(t):
        return t[0:64, 0:512].rearrange("p (h t) -> p h t", h=4)

    def emit_proj(tt, part):
        ts_ = slice(tt * 128, (tt + 1) * 128)
        z = zbuf[tt % 2]; zg = zgbuf[tt % 2]

        def proj(out, c0, m):
            for kc in range(8):
                P.mm(out, wa[:, kc, c0:c0 + m], hT[:, kc, ts_], start=(kc == 0), stop=False)
            if tt == 0:
                for kc in range(8):
                    P.mm(out[:, 1:128], wb[:, kc, c0:c0 + m], hT[:, kc, 0:127], start=False, stop=(kc == 7))
            else:
                for kc in range(8):
                    P.mm(out, wb[:, kc, c0:c0 + m], hT[:, kc, tt * 128 - 1:(tt + 1) * 128 - 1], start=False, stop=(kc == 7))
        if part < 3:
            grp = part
            pa_, pb_ = (B4, B5) if grp != 1 else (B6, B7)
            c0 = grp * 512
            for h in range(8):
                bank = pa_ if h < 4 else pb_
                proj(bank[0:64, (h % 4) * 128:(h % 4 + 1) * 128], c0 + h * 64, 64)
            P.copy(z[:, grp * 8:grp * 8 + 4, :], v4(pa_), eng='act')
            P.copy(z[:, grp * 8 + 4:grp * 8 + 8, :], v4(pb_), eng='act')
        else:
            for i in range(2):
                proj(B6[0:64, i * 128:(i + 1) * 128], 1536 + i * 64, 64)
            proj(B6[:, 256:384], 1664, 128)
            proj(B7[0:32, 0:128], 1792, 32)
            P.copy(z[:, 24:26, :], B6[0:64, 0:256].rearrange("p (c t) -> p c t", c=2), eng='act')
            P.copy(zg[:, 0, :], B6[:, 256:384], eng='act')
            P.copy(zg[0:32, 1, :], B7[0:32, 0:128], eng='act')

    for part in range(4):
        emit_proj(0, part)
    for tt in range(NT):
        ts_ = slice(tt * 128, (tt + 1) * 128)
        z = zbuf[tt % 2]; zg = zgbuf[tt % 2]
        r_ = z[:, 0:8, :]; k_ = z[:, 8:16, :]; vT = z[:, 16:24, :]
        Kh = z[:, 8:16, :]; Bh = z[:, 0:8, :]
        nxt = (lambda p: emit_proj(tt + 1, p)) if tt + 1 < NT else (lambda p: None)
        P.act(txw[:], z[:, 24, :], AF.Tanh)
        P.copy(xab[:], z[:, 25, :], eng='act')
        P.act(sxg[:], zg[:], AF.Sigmoid)
        for h in range(8):
            P.mm(T01[0:64, h * 128:(h + 1) * 128], dup[:, h * 64:(h + 1) * 64], txw[:])
            P.mm(T23[0:64, h * 128:(h + 1) * 128], aup[:, h * 64:(h + 1) * 64], xab[:])
        nxt(0)
        P.tt(Ta[:], v8(T01), b8(w0), ALU.add)
        P.act(Ta[:], Ta[:], AF.Sigmoid)
        P.tt(Tb[:], v8(T23), b8(a0), ALU.add)
        P.act(Tb[:], Tb[:], AF.Sigmoid)
        P.scan(Tc[:].rearrange("p h t -> p (h t)"), smask[:], Ta[:].rearrange("p h t -> p (h t)"), 0.0, ALU.mult, ALU.add)
        P.act(Td[:], Tc[:], AF.Exp, scale=-0.6065306597126334)
        P.act(Te[:], Tc[:], AF.Exp, scale=0.6065306597126334)
        P.tt(Ta[:], Tc[:], Ta[:], ALU.subtract)
        P.act(Ta[:], Ta[:], AF.Exp, scale=-0.6065306597126334)
        P.copy(gT[:], Td[:].rearrange("p h (c t) -> p h c t", c=2)[:, :, :, 63], eng='act')
        P.tt(Tf[:], k_, b8(kkw), ALU.mult)
        P.act(Tg[:], Tf[:], AF.Square)
        P.mm(T01[0:64, 0:512], self.ones[0:64, 0:64], Tg[:, 0:4, :])
        P.mm(T01[0:64, 512:1024], self.ones[0:64, 0:64], Tg[:, 4:8, :])
        nxt(1)
        P.act(Tg[:], v8(T01), AF.Sqrt)
        P.ts(Tg[:], Tg[:], 1e-12, ALU.max)
        P.recip(Tg[:], Tg[:])
        P.tt(Tf[:], Tf[:], Tg[:], ALU.mult)
        P.stt(Tg[:], Tb[:], -1.0, b8(kaw), ALU.add, ALU.mult)
        P.stt(Tg[:], Tg[:], 1.0, k_, ALU.add, ALU.mult)
        P.tt(Th[:], r_, Tg[:], ALU.mult)
        P.tt(Th[:], Th[:], b8(rkw), ALU.mult)
        r4 = lambda a: a.rearrange("p h (c t) -> p h c t", c=2)
        P.tt(AR[:, :, :, 1, :], r4(r_), r4(Td[:]), ALU.mult)
        P.stt(AR[:, :, :, 0, :], r4(Tf[:]), -1.0, r4(Ta[:]), ALU.mult, ALU.mult)
        P.tt(Kh, Tg[:], Te[:], ALU.mult)
        P.tt(Tf[:], Tf[:], Tb[:], ALU.mult)
        P.tt(Bh, Tf[:], Te[:], ALU.mult)
        if l == 0:
            P.dma('sp', vfirst[tt], vT, track_dram=False)
        else:
            vf = Ta
            vm = Tb
            P.dma('sp', vf[:], vfirst[tt], track_dram=False)
            for kc in range(8):
                P.mm(T01[0:32, 0:128], wdown[:, kc, :], hT[:, kc, ts_], start=(kc == 0), stop=(kc == 7))
            P.copy(tb[:], T01[0:32, 0:128], eng='act')
            for h in range(8):
                P.mm(T23[0:64, h * 128:(h + 1) * 128], wup[:, h * 64:(h + 1) * 64], tb[:])
            P.tt(vm[:], v8(T23), b8(v0w), ALU.add)
            P.act(vm[:], vm[:], AF.Sigmoid)
            P.tt(vf[:], vf[:], vT, ALU.subtract)
            P.tt(vf[:], vf[:], vm[:], ALU.mult)
            P.tt(vT, vT, vf[:], ALU.add)
        nxt(2)
        for Xc in range(2):
            for h in range(8):
                P.tr(T23[0:64, (Xc * 8 + h) * 64:(Xc * 8 + h + 1) * 64], vT[:, h, Xc * 64:(Xc + 1) * 64], I64)
        P.copy(Vc[:].rearrange("p c h i -> p (c h i)"), T23[0:64, :], eng='act')
        P.copy(Vcb[:].rearrange("p c h i -> p (c h i)"), T23[0:64, :], eng='dve')
        P.copy(Khb[:], Kh, eng='act')
        P.copy(Bhb[:], Bh, eng='act')
        for Xc in range(2):
            for h in range(8):
                P.tr(T01[0:64, (Xc * 8 + h) * 64:(Xc * 8 + h + 1) * 64], Kh[:, h, Xc * 64:(Xc + 1) * 64], I64)
        P.copy(KTb[:].rearrange("p c h i -> p (c h i)"), T01[0:64, :], eng='act')
        for Xc in range(2):
            for h in range(8):
                P.tr(T23[0:64, (Xc * 8 + h) * 64:(Xc * 8 + h + 1) * 64], Bh[:, h, Xc * 64:(Xc + 1) * 64], I64)
        P.copy(BTb[:].rearrange("p c h i -> p (c h i)"), T23[0:64, :], eng='dve')
        nxt(3)
        I8 = I64.unsqueeze(1).broadcast_to([64, 8, 64])
        for Xc in range(2):
            cs = slice(Xc * 64, (Xc + 1) * 64)
            pS1 = v8(T01); pS2 = v8(T23); pQ0 = v8(B4, 0, 64)
            for h in range(8):
                ar = AR[:, h, Xc, :, :].rearrange("p a t -> p (a t)")
                P.mm(pS1[:, h, :], Bhb[:, h, cs], ar)
                P.mm(pS2[:, h, :], Khb[:, h, cs], ar)
                P.mm(pQ0[:, h, :], AR[:, h, Xc, 0, :], Bhb[:, h, cs])
            P.tt(M1s[Xc][:], pS1, m1[:].unsqueeze(1).broadcast_to([64, 8, 128]), ALU.mult)
            P.tt(M2s[Xc][:], pS2, m1[:].unsqueeze(1).broadcast_to([64, 8, 128]), ALU.mult)
            P.tt(Qs[Xc][:], pQ0, SL[:].unsqueeze(1).broadcast_to([64, 8, 64]), ALU.mult)
            P.tt(XTs[Xc], M1s[Xc][:, :, 0:64], I8, ALU.add)
            P.tt(Xbs[Xc][:], Qs[Xc][:], I8, ALU.add)
        bankP = [v8(B4, 0, 64), v8(T01, 0, 64)]
        bankQ = [v8(B5, 0, 64), v8(T01, 512, 64)]
        bankX1 = [v8(B6, 0, 64), v8(T23, 0, 64)]
        bankX2 = [v8(B7, 0, 64), v8(T23, 512, 64)]
        for kq in range(1, 6):
            last = (kq == 5)
            for Xc in range(2):
                Pm = M1s[Xc][:, :, 0:64]
                for h in range(8):
                    P.mm(bankP[Xc][:, h, :], Qs[Xc][:, h, :], Pm[:, h, :])
                if not last:
                    for h in range(8):
                        P.mm(bankQ[Xc][:, h, :], Pm[:, h, :], Qs[Xc][:, h, :])
            for Xc in range(2):
                Pm = M1s[Xc][:, :, 0:64]
                P.copy(Pm, bankP[Xc], eng='act')
                if not last:
                    P.copy(Qs[Xc][:], bankQ[Xc], eng='act')
            for Xc in range(2):
                Pm = M1s[Xc][:, :, 0:64]
                for h in range(8):
                    P.mm(bankX1[Xc][:, h, :], Xbs[Xc][:, h, :], Pm[:, h, :])
                if not last:
                    for h in range(8):
                        P.mm(bankX2[Xc][:, h, :], Pm[:, h, :], Xbs[Xc][:, h, :])
            for Xc in range(2):
                P.tt(XTs[Xc], XTs[Xc], bankX1[Xc], ALU.add)
                if not last:
                    P.tt(Xbs[Xc][:], Xbs[Xc][:], bankX2[Xc], ALU.add)
        for Xc in range(2):
            P.copy(XTb[Xc][:], XTs[Xc], eng='act')
        for Xc in range(2):
            cs = slice(Xc * 64, (Xc + 1) * 64)
            M1 = M1s[Xc]; M2 = M2s[Xc]
            if Xc == 0:
                pW = v8(B4, 0, 64); pU = v8(B5, 0, 64); pY = v8(B6, 0, 64); pSt = v8(B7, 0, 64)
                gbank = B4; obank = B5
            else:
                pW = v8(T01, 0, 64); pU = v8(T01, 512, 64); pY = v8(T23, 0, 64); pSt = v8(T23, 512, 64)
                gbank = T01[:, 0:512]; obank = T01[:, 512:1024]
            for h in range(8):
                P.mm(pW[:, h, :], M2[:, h, 0:64], Vcb[:, Xc, h, :], start=True, stop=False)
                P.mm(pW[:, h, :], AR[:, h, Xc, 0, :], S0Tb[:, h, :], start=False, stop=True)
            P.copy(Wsb[:], pW, eng='act')
            for h in range(8):
                P.mm(pU[:, h, :], XTb[Xc][:, h, :], Wsb[:, h, :])
            P.copy(Usb[:], pU, eng='act')
            for h in range(8):
                P.mm(pY[:, h, :], AR[:, h, Xc, 1, :], S0Tb[:, h, :], start=True, stop=False)
                P.mm(pY[:, h, :], M2[:, h, 64:128], Vcb[:, Xc, h, :], start=False, stop=False)
                P.mm(pY[:, h, :], M1[:, h, 64:128], Usb[:, h, :], start=False, stop=True)
            for h in range(8):
                P.mm(pSt[:, h, :], KTb[:, Xc, h, :], Vcb[:, Xc, h, :], start=True, stop=False)
                P.mm(pSt[:, h, :], BTb[:, Xc, h, :], Usb[:, h, :], start=False, stop=True)
            P.reduce(st8[:], pY, ALU.add)
            P.ts(st8[:], st8[:], -1.0 / 64, ALU.mult)
            P.tt(Y1, pY, st8[:].unsqueeze(2).broadcast_to([64, 8, 64]), ALU.add)
            P.act(Y2, Y1, AF.Square)
            P.reduce(st8b[:], Y2, ALU.add)
            P.act(st8b[:], st8b[:], AF.Sqrt, bias=64e-5, scale=1.0 / 64)
            P.recip(st8b[:], st8b[:])
            P.tt(Y1, Y1, st8b[:].unsqueeze(2).broadcast_to([64, 8, 64]), ALU.mult)
            Y1f = Y1.rearrange("p h i -> p (h i)")
            P.tt(Y1f, Y1f, lnw[:], ALU.mult)
            P.tt(Y1f, Y1f, lnb[:], ALU.add)
            P.tt(S0T[:], S0T[:], pSt, ALU.add)
            P.tt(S0T[:], S0T[:], gT[:, :, Xc:Xc + 1].broadcast_to([64, 8, 64]), ALU.mult)
            P.copy(S0Tb[:], S0T[:], eng='act')
            for h in range(8):
                P.mm(obank[0:64, h:h + 1], Th[:, h, cs], self.ones[0:64, 0:1])
            P.copy(sbon[:], obank[0:64, 0:8], eng='act')
            P.tt(Y2, Vc[:, Xc, :, :], sbon[:].unsqueeze(2).broadcast_to([64, 8, 64]), ALU.mult)
            P.tt(Y1, Y1, Y2, ALU.add)
            P.mm(gbank[0:64, :], sxg[:, 0, cs], gu1[:], start=True, stop=False)
            P.mm(gbank[0:64, :], sxg[0:32, 1, cs], gu2[:], start=False, stop=True)
            P.tt(obf, Y1f, gbank[0:64, :], ALU.mult)
            for c in range(4):
                P.tr(obank[:, 256 + c * 64:256 + (c + 1) * 64], obf[:, c * 128:(c + 1) * 128], I64)
            st = self.ostage[self.ostage_i % 2]
            P.copy(st[:, :, cs], obank[:, 256:512].rearrange("p (c t) -> p c t", c=4), eng='act')
            if Xc == 1:
                self.ostage_i += 1
                P.dma('sp', oT[tt], st[:], track_dram=False)
    P.end_phase()


K.rwkv = rwkv4


def merge2(self, l, hT, oTs, x_in, x_out):
    P = self.P
    I = self.inp
    P.begin_phase()
    mT = P.sb('mT', [128, 8, S], BF16)
    P.begin_phase()
    oS = [P.sb('oS%d' % i, [128, NT, 4, 128], BF16) for i in range(4)]
    for tq in range(4):
        for i in range(4):
            P.dma('sp', oS[i][:, tq * 4:(tq + 1) * 4, :, :], oTs[i][tq * 4:(tq + 1) * 4].rearrange("t p c s -> p t c s"))
    wb = [P.sb('wb%d' % i, [128, 4, 4, 128], BF16) for i in range(2)]
    wg = [P.sb('wg%d' % i, [128, 8, 4, 128], BF16) for i in range(2)]
    pu = [P.ps('pu%d' % i, [128, 512]) for i in range(2)]
    pg = [P.ps('pg%d' % i, [128, 512]) for i in range(2)]
    sg = [P.sb('sg%d' % i, [128, 512]) for i in range(2)]
    macc = P.sb('macc', [128, 512]); tmpm = P.sb('tmpm', [128, 512])
    it = 0
    for j in range(8):
        b = j % 2
        for i in range(4):
            P.dma('pool', wb[b][:, i, :, :], I['w_branch'][l, i, :, j * 128:(j + 1) * 128].rearrange("(k p) d -> p k d", p=128))
            P.dma('pool', wg[b][:, :, i, :],
                  I['w_in'][l, :, C_GATE + i * 1024 + j * 128:C_GATE + i * 1024 + (j + 1) * 128].rearrange("(k p) d -> p k d", p=128))
        for tb in range(4):
            tsl = slice(tb * 512, (tb + 1) * 512)
            for i in range(4):
                q = it % 2
                it += 1
                for kc in range(4):
                    P.mm(pu[q][:], wb[b][:, i, kc, :], oS[i][:, tb * 4:(tb + 1) * 4, kc, :], start=(kc == 0), stop=(kc == 3))
                for kc in range(8):
                    P.mm(pg[q][:], wg[b][:, kc, i, :], hT[:, kc, tsl], start=(kc == 0), stop=(kc == 7))
                P.act(sg[q][:], pg[q][:], AF.Sigmoid)
                if i == 0:
                    P.tt(macc[:], sg[q][:], pu[q][:], ALU.mult)
                elif i < 3:
                    P.tt(tmpm[:], sg[q][:], pu[q][:], ALU.mult)
                    P.tt(macc[:], macc[:], tmpm[:], ALU.add)
                else:
                    P.tt(tmpm[:], sg[q][:], pu[q][:], ALU.mult)
                    P.tt(mT[:, j, tsl], macc[:], tmpm[:], ALU.add)
    P.end_phase()
    P.begin_phase()
    wo = P.sb('wo', [128, 8, D], BF16)
    self.load_w(wo[:], I['w_out'][l])
    po = [P.ps('po%d' % i, [128, 512]) for i in range(4)]
    xt = [P.sb('xt%d' % i, [128, D]) for i in range(6)]
    for tt in range(min(6, NT)):
        P.dma('sp', xt[tt % 6][:], x_in[tt * 128:(tt + 1) * 128, :])
    for tt in range(NT):
        b = tt % 6
        for dh in range(2):
            pp = po[(tt % 2) * 2 + dh]
            for kc in range(8):
                P.mm(pp[:], mT[:, kc, tt * 128:(tt + 1) * 128], wo[:, kc, dh * 512:(dh + 1) * 512], start=(kc == 0), stop=(kc == 7))
            P.tt(xt[b][:, dh * 512:(dh + 1) * 512], xt[b][:, dh * 512:(dh + 1) * 512], pp[:], ALU.add)
        P.dma('sp', x_out[tt * 128:(tt + 1) * 128, :], xt[b][:], track_dram=False)
        if tt + 6 < NT:
            P.dma('sp', xt[b][:], x_in[(tt + 6) * 128:(tt + 7) * 128, :])
    P.end_phase()
    P.end_phase()


K.merge = merge2


def ffn(self, l, hT, x_in, x_out, moe):
    P = self.P
    I = self.inp
    P.begin_phase()
    yacc = P.sb('yacc', [128, NT, D])
    for tt in range(NT):
        P.dma('sp', yacc[:, tt, :], x_in[tt * 128:(tt + 1) * 128, :])
    if moe:
        E, F = 8, 3584
        logits = P.sb('logits', [128, NT, 8])
        self.norm_T(x_in, I['norm_ffn'][l], hT, router=(I['moe_router'][0], logits), x_sb=yacc)
        W1 = [I['moe_w1'][0, e] for e in range(E)]; W3 = [I['moe_w3'][0, e] for e in range(E)]; W2 = [I['moe_w2'][0, e] for e in range(E)]
        gate = P.sb('gate', [128, NT, 8])
        mk1 = P.sb('mk1', [128, NT, 8]); mk2 = P.sb('mk2', [128, NT, 8]); l2 = P.sb('l2', [128, NT, 8])
        m1 = P.sb('m1', [128, NT]); m2 = P.sb('m2', [128, NT]); g1 = P.sb('g1', [128, NT]); g2 = P.sb('g2', [128, NT])
        bc = lambda a: a.unsqueeze(2).broadcast_to([128, NT, 8])
        P.reduce(m1[:], logits[:], ALU.max)
        P.tt(mk1[:], logits[:], bc(m1[:]), ALU.is_equal)
        P.stt(l2[:], mk1[:], -1e30, logits[:], ALU.mult, ALU.add)
        P.reduce(m2[:], l2[:], ALU.max)
        P.tt(mk2[:], l2[:], bc(m2[:]), ALU.is_equal)
        P.tt(g2[:], m2[:], m1[:], ALU.subtract)
        P.act(g2[:], g2[:], AF.Exp)
        P.ts(g1[:], g2[:], 1.0, ALU.add)
        P.recip(g1[:], g1[:])
        P.tt(g2[:], g2[:], g1[:], ALU.mult)
        P.tt(mk1[:], mk1[:], bc(g1[:]), ALU.mult)
        P.tt(mk2[:], mk2[:], bc(g2[:]), ALU.mult)
        P.tt(gate[:], mk1[:], mk2[:], ALU.add)
    else:
        E, F = 1, 2816
        self.norm_T(x_in, I['norm_ffn'][l], hT, x_sb=yacc)
        W1 = [I['ffn_w1'][0]]; W3 = [I['ffn_w3'][0]]; W2 = [I['ffn_w2'][0]]
    w1g = [P.sb('w1g%d' % i, [128, 8, 512], BF16) for i in range(2)]
    w3g = [P.sb('w3g%d' % i, [128, 8, 512], BF16) for i in range(2)]
    w2g = [P.sb('w2g%d' % i, [128, 4, D], BF16) for i in range(2)]
    actT = P.sb('actT', [128, 4, S], BF16)
    s1 = [P.sb('s1_%d' % i, [128, 512]) for i in range(2)]
    p1 = [P.ps('p1_%d' % i, [128, 512]) for i in range(2)]
    p3 = [P.ps('p3_%d' % i, [128, 512]) for i in range(2)]
    py = [P.ps('py_%d' % i, [128, 512]) for i in range(4)]
    groups = []
    for e in range(E):
        f0 = 0
        while f0 < F:
            fw = min(512, F - f0)
            groups.append((e, f0, fw))
            f0 += fw
    first = True
    it = 0
    iy = 0
    for gi, (e, f0, fw) in enumerate(groups):
        b = gi % 2
        nfc = fw // 128
        P.dma('pool', w1g[b][:, :, 0:fw], W1[e][:, f0:f0 + fw].rearrange("(k p) c -> p k c", p=128))
        P.dma('pool', w3g[b][:, :, 0:fw], W3[e][:, f0:f0 + fw].rearrange("(k p) c -> p k c", p=128))
        P.dma('pool', w2g[b][:, 0:nfc, :], W2[e][f0:f0 + fw, :].rearrange("(k p) c -> p k c", p=128))
        for tb in range(4):
            tsl = slice(tb * 512, (tb + 1) * 512)
            for fc in range(nfc):
                q = it % 2
                it += 1
                for kc in range(8):
                    P.mm(p1[q][:], w1g[b][:, kc, fc * 128:(fc + 1) * 128], hT[:, kc, tsl], start=(kc == 0), stop=(kc == 7))
                for kc in range(8):
                    P.mm(p3[q][:], w3g[b][:, kc, fc * 128:(fc + 1) * 128], hT[:, kc, tsl], start=(kc == 0), stop=(kc == 7))
                P.act(s1[q][:], p1[q][:], AF.Silu)
                P.tt(actT[:, fc, tsl], s1[q][:], p3[q][:], ALU.mult)
        for tt in range(NT):
            for dh in range(2):
                pp = py[iy % 4]
                iy += 1
                for fc in range(nfc):
                    P.mm(pp[:], actT[:, fc, tt * 128:(tt + 1) * 128], w2g[b][:, fc, dh * 512:(dh + 1) * 512],
                         start=(fc == 0), stop=(fc == nfc - 1))
                ya = yacc[:, tt, dh * 512:(dh + 1) * 512]
                if moe:
                    P.stt(ya, pp[:], gate[:, tt, e:e + 1], ya, ALU.mult, ALU.add)
                else:
                    P.tt(ya, ya, pp[:], ALU.add)
    for tt in range(NT):
        P.dma('sp', x_out[tt * 128:(tt + 1) * 128, :], yacc[:, tt, :], track_dram=False)
    P.end_phase()


K.ffn = ffn


def build_all(self, nlayers=2, dbg_stop=None):
    P = self.P
    nc = self.nc
    I = self.inp
    self.consts()
    hT = P.sb('hT', [128, 8, S], BF16, persist=True)
    oTs = [nc.dram_tensor('oT%d' % i, [NT, 128, 4, 128], BF16, kind='Internal').ap() for i in range(4)]
    vfirst = nc.dram_tensor('vfirst', [NT, 64, 8, 128], F32, kind='Internal').ap()
    xm = [nc.dram_tensor('xm%d' % i, [S, D], F32, kind='Internal').ap() for i in range(2)]
    xf = nc.dram_tensor('xf0', [S, D], F32, kind='Internal').ap()
    y = self.dout('y', [S, D])
    xcur = I['x']
    for l in range(nlayers):
        self.norm_T(xcur, I['norm_mix'][l], hT)
        self.rwkv(l, hT, oTs[0], vfirst)
        self.mamba(l, hT, oTs[1])
        self.attention(l, hT, oTs[2])
        self.gla(l, hT, oTs[3])
        xo = y if (dbg_stop == ('m', l)) else xm[l]
        self.merge(l, hT, oTs, xcur, xo)
        if dbg_stop == ('m', l):
            break
        xo2 = y if (l == nlayers - 1 or dbg_stop == ('f', l)) else xf
        self.ffn(l, hT, xm[l], xo2, moe=(l % 2 == 1))
        if dbg_stop == ('f', l):
            break
        xcur = xo2
    P.finish()
    P.emit()


K.build_all = build_all


def host_consts(inputs):
    c = {}
    c['c_ident'] = np.eye(128, dtype=np.float32)
    s = np.arange(128)[:, None]
    l = np.arange(128)[None, :]
    same = (s // 64) == (l // 64)
    c['c_blktri'] = (same & (s <= l)).astype(np.float32)
    c['c_negmask'] = np.where(same & (s <= l), 0.0, -1e30).astype(np.float32)
    c['c_blkones'] = same.astype(np.float32)
    rb = np.asarray(inputs['att_rel_bias'], dtype=np.float32)
    j = np.arange(5)[None, :, None]
    kk = np.arange(128)[:, None, None]
    ll = np.arange(128)[None, None, :]
    rel = np.clip((j - 4) * 128 + kk - ll, -128, 128) + 128
    c['c_attbias'] = np.ascontiguousarray(np.transpose(rb[:, rel], (1, 0, 2, 3)))
    dq = 2 * (4 - j) + ll // 64 - kk // 64
    c['c_attmask'] = ((dq >= 0) & (dq <= 8)).astype(np.float32)
    cw = np.asarray(inputs['ssm_conv_w'], dtype=np.float32)
    L = cw.shape[0]
    c['h_convw'] = np.ascontiguousarray(cw.reshape(L, 4, 8, 128).transpose(0, 3, 2, 1))
    cb = np.asarray(inputs['ssm_conv_b'], dtype=np.float32)
    c['h_convb'] = np.ascontiguousarray(cb.reshape(L, 8, 128).transpose(0, 2, 1))
    mu = np.asarray(inputs['rwkv_mu'], dtype=np.float32)
    m64 = np.zeros((L, 64, 26), np.float32)
    m64[:, :, 0:24] = mu[:, 0:1536].reshape(L, 24, 64).transpose(0, 2, 1)
    m64[:, :, 24] = mu[:, 1536:1600]
    m64[:, :, 25] = mu[:, 1600:1664]
    c['h_mu64'] = m64
    mg = np.zeros((L, 128, 2), np.float32)
    mg[:, :, 0] = mu[:, 1664:1792]
    mg[:, 0:32, 1] = mu[:, 1792:1824]
    c['h_mug'] = mg
    rw = np.zeros((L, 64, 6, 8), np.float32)
    for i, nm in enumerate(['rwkv_w0', 'rwkv_a0', 'rwkv_k_k', 'rwkv_k_a', 'rwkv_r_k']):
        rw[:, :, i, :] = np.asarray(inputs[nm], dtype=np.float32).reshape(L, 8, 64).transpose(0, 2, 1)
    v0 = np.asarray(inputs['vres_v0'], dtype=np.float32)
    rw[1:, :, 5, :] = v0.reshape(L - 1, 8, 64).transpose(0, 2, 1)
    c['h_rw'] = rw
    s64 = np.arange(64)[:, None]; t64 = np.arange(64)[None, :]
    SU = (s64 < t64).astype(np.float32); UI = (s64 <= t64).astype(np.float32)
    c['c_m1'] = np.concatenate([SU, UI], axis=1)
    c['c_sl'] = (t64 < s64).astype(np.float32)
    sm = np.ones((64, 1024), np.float32); sm[:, ::64] = 0.0
    c['c_scanmask'] = sm
    return c


_PARAM_NAMES = None


def _build(inputs, hc):
    nc = bass.Bass("TRN2", target_bir_lowering=False)
    k = K(nc)
    k.din('x', [S, D])
    for n, a in inputs.items():
        if n != 'x':
            k.din(n, list(a.shape))
    for n, a in hc.items():
        k.din(n, list(a.shape))
    k.build_all()
    return nc, k


def kernel(**inputs):
    inputs = {n: np.ascontiguousarray(np.asarray(a, dtype=np.float32)) for n, a in inputs.items()}
    hc = host_consts(inputs)
    nc, k = _build(inputs, hc)
    n_cores = 8
    shared = {n: inputs[n] for n in inputs if n != 'x'}
    shared.update(hc)
    in_maps = []
    for b in range(n_cores):
        m = {n: shared[n] for n in k.inp if n != 'x'}
        m['x'] = np.ascontiguousarray(inputs['x'][b])
        in_maps.append(m)
    res = run_bass_kernel_spmd(nc, in_maps, core_ids=list(range(n_cores)))
    out = np.stack([np.asarray(r['y'], dtype=np.float32) for r in res.results], axis=0)
    return out
```

```python
import contextlib
import numpy as np
import concourse.bass as bass
import concourse.mybir as mybir
from concourse.bass_utils import run_bass_kernel_spmd

F32 = mybir.dt.float32
BF16 = mybir.dt.bfloat16
AF = mybir.ActivationFunctionType
ALU = mybir.AluOpType
AX = mybir.AxisListType

ENGS = ['pe', 'act', 'dve', 'pool', 'sp']


def _prod(xs):
    r = 1
    for v in xs:
        r *= int(v)
    return r


class Prog:
    def __init__(self, nc, n_dma_sems=48, same_engine_sync=True, relax=True):
        self.nc = nc
        self.same_engine_sync = same_engine_sync
        self.relax_same_engine = relax
        self.relax_min = 512
        self.root = contextlib.ExitStack()
        self.stream = {e: [] for e in ENGS}
        self.cnt = {e: 0 for e in ENGS}
        self.known = {e: {} for e in ENGS}
        self.acc = {}
        self.esem = {e: self.root.enter_context(nc.semaphore('s_' + e)) for e in ENGS if e != 'sp'}
        self.dsem = [self.root.enter_context(nc.semaphore('d_%d' % i)) for i in range(n_dma_sems)]
        self.dval = [0] * n_dma_sems
        self.drr = {'sp': 0, 'pool': 0, 'act': 0}
        self.phases = []
        self.uid = 0
        self.ninst = 0

    def begin_phase(self):
        self.phases.append(contextlib.ExitStack())

    def end_phase(self):
        self.barrier()
        self.phases.pop().close()
        self.acc = {k: v for k, v in self.acc.items() if k.startswith('D:')}

    def _stk(self, persist):
        return self.root if (persist or not self.phases) else self.phases[-1]

    def sb(self, name, shape, dtype=F32, persist=False):
        self.uid += 1
        return self._stk(persist).enter_context(self.nc.sbuf_tensor('%s_%d' % (name, self.uid), list(shape), dtype))

    def ps(self, name, shape, dtype=F32, persist=False):
        self.uid += 1
        es = 2 if dtype == BF16 else 4
        n = _prod(shape[1:])
        nb = (n * es + 2047) // 2048
        t = self._stk(persist).enter_context(
            self.nc.psum_tensor('%s_%d' % (name, self.uid), [128, nb * 2048 // es], dtype))
        v = t[0:shape[0], 0:n]
        if len(shape) == 3:
            v = v.rearrange("p (a b) -> p a b", a=shape[1])
        elif len(shape) == 4:
            v = v.rearrange("p (a b c) -> p a b c", a=shape[1], b=shape[2])
        return v

    def dram(self, name, shape, dtype=F32, kind='Internal'):
        return self.nc.dram_tensor(name, list(shape), dtype, kind=kind)

    @staticmethod
    def _box(ap):
        t = ap.tensor
        nm = t.name
        tn = type(t).__name__
        if tn.startswith('DRam'):
            return [('D:' + nm, 0, 1, 0, 1)]
        if tn.startswith('PSum'):
            es = 2 if ap.dtype == BF16 else 4
            shape = list(t.shape)
            row = _prod(shape[1:])
            off = int(ap.offset)
            pairs = [(int(s), int(c)) for s, c in ap.ap]
            f0 = off % row
            ext = sum(abs(s) * (c - 1) for s, c in pairs[1:])
            b0 = (f0 * es) // 2048
            b1 = ((f0 + ext) * es) // 2048
            return [('PS:%s:%d' % (nm, b), 0, 128, 0, 1) for b in range(b0, b1 + 1)]
        shape = list(t.shape)
        row = _prod(shape[1:])
        off = int(ap.offset)
        pairs = [(int(s), int(c)) for s, c in ap.ap]
        p0 = off // row
        f0 = off % row
        ps_, pc = pairs[0]
        if ps_ == 0:
            pc = 1
        p1 = p0 + pc
        ext = sum(abs(s) * (c - 1) for s, c in pairs[1:])
        sig = (off, tuple(pairs))
        n = _prod([c for _, c in pairs[1:]])
        return [(nm, p0, p1, f0, f0 + ext + 1, sig, n)]

    @staticmethod
    def _ov(a, b):
        return a[1] < b[2] and b[1] < a[2] and a[3] < b[4] and b[3] < a[4]

    @staticmethod
    def _inside(a, b):
        return a[1] >= b[1] and a[2] <= b[2] and a[3] >= b[3] and a[4] <= b[4]

    def _deps(self, rb, wb, E=None):
        deps = set()
        relax = self.relax_same_engine and E in ('act', 'dve', 'pool')
        for b in rb:
            for (rbx, kind, tok) in self.acc.get(b[0], ()):
                if kind == 'w' and self._ov(b, rbx):
                    if (relax and tok[0] == 'e' and tok[1] == E and len(b) > 5 and len(rbx) > 5
                            and b[5] == rbx[5] and b[6] >= self.relax_min):
                        continue
                    deps.add(tok)
        for b in wb:
            sbuf = len(b) > 5
            for (rbx, kind, tok) in self.acc.get(b[0], ()):
                if self._ov(b, rbx):
                    if relax and sbuf and tok[0] == 'e' and tok[1] == E:
                        continue
                    deps.add(tok)
        return deps

    def _record(self, rb, wb, tok):
        for b in wb:
            lst = self.acc.setdefault(b[0], [])
            lst[:] = [r for r in lst if not self._inside(r[0], b)]
            lst.append((b, 'w', tok))
        for b in rb:
            lst = self.acc.setdefault(b[0], [])
            if tok[0] == 'e':
                lst[:] = [r for r in lst if not (r[1] == 'r' and r[2][0] == 'e' and r[2][1] == tok[1]
                                                 and self._inside(r[0], b))]
            lst.append((b, 'r', tok))

    def _emit_waits(self, E, deps):
        need = {}
        for tok in deps:
            if tok[0] == 'e':
                _, f, idx = tok
                if f == E and (E == 'pe' or not self.same_engine_sync):
                    continue
                key = ('e', f)
            else:
                _, si, idx = tok
                key = ('d', si)
            if idx > need.get(key, 0):
                need[key] = idx
        for key, idx in need.items():
            if self.known[E].get(key, 0) >= idx:
                continue
            self.known[E][key] = idx
            sem = self.esem[key[1]] if key[0] == 'e' else self.dsem[key[1]]
            self.stream[E].append(('w', sem, idx))

    def op(self, E, fn, reads=(), writes=()):
        rb = [b for a in reads for b in self._box(a)]
        wb = [b for a in writes for b in self._box(a)]
        wb += [b for b in rb if b[0].startswith('PS:')]
        deps = self._deps(rb, wb, E)
        self._emit_waits(E, deps)
        self.cnt[E] += 1
        tok = ('e', E, self.cnt[E])
        self.stream[E].append(('i', fn, self.esem[E], 1))
        self._record(rb, wb, tok)
        self.ninst += 1

    def dma(self, Q, out, in_, track_dram=True, **kw):
        rb = self._box(in_)
        wb = self._box(out)
        if not track_dram:
            rb = [b for b in rb if not b[0].startswith('D:')]
            wb = [b for b in wb if not b[0].startswith('D:')]
        deps = self._deps(rb, wb)
        self._emit_waits(Q, deps)
        half = len(self.dsem) // 2
        base = half if Q == 'pool' else 0
        si = base + self.drr[Q] % half
        self.drr[Q] += 1
        if self.dval[si] > 0 and self.known[Q].get(('d', si), 0) < self.dval[si]:
            self.known[Q][('d', si)] = self.dval[si]
            self.stream[Q].append(('w', self.dsem[si], self.dval[si]))
        self.dval[si] += 16
        tok = ('d', si, self.dval[si])
        self.stream[Q].append(('i', (lambda eng, o=out, i=in_, k=kw: eng.dma_start(out=o, in_=i, **k)),
                               self.dsem[si], 16))
        self._record(rb, wb, tok)
        self.ninst += 1

    def barrier(self):
        for E in ENGS:
            deps = set()
            for f in ENGS:
                if f != 'sp' and self.cnt[f] > 0:
                    deps.add(('e', f, self.cnt[f]))
            for si, v in enumerate(self.dval):
                if v > 0:
                    deps.add(('d', si, v))
            self._emit_waits(E, deps)

    def finish(self):
        self.barrier()

    def emit(self):
        nc = self.nc
        streams = self.stream

        def run(eng, items):
            for it in items:
                if it[0] == 'w':
                    eng.wait_ge(it[1], it[2])
                else:
                    try:
                        ins = it[1](eng)
                    except Exception:
                        d = it[1].__defaults__
                        print('FAILED INSTR defaults:', [(getattr(x, 'tensor', None) and x.tensor.name, getattr(x, 'shape', None)) for x in (d or [])])
                        raise
                    ins.then_inc(it[2], it[3])

        with nc.Block() as block:
            @block.tensor
            def _(eng):
                run(eng, streams['pe'])

            @block.scalar
            def _(eng):
                run(eng, streams['act'])

            @block.vector
            def _(eng):
                run(eng, streams['dve'])

            @block.gpsimd
            def _(eng):
                run(eng, streams['pool'])

            @block.sync
            def _(eng):
                run(eng, streams['sp'])
        self.root.close()

    def mm(self, out, lhsT, rhs, start=True, stop=True):
        self.op('pe', lambda e: e.matmul(out, lhsT, rhs, start=start, stop=stop),
                reads=[lhsT, rhs], writes=[out])

    def tr(self, out, in_, ident):
        self.op('pe', lambda e: e.transpose(out, in_, ident), reads=[in_, ident], writes=[out])

    def act(self, out, in_, func, bias=None, scale=None, accum_out=None):
        kw = {}
        reads = [in_]
        writes = [out]
        if bias is not None:
            kw['bias'] = bias
            if not isinstance(bias, (int, float)):
                reads.append(bias)
        if scale is not None:
            kw['scale'] = scale
            if not isinstance(scale, (int, float)):
                reads.append(scale)
        if accum_out is not None:
            kw['accum_out'] = accum_out
            writes.append(accum_out)
        self.op('act', lambda e: e.activation(out, in_, func, **kw), reads=reads, writes=writes)

    def tt(self, out, in0, in1, op, eng='dve'):
        self.op(eng, lambda e: e.tensor_tensor(out, in0, in1, op), reads=[in0, in1], writes=[out])

    def ts(self, out, in0, s1, op0, s2=None, op1=None, eng='dve', accum_out=None):
        reads = [in0]
        writes = [out]
        if not isinstance(s1, (int, float)):
            reads.append(s1)
        if s2 is not None and not isinstance(s2, (int, float)):
            reads.append(s2)
        kw = {}
        if accum_out is not None:
            kw['accum_out'] = accum_out
            writes.append(accum_out)
        o1 = op1 if op1 is not None else ALU.bypass
        self.op(eng, lambda e: e.tensor_scalar(out, in0, s1, s2, op0, o1, **kw), reads=reads, writes=writes)

    def stt(self, out, in0, scalar, in1, op0, op1, eng='dve'):
        reads = [in0, in1]
        if not isinstance(scalar, (int, float)):
            reads.append(scalar)
        self.op(eng, lambda e: e.scalar_tensor_tensor(out, in0, scalar, in1, op0, op1), reads=reads, writes=[out])

    def copy(self, out, in_, eng='dve'):
        if eng == 'act':
            self.op('act', lambda e: e.copy(out, in_), reads=[in_], writes=[out])
        else:
            self.op(eng, lambda e: e.tensor_copy(out, in_), reads=[in_], writes=[out])

    def memset(self, ap, val, eng='dve'):
        self.op(eng, lambda e: e.memset(ap, val), reads=[], writes=[ap])

    def recip(self, out, in_):
        self.op('dve', lambda e: e.reciprocal(out, in_), reads=[in_], writes=[out])

    def reduce(self, out, in_, op, axis=None):
        ax = axis if axis is not None else AX.X
        self.op('dve', lambda e: e.tensor_reduce(out, in_, ax, op), reads=[in_], writes=[out])

    def scan(self, out, d0, d1, init, op0, op1):
        reads = [d0, d1]
        if not isinstance(init, (int, float)):
            reads.append(init)
        self.op('dve', lambda e: e.tensor_tensor_scan(out, d0, d1, init, op0, op1), reads=reads, writes=[out])


D = 1024
S = 2048
NT = 16
NCH = 32
IN_COLS = 10552
C_RWKV, C_SSM, C_ATT, C_GLA, C_GATE = 0, 1824, 3368, 4904, 6456
EPS = 1e-6


class K:
    def __init__(self, nc, dbg=None, stop_after=None):
        self.nc = nc
        self.P = Prog(nc)
        self.dbg = dbg or []
        self.stop_after = stop_after
        self.inp = {}
        self.out = {}

    def din(self, name, shape, dtype=F32):
        if name in self.inp:
            return self.inp[name]
        t = self.nc.dram_tensor(name, list(shape), dtype, kind="ExternalInput").ap()
        self.inp[name] = t
        return t

    def dout(self, name, shape):
        t = self.nc.dram_tensor(name, list(shape), F32, kind="ExternalOutput").ap()
        self.out[name] = t
        return t

    def dscr(self, name, shape, dtype=F32):
        return self.nc.dram_tensor(name, list(shape), dtype, kind="Internal").ap()

    def consts(self):
        P = self.P
        self.ident = P.sb('ident', [128, 128], F32, persist=True)
        self.identb = P.sb('identb', [128, 128], BF16, persist=True)
        self.blktri = P.sb('blktri', [128, 128], F32, persist=True)
        self.blktrib = P.sb('blktrib', [128, 128], BF16, persist=True)
        self.blkones = P.sb('blkones', [128, 128], F32, persist=True)
        self.ones = P.sb('ones', [128, 128], F32, persist=True)
        c_ident = self.din('c_ident', [128, 128])
        c_blktri = self.din('c_blktri', [128, 128])
        c_blkones = self.din('c_blkones', [128, 128])
        P.dma('sp', self.ident[:], c_ident)
        P.dma('pool', self.identb[:], c_ident)
        P.dma('sp', self.blktri[:], c_blktri)
        P.dma('pool', self.blktrib[:], c_blktri)
        P.dma('sp', self.blkones[:], c_blkones)
        P.memset(self.ones[:], 1.0)
        self.ostage = [P.sb('ostage%d' % i, [128, 4, 128], BF16, persist=True) for i in range(2)]
        self.ostage_i = 0

    def load_w(self, dst, src, q='pool'):
        self.P.dma(q, dst, src.rearrange("(k p) c -> p k c", p=128))

    def norm_T(self, x_dram, g_row, hT, router=None, x_sb=None):
        P = self.P
        P.begin_phase()
        g_bc = P.sb('g_bc', [128, D])
        P.dma('sp', g_bc[:], g_row.partition_broadcast(128))
        if x_sb is None:
            x_sb = P.sb('xall', [128, NT, D])
            for tt in range(NT):
                P.dma('sp', x_sb[:, tt, :], x_dram[tt * 128:(tt + 1) * 128, :])
        junk = P.sb('junk', [128, D])
        hb = [P.sb('hb%d' % i, [128, D], BF16) for i in range(2)]
        ss = P.sb('ss', [128, NT])
        rstd = P.sb('rstd', [128, NT])
        ptb = [P.ps('ptb%d' % i, [128, 8, 128], BF16) for i in range(2)]
        if router is not None:
            wr_d, logits = router
            wr = P.sb('wr', [128, 8, 8])
            P.dma('sp', wr[:], wr_d.rearrange("(k p) c -> p k c", p=128))
            h32 = [P.sb('h32_%d' % i, [128, D]) for i in range(2)]
            h32T = [P.sb('h32T_%d' % i, [128, 8, 128]) for i in range(2)]
            pt32 = [P.ps('pt32_%d' % i, [128, 4, 128]) for i in range(2)]
            plog = P.ps('plog', [128, 8])
        for tt in range(NT):
            P.act(junk[:], x_sb[:, tt, :], AF.Square, accum_out=ss[:, tt:tt + 1])
        P.act(rstd[:], ss[:], AF.Sqrt, bias=EPS, scale=1.0 / D)
        P.recip(rstd[:], rstd[:])
        for tt in range(NT):
            b = tt % 2
            xcur = x_sb[:, tt, :]
            P.stt(hb[b][:], xcur, rstd[:, tt:tt + 1], g_bc[:], ALU.mult, ALU.mult)
            for kc in range(8):
                P.tr(ptb[b][:, kc, :], hb[b][:, kc * 128:(kc + 1) * 128], self.identb[:])
            if tt % 2 == 0:
                P.copy(hT[:, :, tt * 128:(tt + 1) * 128], ptb[b][:], eng='act')
            else:
                P.copy(hT[:, :, tt * 128:(tt + 1) * 128], ptb[b][:], eng='dve')
            if router is not None:
                P.stt(h32[b][:], xcur, rstd[:, tt:tt + 1], g_bc[:], ALU.mult, ALU.mult)
                for half in range(2):
                    for k4 in range(4):
                        kc = half * 4 + k4
                        P.tr(pt32[half][:, k4, :], h32[b][:, kc * 128:(kc + 1) * 128], self.ident[:])
                    P.copy(h32T[b][:, half * 4:(half + 1) * 4, :], pt32[half][:], eng='act' if half == 0 else 'dve')
                for kc in range(8):
                    P.mm(plog[:], h32T[b][:, kc, :], wr[:, kc, :], start=(kc == 0), stop=(kc == 7))
                P.copy(logits[:, tt, :], plog[:])
        P.end_phase()

    def to_T(self, o_bf, oT, tt, ptr, eng='act'):
        P = self.P
        for c in range(4):
            P.tr(ptr[:, c, :], o_bf[:, c * 128:(c + 1) * 128], self.identb[:])
        st = self.ostage[self.ostage_i % 2]
        self.ostage_i += 1
        P.copy(st[:], ptr, eng=eng)
        P.dma('sp', oT[tt], st[:], track_dram=False)

    def to_T_sb(self, o_bf, oT, tt, ptr, eng='act'):
        P = self.P
        for c in range(4):
            P.tr(ptr[:, c, :], o_bf[:, c * 128:(c + 1) * 128], self.identb[:])
        P.copy(oT[:, :, tt * 128:(tt + 1) * 128], ptr[:], eng=eng)

    def proj_tm(self, pout, hT, w, tt, ncols, c0=0):
        for kc in range(8):
            self.P.mm(pout, hT[:, kc, tt * 128:(tt + 1) * 128], w[:, kc, c0:c0 + ncols], start=(kc == 0), stop=(kc == 7))

    def proj_fm(self, pout, hT, w, tb, c0, m, ntok=512):
        for kc in range(8):
            self.P.mm(pout, w[:, kc, c0:c0 + m], hT[:, kc, tb * ntok:(tb + 1) * ntok], start=(kc == 0), stop=(kc == 7))

    def attention(self, l, hT, oT):
        P = self.P
        w_in = self.inp['w_in']
        P.begin_phase()
        qT = P.sb('qT', [128, 4, S], BF16)
        kT = P.sb('kT', [128, 4, S], BF16)
        vaug = P.sb('vaug', [128, NT, 8, 65], BF16)
        EBm = P.sb('EBm', [128, 8, 5, 128], BF16)
        P.begin_phase()
        EB = P.sb('EB', [128, 8, 5, 128])
        amask = P.sb('amask', [128, 5, 128])
        P.dma('sp', EB[:], self.inp['c_attbias'])
        P.dma('sp', amask[:], self.inp['c_attmask'])
        P.ts(amask[:], amask[:], 30000.0, ALU.mult, -30000.0, ALU.add)
        P.stt(EBm[:], EB[:], 8.0, amask[:].unsqueeze(1).broadcast_to([128, 8, 5, 128]), ALU.mult, ALU.add)
        P.end_phase()
        P.memset(vaug[:, :, :, 64:65], 1.0)
        P.begin_phase()
        w = P.sb('w_att', [128, 8, 1536], BF16)
        self.load_w(w[:], w_in[l, :, C_ATT:C_ATT + 1536])
        gq = P.sb('gq', [128, 64])
        gk = P.sb('gk', [128, 64])
        P.dma('sp', gq[:], self.inp['att_q_gain'][l].partition_broadcast(128))
        P.dma('sp', gk[:], self.inp['att_k_gain'][l].partition_broadcast(128))
        pq = [P.ps('pq%d' % i, [128, 512]) for i in range(3)]
        ptr = [P.ps('ptr%d' % i, [128, 4, 128], BF16) for i in range(2)]
        sq = [P.sb('sq%d' % i, [128, 8, 64]) for i in range(2)]
        ssq = [P.sb('ssq%d' % i, [128, 8]) for i in range(2)]
        tmp = [P.sb('tmp%d' % i, [128, 8, 64]) for i in range(2)]
        nb = [P.sb('nb%d' % i, [128, 512], BF16) for i in range(2)]
        qk = [(gq, qT), (gk, kT)]
        for tt in range(NT):
            for i in range(3):
                self.proj_tm(pq[i][:], hT, w, tt, 512, c0=i * 512)
            pv3 = [pq[i][:].rearrange("p (h d) -> p h d", h=8) for i in range(2)]
            for i in range(2):
                P.act(sq[i][:], pv3[i], AF.Square)
            for i in range(2):
                P.reduce(ssq[i][:], sq[i][:], ALU.add)
            for i in range(2):
                P.act(ssq[i][:], ssq[i][:], AF.Sqrt, bias=EPS, scale=1.0 / 64)
            for i in range(2):
                P.recip(ssq[i][:], ssq[i][:])
            for i in range(2):
                P.tt(tmp[i][:], pv3[i], ssq[i][:].unsqueeze(2).broadcast_to([128, 8, 64]), ALU.mult)
            for i in range(2):
                P.tt(nb[i][:].rearrange("p (h d) -> p h d", h=8), tmp[i][:], qk[i][0][:].unsqueeze(1).broadcast_to([128, 8, 64]), ALU.mult)
            for i in range(2):
                self.to_T_sb(nb[i], qk[i][1], tt, ptr[i], eng='act')
            P.copy(vaug[:, tt, :, 0:64], pq[2][:].rearrange("p (h d) -> p h d", h=8), eng='act')
        P.end_phase()
        P.begin_phase()
        pS0 = [P.ps('pS0_%d' % i, [128, 4, 128]) for i in range(2)]
        pS1 = [P.ps('pS1_%d' % i, [128, 128]) for i in range(2)]
        po = [P.ps('po%d' % i, [128, 4, 65]) for i in range(2)]
        ptr = P.ps('ptr', [128, 4, 128], BF16)
        Pm = [P.sb('Pm%d' % i, [128, 5, 128], BF16) for i in range(2)]
        rec = P.sb('rec', [128, 4, 1])
        obf = [P.sb('obf%d' % i, [128, 512], BF16) for i in range(2)]
        iters = [(qt, h) for qt in range(NT) for h in range(8)]

        def emit_S(it):
            qt, h = iters[it]
            hp, pb = h // 2, (h % 2) * 64
            b = it % 2
            for j in range(5):
                kt = qt - 4 + j
                if kt < 0:
                    continue
                dst = pS0[b][:, j, :] if j < 4 else pS1[b][:]
                P.mm(dst, kT[pb:pb + 64, hp, kt * 128:(kt + 1) * 128], qT[pb:pb + 64, hp, qt * 128:(qt + 1) * 128],
                     start=True, stop=False)
                P.mm(dst, self.identb[:], EBm[:, h, j, :], start=False, stop=True)

        emit_S(0)
        for it, (qt, h) in enumerate(iters):
            ob = obf[qt % 2]
            b = it % 2
            js = [j for j in range(5) if qt - 4 + j >= 0]
            j0 = js[0]
            if it + 1 < len(iters):
                emit_S(it + 1)
            if j0 < 4:
                P.act(Pm[b][:, j0:4, :], pS0[b][:, j0:4, :], AF.Exp, scale=0.125)
            P.act(Pm[b][:, 4, :], pS1[b][:], AF.Exp, scale=0.125)
            pob = po[(h // 4) % 2]
            for j in js:
                kt = qt - 4 + j
                P.mm(pob[:, h % 4, :], Pm[b][:, j, :], vaug[:, kt, h, :], start=(j == j0), stop=(j == 4))
            if h % 4 == 3:
                P.recip(rec[:], pob[:, :, 64:65])
                h0 = h - 3
                P.tt(ob[:, h0 * 64:(h0 + 4) * 64].rearrange("p (h d) -> p h d", h=4), pob[:, :, 0:64],
                     rec[:].broadcast_to([128, 4, 64]), ALU.mult)
            if h == 7:
                self.to_T(ob, oT, qt, ptr, eng='act')
        P.end_phase()
        P.end_phase()


def gla(self, l, hT, oT):
    P = self.P
    w_in = self.inp['w_in']
    P.begin_phase()
    w = P.sb('w_gla', [128, 8, 1552], BF16)
    self.load_w(w[:], w_in[l, :, C_GLA:C_GLA + 1552])
    gup = P.sb('gup', [16, 256], BF16)
    P.dma('pool', gup[:], self.inp['gla_gate_up'][l])
    gbias = P.sb('gbias', [1, 256], BF16)
    P.dma('pool', gbias[:], self.inp['gla_gate_bias'][l:l + 1, :])
    onesb = P.sb('onesb', [1, 128], BF16)
    P.memset(onesb[:], 1.0)
    nw = P.sb('nw', [128, 128])
    P.dma('sp', nw[:], self.inp['gla_norm_w'][l].partition_broadcast(128))
    B0 = P.ps('B0', [128, 512]); B1 = P.ps('B1', [128, 512]); B2 = P.ps('B2', [128, 512]); B3 = P.ps('B3', [128, 512])
    B4 = P.ps('B4', [128, 512]); B5 = P.ps('B5', [128, 512]); B6 = P.ps('B6', [128, 512])
    B7 = P.ps('B7', [128, 1024], BF16)
    pla = B0[:, 0:256]
    pxg = B0[0:16, 256:384]
    p64 = B1[0:64, :].rearrange("p (h t) -> p h t", h=4)
    pv = B2[:]
    pa = B3[:].rearrange("p (h t) -> p h t", h=4)
    pcA = B4[0:64, :].rearrange("p (h t) -> p h t", h=4)
    pcB = B5[0:64, :].rearrange("p (h t) -> p h t", h=4)
    po = B6[:].rearrange("p (h t) -> p h t", h=4)
    ptk = B7[:, 0:256].rearrange("p (h t) -> p h t", h=4)
    ptr = B7[:, 512:1024].rearrange("p (h t) -> p h t", h=4)
    xg = P.sb('xg', [16, 128], BF16)
    ee = P.sb('ee', [128, 256])
    la = P.sb('la', [128, 256])
    E1 = P.sb('E1', [64, 4, 128]); E2 = P.sb('E2', [64, 4, 128])
    ebl = P.sb('ebl', [64, 4, 2])
    qgA = P.sb('qgA', [64, 4, 128], BF16); qgB = P.sb('qgB', [64, 4, 128], BF16)
    qg = P.sb('qg', [64, 4, 128], BF16)
    kg = P.sb('kg', [64, 4, 128], BF16); kd = P.sb('kd', [64, 4, 128], BF16)
    kdt = P.sb('kdt', [128, 4, 64], BF16)
    vt = P.sb('vt', [128, 512], BF16)
    sg = P.sb('sg', [128, 512])
    att = P.sb('att', [128, 4, 128], BF16)
    Sm = P.sb('Sm', [64, 4, 128])
    SAb = P.sb('SAb', [64, 4, 128], BF16); SBb = P.sb('SBb', [64, 4, 128], BF16)
    sq = P.sb('sq', [128, 4, 128]); ssq = P.sb('ssq', [128, 4])
    o1 = P.sb('o1', [128, 4, 128]); obf = P.sb('obf', [128, 512], BF16)
    P.memset(qgA[:], 0.0); P.memset(qgB[:], 0.0); P.memset(Sm[:], 0.0)
    for tt in range(NT):
        ts_ = slice(tt * 128, (tt + 1) * 128)
        for kc in range(8):
            P.mm(pxg, w[:, kc, 1024:1040], hT[:, kc, ts_], start=(kc == 0), stop=(kc == 7))
        P.copy(xg[:], pxg, eng='act')
        P.mm(pla, xg[:], gup[:], start=True, stop=False)
        P.mm(pla, onesb[:], gbias[:], start=False, stop=True)
        P.act(ee[:], pla, AF.Exp, scale=-1.0)
        P.act(ee[:], ee[:], AF.Ln, bias=1.0)
        P.ts(la[:], ee[:], -1.0 / 16.0, ALU.mult)
        for h in range(4):
            P.mm(p64[:, h, :], la[:, h * 64:(h + 1) * 64], self.blktri[:])
        P.act(E1[:], p64, AF.Exp)
        P.act(E2[:], p64, AF.Exp, scale=-1.0)
        P.act(ebl[:], B1[0:64, :].rearrange("p (h c t) -> p h c t", h=4, c=2)[:, :, :, 63], AF.Exp)
        for h in range(4):
            for kc in range(8):
                P.mm(p64[:, h, :], w[:, kc, h * 64:(h + 1) * 64], hT[:, kc, ts_], start=(kc == 0), stop=(kc == 7))
        P.stt(qg[:], p64, 0.125, E1[:], ALU.mult, ALU.mult)
        P.copy(qgA[:, :, 0:64], qg[:, :, 0:64], eng='act')
        P.copy(qgB[:, :, 64:128], qg[:, :, 64:128], eng='act')
        for h in range(4):
            for kc in range(8):
                P.mm(p64[:, h, :], w[:, kc, 256 + h * 64:256 + (h + 1) * 64], hT[:, kc, ts_], start=(kc == 0), stop=(kc == 7))
        P.tt(kg[:], p64, E2[:], ALU.mult)
        P.tt(kd[:].rearrange("p h (c t) -> p h c t", c=2), kg[:].rearrange("p h (c t) -> p h c t", c=2),
             ebl[:].unsqueeze(3).broadcast_to([64, 4, 2, 64]), ALU.mult)
        for h in range(4):
            P.tr(ptk[:, h, :], kd[:, h, :], self.identb[0:64, 0:64])
        P.copy(kdt[:], ptk, eng='act')
        self.proj_tm(pv, hT, w, tt, 512, c0=512)
        P.copy(vt[:], pv, eng='act')
        self.proj_tm(pv, hT, w, tt, 512, c0=1040)
        P.act(sg[:], pv, AF.Silu)
        for h in range(4):
            P.mm(pa[:, h, :], kg[:, h, :], qg[:, h, :])
        P.tt(att[:], pa, self.blktri[:].unsqueeze(1).broadcast_to([128, 4, 128]), ALU.mult)
        for h in range(4):
            P.mm(pcA[:, h, :], kdt[0:64, h, :], vt[0:64, h * 128:(h + 1) * 128])
            P.mm(pcB[:, h, :], kdt[64:128, h, :], vt[64:128, h * 128:(h + 1) * 128])
        P.copy(SAb[:], Sm[:], eng='act')
        P.tt(Sm[:], Sm[:], ebl[:, :, 0:1].broadcast_to([64, 4, 128]), ALU.mult)
        P.tt(Sm[:], Sm[:], pcA, ALU.add)
        P.copy(SBb[:], Sm[:], eng='act')
        for h in range(4):
            P.mm(po[:, h, :], att[:, h, :], vt[:, h * 128:(h + 1) * 128], start=True, stop=False)
            P.mm(po[:, h, :], qgA[:, h, :], SAb[:, h, :], start=False, stop=False)
            P.mm(po[:, h, :], qgB[:, h, :], SBb[:, h, :], start=False, stop=True)
        P.tt(Sm[:], Sm[:], ebl[:, :, 1:2].broadcast_to([64, 4, 128]), ALU.mult)
        P.tt(Sm[:], Sm[:], pcB, ALU.add)
        P.act(sq[:], po, AF.Square)
        P.reduce(ssq[:], sq[:], ALU.add)
        P.act(ssq[:], ssq[:], AF.Sqrt, bias=EPS, scale=1.0 / 128)
        P.recip(ssq[:], ssq[:])
        P.tt(o1[:], po, ssq[:].unsqueeze(2).broadcast_to([128, 4, 128]), ALU.mult)
        P.tt(o1[:], o1[:], nw[:].unsqueeze(1).broadcast_to([128, 4, 128]), ALU.mult)
        P.tt(obf[:].rearrange("p (h t) -> p h t", h=4), o1[:], sg[:].rearrange("p (h t) -> p h t", h=4), ALU.mult)
        self.to_T(obf, oT, tt, ptr, eng='act')
    P.end_phase()


K.gla = gla


def mamba(self, l, hT, oT):
    P = self.P
    w_in = self.inp['w_in']
    P.begin_phase()
    w = P.sb('w_ssm', [128, 8, 1544], BF16)
    self.load_w(w[:], w_in[l, :, C_SSM:C_SSM + 1544])
    cw = P.sb('cw', [128, 8, 4])
    P.dma('sp', cw[:], self.inp['h_convw'][l])
    cbias = P.sb('cbias', [128, 8])
    P.dma('sp', cbias[:], self.inp['h_convb'][l])
    dtb = P.sb('dtb', [128, 8]); abc = P.sb('abc', [128, 8]); dsk = P.sb('dsk', [128, 8])
    P.dma('sp', dtb[:], self.inp['ssm_dt_bias'][l].partition_broadcast(128))
    P.dma('sp', abc[:], self.inp['ssm_a_log'][l].partition_broadcast(128))
    P.dma('sp', dsk[:], self.inp['ssm_d'][l].partition_broadcast(128))
    P.act(abc[:], abc[:], AF.Exp)
    P.ts(abc[:], abc[:], -1.0, ALU.mult)
    nw = P.sb('nw', [128, 512])
    P.dma('sp', nw[:], self.inp['ssm_norm_w'][l].partition_broadcast(128))
    negm = P.sb('negm', [128, 128])
    P.dma('sp', negm[:], self.inp['c_negmask'])
    BR = P.ps('BR', [128, 8, 128])
    Bs = P.ps('Bs', [128, 512])
    BstA = P.ps('BstA', [128, 8, 64]); BstB = P.ps('BstB', [128, 8, 64])
    By = P.ps('By', [128, 8, 64])
    Bg = P.ps('Bg', [128, 512])
    Bt = P.ps('Bt', [128, 1024], BF16)
    pdt = Bs[:, 0:8]; pacs = Bs[:, 8:16]; ptot = Bs[:, 16:24]
    pcb = Bs[:, 256:512].rearrange("p (g t) -> p g t", g=2)
    ptx = Bt[:, 0:512].rearrange("p (c t) -> p c t", c=4)
    ptb_ = Bt[:, 0:256].rearrange("p (c t) -> p c t", c=2)
    ptr = Bt[:, 0:512].rearrange("p (c t) -> p c t", c=4)
    raw = P.sb('raw', [128, 8, 131])
    acc = P.sb('acc', [128, 8, 128]); tmp = P.sb('tmp', [128, 8, 128])
    xsb = P.sb('xsb', [128, 4, 128], BF16); Bb = P.sb('Bb', [128, 2, 128], BF16); Cb = P.sb('Cb', [128, 2, 128], BF16)
    xs_tm = P.sb('xs_tm', [128, 8, 64], BF16)
    bm_tm = P.sb('bm_tm', [128, 2, 128], BF16)
    dt = P.sb('dt', [128, 8]); dA = P.sb('dA', [128, 8]); acs = P.sb('acs', [128, 8]); dte = P.sb('dte', [128, 8])
    xdt = P.sb('xdt', [128, 8, 64], BF16); xdtd = P.sb('xdtd', [128, 8, 64], BF16)
    Dg = P.sb('Dg', [128, 8, 128]); seg = P.sb('seg', [128, 8, 128]); Lm = P.sb('Lm', [128, 8, 128])
    eacs = P.sb('eacs', [128, 8, 128])
    MT = P.sb('MT', [128, 8, 128], BF16)
    cmhA = P.sb('cmhA', [128, 8, 128], BF16); cmhB = P.sb('cmhB', [128, 8, 128], BF16)
    Em = P.sb('Em', [128, 8, 64]); EAb = P.sb('EAb', [128, 8, 64], BF16); EBb = P.sb('EBb', [128, 8, 64], BF16)
    y2 = P.sb('y2', [128, 8, 64]); sgt = P.sb('sgt', [128, 512]); sq = P.sb('sq', [128, 2, 256]); ssq = P.sb('ssq', [128, 2])
    obf = P.sb('obf', [128, 512], BF16)
    P.memset(raw[:], 0.0); P.memset(cmhA[:], 0.0); P.memset(cmhB[:], 0.0); P.memset(Em[:], 0.0)
    for tt in range(NT):
        ts_ = slice(tt * 128, (tt + 1) * 128)
        if tt > 0:
            P.copy(raw[:, :, 0:3], raw[:, :, 128:131], eng='act')
        for j in range(8):
            for kc in range(8):
                P.mm(BR[:, j, :], w[:, kc, 512 + j * 128:512 + (j + 1) * 128], hT[:, kc, ts_], start=(kc == 0), stop=(kc == 7))
        P.copy(raw[:, 0:4, 3:131], BR[:, 0:4, :], eng='act')
        P.copy(raw[:, 4:8, 3:131], BR[:, 4:8, :], eng='act')
        P.tt(acc[:], raw[:, :, 3:131], cw[:, :, 3:4].broadcast_to([128, 8, 128]), ALU.mult)
        for i in range(3):
            P.tt(tmp[:], raw[:, :, i:i + 128], cw[:, :, i:i + 1].broadcast_to([128, 8, 128]), ALU.mult)
            P.tt(acc[:], acc[:], tmp[:], ALU.add)
        P.tt(acc[:], acc[:], cbias[:].unsqueeze(2).broadcast_to([128, 8, 128]), ALU.add)
        P.act(xsb[:], acc[:, 0:4, :], AF.Silu)
        P.act(Bb[:], acc[:, 4:6, :], AF.Silu)
        P.act(Cb[:], acc[:, 6:8, :], AF.Silu)
        for c in range(4):
            P.tr(ptx[:, c, :], xsb[:, c, :], self.identb[:])
        P.copy(xs_tm[:].rearrange("p h d -> p (h d)"), Bt[:, 0:512], eng='act')
        for g in range(2):
            P.tr(ptb_[:, g, :], Bb[:, g, :], self.identb[:])
        P.copy(bm_tm[:], ptb_, eng='act')
        self.proj_tm(pdt, hT, w, tt, 8, c0=1536)
        P.tt(dt[:], pdt, dtb[:], ALU.add)
        P.act(dt[:], dt[:], AF.Exp)
        P.act(dt[:], dt[:], AF.Ln, bias=1.0)
        P.tt(dA[:], dt[:], abc[:], ALU.mult)
        P.mm(pacs, self.blktri[:], dA[:])
        P.mm(ptot, self.blkones[:], dA[:])
        P.copy(acs[:], pacs)
        P.tt(dte[:], ptot, acs[:], ALU.subtract)
        P.act(dte[:], dte[:], AF.Exp)
        P.tt(xdt[:], xs_tm[:], dt[:].unsqueeze(2).broadcast_to([128, 8, 64]), ALU.mult)
        P.tt(xdtd[:], xdt[:], dte[:].unsqueeze(2).broadcast_to([128, 8, 64]), ALU.mult)
        P.tt(Dg[:], self.ident[:].unsqueeze(1).broadcast_to([128, 8, 128]), acs[:].unsqueeze(2).broadcast_to([128, 8, 128]), ALU.mult)
        P.mm(BR[:, 0:4, :], self.ones[:], Dg[:, 0:4, :])
        P.mm(BR[:, 4:8, :], self.ones[:], Dg[:, 4:8, :])
        P.tt(seg[:], BR[:], acs[:].unsqueeze(2).broadcast_to([128, 8, 128]), ALU.subtract)
        P.tt(seg[:], seg[:], negm[:].unsqueeze(1).broadcast_to([128, 8, 128]), ALU.add)
        P.act(Lm[:], seg[:], AF.Exp)
        P.act(eacs[:], BR[:], AF.Exp)
        for g in range(2):
            P.mm(pcb[:, g, :], Bb[:, g, :], Cb[:, g, :])
        P.tt(MT[:].rearrange("p (g e) t -> p g e t", g=2), Lm[:].rearrange("p (g e) t -> p g e t", g=2),
             pcb.unsqueeze(2).broadcast_to([128, 2, 4, 128]), ALU.mult)
        P.tt(cmhA[:, :, 0:64].rearrange("p (g e) t -> p g e t", g=2), eacs[:, :, 0:64].rearrange("p (g e) t -> p g e t", g=2),
             Cb[:, :, 0:64].unsqueeze(2).broadcast_to([128, 2, 4, 64]), ALU.mult)
        P.tt(cmhB[:, :, 64:128].rearrange("p (g e) t -> p g e t", g=2), eacs[:, :, 64:128].rearrange("p (g e) t -> p g e t", g=2),
             Cb[:, :, 64:128].unsqueeze(2).broadcast_to([128, 2, 4, 64]), ALU.mult)
        for g in range(2):
            P.mm(BstA[:, g * 4:(g + 1) * 4, :], bm_tm[0:64, g, :], xdtd[0:64, g * 4:(g + 1) * 4, :])
            P.mm(BstB[:, g * 4:(g + 1) * 4, :], bm_tm[64:128, g, :], xdtd[64:128, g * 4:(g + 1) * 4, :])
        P.copy(EAb[:], Em[:], eng='act')
        P.tt(Em[:], Em[:], eacs[:, :, 63:64].broadcast_to([128, 8, 64]), ALU.mult)
        P.tt(Em[:], Em[:], BstA[:], ALU.add)
        P.copy(EBb[:], Em[:], eng='act')
        for h in range(8):
            P.mm(By[:, h, :], MT[:, h, :], xdt[:, h, :], start=True, stop=False)
            P.mm(By[:, h, :], cmhA[:, h, :], EAb[:, h, :], start=False, stop=False)
            P.mm(By[:, h, :], cmhB[:, h, :], EBb[:, h, :], start=False, stop=True)
        P.tt(Em[:], Em[:], eacs[:, :, 127:128].broadcast_to([128, 8, 64]), ALU.mult)
        P.tt(Em[:], Em[:], BstB[:], ALU.add)
        P.tt(y2[:], xs_tm[:], dsk[:].unsqueeze(2).broadcast_to([128, 8, 64]), ALU.mult)
        P.tt(y2[:], y2[:], By[:], ALU.add)
        self.proj_tm(Bg[:], hT, w, tt, 512, c0=0)
        P.act(sgt[:], Bg[:], AF.Silu)
        y2f = y2[:].rearrange("p h d -> p (h d)")
        P.tt(y2f, y2f, sgt[:], ALU.mult)
        y2g = y2[:].rearrange("p (g e) d -> p g (e d)", g=2)
        P.act(sq[:], y2g, AF.Square)
        P.reduce(ssq[:], sq[:], ALU.add)
        P.act(ssq[:], ssq[:], AF.Sqrt, bias=EPS, scale=1.0 / 256)
        P.recip(ssq[:], ssq[:])
        P.tt(sq[:], y2g, ssq[:].unsqueeze(2).broadcast_to([128, 2, 256]), ALU.mult)
        P.tt(obf[:], sq[:].rearrange("p g t -> p (g t)"), nw[:], ALU.mult)
        self.to_T(obf, oT, tt, ptr, eng='act')
    P.end_phase()


K.mamba = mamba


def mamba2(self, l, hT, oT):
    P = self.P
    w_in = self.inp['w_in']
    P.begin_phase()
    w = P.sb('w_ssm', [128, 8, 1544], BF16)
    self.load_w(w[:], w_in[l, :, C_SSM:C_SSM + 1544])
    cw = P.sb('cw', [128, 8, 4])
    P.dma('sp', cw[:], self.inp['h_convw'][l])
    cbias = P.sb('cbias', [128, 8])
    P.dma('sp', cbias[:], self.inp['h_convb'][l])
    dtb = P.sb('dtb', [128, 8]); abc = P.sb('abc', [128, 8]); dsk = P.sb('dsk', [128, 8])
    P.dma('sp', dtb[:], self.inp['ssm_dt_bias'][l].partition_broadcast(128))
    P.dma('sp', abc[:], self.inp['ssm_a_log'][l].partition_broadcast(128))
    P.dma('sp', dsk[:], self.inp['ssm_d'][l].partition_broadcast(128))
    P.act(abc[:], abc[:], AF.Exp)
    P.ts(abc[:], abc[:], -1.0, ALU.mult)
    nw = P.sb('nw', [128, 512])
    P.dma('sp', nw[:], self.inp['ssm_norm_w'][l].partition_broadcast(128))
    negm = P.sb('negm', [128, 128])
    P.dma('sp', negm[:], self.inp['c_negmask'])
    BR = P.ps('BR', [128, 8, 128])
    Bs = P.ps('Bs', [128, 512])
    BstA = P.ps('BstA', [128, 8, 64]); BstB = P.ps('BstB', [128, 8, 64])
    By = P.ps('By', [128, 8, 64])
    Bc = P.ps('Bc', [128, 4, 128])
    Bt = P.ps('Bt', [128, 1024], BF16)
    pdt = Bs[:, 0:8]; pacs = Bs[:, 8:16]; ptot = Bs[:, 16:24]
    pcb = Bs[:, 256:512].rearrange("p (g t) -> p g t", g=2)
    ptx = Bt[:, 0:512].rearrange("p (c t) -> p c t", c=4)
    ptb_ = Bt[:, 0:256].rearrange("p (c t) -> p c t", c=2)
    ptr = Bt[:, 0:512].rearrange("p (c t) -> p c t", c=4)
    raw = P.sb('raw', [128, 8, 131])
    acc = P.sb('acc', [128, 8, 128]); tmp = P.sb('tmp', [128, 8, 128])
    xsb = P.sb('xsb', [128, 4, 128], BF16); Bb = P.sb('Bb', [128, 2, 128], BF16); Cb = P.sb('Cb', [128, 2, 128], BF16)
    xs_tm = P.sb('xs_tm', [128, 8, 64], BF16)
    bm_tm = P.sb('bm_tm', [128, 2, 128], BF16)
    dt = P.sb('dt', [128, 8]); dA = P.sb('dA', [128, 8]); acs = P.sb('acs', [128, 8]); dte = P.sb('dte', [128, 8])
    xdt = P.sb('xdt', [128, 8, 64], BF16); xdtd = P.sb('xdtd', [128, 8, 64], BF16)
    Dg = P.sb('Dg', [128, 8, 128]); seg = P.sb('seg', [128, 8, 128]); Lm = P.sb('Lm', [128, 8, 128])
    eacs = P.sb('eacs', [128, 8, 128])
    MT = P.sb('MT', [128, 8, 128], BF16)
    cmhA = P.sb('cmhA', [128, 8, 128], BF16); cmhB = P.sb('cmhB', [128, 8, 128], BF16)
    Em = P.sb('Em', [128, 8, 64]); EAb = P.sb('EAb', [128, 8, 64], BF16); EBb = P.sb('EBb', [128, 8, 64], BF16)
    y2 = P.sb('y2', [128, 8, 64]); sgt = P.sb('sgt', [128, 512]); sq = P.sb('sq', [128, 2, 256]); ssq = P.sb('ssq', [128, 2])
    obf = P.sb('obf', [128, 512], BF16)
    P.memset(raw[:], 0.0); P.memset(cmhA[:], 0.0); P.memset(cmhB[:], 0.0); P.memset(Em[:], 0.0)
    def emit_proj(tq):
        tsq = slice(tq * 128, (tq + 1) * 128)
        if tq > 0:
            P.copy(raw[:, :, 0:3], raw[:, :, 128:131], eng='act')
        for j in range(8):
            for kc in range(8):
                P.mm(BR[:, j, :], w[:, kc, 512 + j * 128:512 + (j + 1) * 128], hT[:, kc, tsq], start=(kc == 0), stop=(kc == 7))
        P.copy(raw[:, 0:4, 3:131], BR[:, 0:4, :], eng='act')
        P.copy(raw[:, 4:8, 3:131], BR[:, 4:8, :], eng='act')

    for tt in range(NT):
        ts_ = slice(tt * 128, (tt + 1) * 128)
        if tt == 0:
            emit_proj(0)
        P.tt(acc[:], raw[:, :, 3:131], cw[:, :, 3:4].broadcast_to([128, 8, 128]), ALU.mult)
        for i in range(3):
            P.tt(tmp[:], raw[:, :, i:i + 128], cw[:, :, i:i + 1].broadcast_to([128, 8, 128]), ALU.mult)
            P.tt(acc[:], acc[:], tmp[:], ALU.add)
        P.tt(acc[:], acc[:], cbias[:].unsqueeze(2).broadcast_to([128, 8, 128]), ALU.add)
        P.act(xsb[:], acc[:, 0:4, :], AF.Silu)
        P.act(Bb[:], acc[:, 4:6, :], AF.Silu)
        P.act(Cb[:], acc[:, 6:8, :], AF.Silu)
        if tt + 1 < NT:
            emit_proj(tt + 1)
        for c in range(4):
            P.tr(ptx[:, c, :], xsb[:, c, :], self.identb[:])
        P.copy(xs_tm[:].rearrange("p h d -> p (h d)"), Bt[:, 0:512], eng='act')
        for g in range(2):
            P.tr(ptb_[:, g, :], Bb[:, g, :], self.identb[:])
        P.copy(bm_tm[:], ptb_, eng='act')
        self.proj_tm(pdt, hT, w, tt, 8, c0=1536)
        P.tt(dt[:], pdt, dtb[:], ALU.add)
        P.act(dt[:], dt[:], AF.Exp)
        P.act(dt[:], dt[:], AF.Ln, bias=1.0)
        P.tt(dA[:], dt[:], abc[:], ALU.mult)
        P.mm(pacs, self.blktri[:], dA[:])
        P.mm(ptot, self.blkones[:], dA[:])
        P.copy(acs[:], pacs)
        P.tt(dte[:], ptot, acs[:], ALU.subtract)
        P.act(dte[:], dte[:], AF.Exp)
        P.tt(xdt[:], xs_tm[:], dt[:].unsqueeze(2).broadcast_to([128, 8, 64]), ALU.mult)
        P.tt(xdtd[:], xdt[:], dte[:].unsqueeze(2).broadcast_to([128, 8, 64]), ALU.mult)
        P.tt(Dg[:], self.ident[:].unsqueeze(1).broadcast_to([128, 8, 128]), acs[:].unsqueeze(2).broadcast_to([128, 8, 128]), ALU.mult)
        for hh in range(2):
            hs = slice(hh * 4, (hh + 1) * 4)
            P.mm(Bc, self.ones[:], Dg[:, hs, :])
            P.tt(seg[:, hs, :], Bc, acs[:, hs].unsqueeze(2).broadcast_to([128, 4, 128]), ALU.subtract)
            P.act(eacs[:, hs, :], Bc, AF.Exp)
        P.tt(seg[:], seg[:], negm[:].unsqueeze(1).broadcast_to([128, 8, 128]), ALU.add)
        P.act(Lm[:], seg[:], AF.Exp)
        for g in range(2):
            P.mm(pcb[:, g, :], Bb[:, g, :], Cb[:, g, :])
        P.tt(MT[:].rearrange("p (g e) t -> p g e t", g=2), Lm[:].rearrange("p (g e) t -> p g e t", g=2),
             pcb.unsqueeze(2).broadcast_to([128, 2, 4, 128]), ALU.mult)
        P.tt(cmhA[:, :, 0:64].rearrange("p (g e) t -> p g e t", g=2), eacs[:, :, 0:64].rearrange("p (g e) t -> p g e t", g=2),
             Cb[:, :, 0:64].unsqueeze(2).broadcast_to([128, 2, 4, 64]), ALU.mult)
        P.tt(cmhB[:, :, 64:128].rearrange("p (g e) t -> p g e t", g=2), eacs[:, :, 64:128].rearrange("p (g e) t -> p g e t", g=2),
             Cb[:, :, 64:128].unsqueeze(2).broadcast_to([128, 2, 4, 64]), ALU.mult)
        for g in range(2):
            P.mm(BstA[:, g * 4:(g + 1) * 4, :], bm_tm[0:64, g, :], xdtd[0:64, g * 4:(g + 1) * 4, :])
            P.mm(BstB[:, g * 4:(g + 1) * 4, :], bm_tm[64:128, g, :], xdtd[64:128, g * 4:(g + 1) * 4, :])
        P.copy(EAb[:], Em[:], eng='act')
        P.tt(Em[:], Em[:], eacs[:, :, 63:64].broadcast_to([128, 8, 64]), ALU.mult)
        P.tt(Em[:], Em[:], BstA[:], ALU.add)
        P.copy(EBb[:], Em[:], eng='act')
        for h in range(8):
            P.mm(By[:, h, :], MT[:, h, :], xdt[:, h, :], start=True, stop=False)
            P.mm(By[:, h, :], cmhA[:, h, :], EAb[:, h, :], start=False, stop=False)
            P.mm(By[:, h, :], cmhB[:, h, :], EBb[:, h, :], start=False, stop=True)
        P.tt(Em[:], Em[:], eacs[:, :, 127:128].broadcast_to([128, 8, 64]), ALU.mult)
        P.tt(Em[:], Em[:], BstB[:], ALU.add)
        P.tt(y2[:], xs_tm[:], dsk[:].unsqueeze(2).broadcast_to([128, 8, 64]), ALU.mult)
        P.tt(y2[:], y2[:], By[:], ALU.add)
        Bg = By.rearrange("p h d -> p (h d)")
        self.proj_tm(Bg, hT, w, tt, 512, c0=0)
        P.act(sgt[:], Bg, AF.Silu)
        y2f = y2[:].rearrange("p h d -> p (h d)")
        P.tt(y2f, y2f, sgt[:], ALU.mult)
        y2g = y2[:].rearrange("p (g e) d -> p g (e d)", g=2)
        P.act(sq[:], y2g, AF.Square)
        P.reduce(ssq[:], sq[:], ALU.add)
        P.act(ssq[:], ssq[:], AF.Sqrt, bias=EPS, scale=1.0 / 256)
        P.recip(ssq[:], ssq[:])
        P.tt(sq[:], y2g, ssq[:].unsqueeze(2).broadcast_to([128, 2, 256]), ALU.mult)
        P.tt(obf[:], sq[:].rearrange("p g t -> p (g t)"), nw[:], ALU.mult)
        self.to_T(obf, oT, tt, ptr, eng='act')
    P.end_phase()


K.mamba = mamba2


def rwkv(self, l, hT, oT, vfirst):
    P = self.P
    I = self.inp
    w_in = I['w_in']
    P.begin_phase()
    w = P.sb('w_rwkv', [128, 8, 1824], BF16)
    self.load_w(w[:], w_in[l, :, 0:1824])
    dup = P.sb('dup', [64, 512], BF16); aup = P.sb('aup', [64, 512], BF16)
    P.dma('pool', dup[:], I['rwkv_decay_up'][l]); P.dma('pool', aup[:], I['rwkv_a_up'][l])
    gu1 = P.sb('gu1', [128, 512], BF16); gu2 = P.sb('gu2', [32, 512], BF16)
    P.dma('pool', gu1[:], I['rwkv_gate_up'][l, 0:128, :]); P.dma('pool', gu2[:], I['rwkv_gate_up'][l, 128:160, :])
    mu = P.sb('mu', [64, 26]); mug = P.sb('mug', [128, 2])
    P.dma('sp', mu[:], I['h_mu64'][l]); P.dma('sp', mug[:], I['h_mug'][l])
    rw = P.sb('rw', [64, 6, 8])
    P.dma('sp', rw[:], I['h_rw'][l])
    w0, a0, kkw, kaw, rkw, v0w = [rw[:, i, :] for i in range(6)]
    lnw = P.sb('lnw', [64, 512]); lnb = P.sb('lnb', [64, 512])
    P.dma('sp', lnw[:], I['rwkv_ln_w'][l].partition_broadcast(64)); P.dma('sp', lnb[:], I['rwkv_ln_b'][l].partition_broadcast(64))
    m1 = P.sb('m1', [64, 128]); SL = P.sb('SL', [64, 64]); smask = P.sb('smask', [64, 1024])
    P.dma('sp', m1[:], I['c_m1']); P.dma('sp', SL[:], I['c_sl']); P.dma('sp', smask[:], I['c_scanmask'])
    if l > 0:
        wdown = P.sb('wdown', [128, 8, 32], BF16); wup = P.sb('wup', [32, 512], BF16)
        self.load_w(wdown[:], I['vres_down'][l - 1]); P.dma('pool', wup[:], I['vres_up'][l - 1])
        tb = P.sb('tb', [32, 128], BF16)
    I64 = self.ident[0:64, 0:64]
    T01 = P.ps('T01', [128, 1024]); T23 = P.ps('T23', [128, 1024])
    B4 = P.ps('B4', [128, 512]); B5 = P.ps('B5', [128, 512]); B6 = P.ps('B6', [128, 512]); B7 = P.ps('B7', [128, 512])
    def v8(t, c0=0, n=128):
        return t[0:64, c0:c0 + 8 * n].rearrange("p (h t) -> p h t", h=8)
    raw = P.sb('raw', [64, 26, 129]); rawg = P.sb('rawg', [128, 2, 129])
    z = P.sb('z', [64, 26, 128]); zg = P.sb('zg', [128, 2, 128])
    txw = P.sb('txw', [64, 128], BF16); xab = P.sb('xab', [64, 128], BF16); sxg = P.sb('sxg', [128, 2, 128], BF16)
    Ta = P.sb('Ta', [64, 8, 128]); Tb = P.sb('Tb', [64, 8, 128]); Tc = P.sb('Tc', [64, 8, 128]); Td = P.sb('Td', [64, 8, 128])
    Te = P.sb('Te', [64, 8, 128]); Tf = P.sb('Tf', [64, 8, 128]); Tg = P.sb('Tg', [64, 8, 128]); Th = P.sb('Th', [64, 8, 128])
    AR = P.sb('AR', [64, 8, 2, 2, 64]); gT = P.sb('gT', [64, 8, 2])
    Vc = P.sb('Vc', [64, 2, 8, 64])
    M1 = P.sb('M1', [64, 8, 128]); M2 = P.sb('M2', [64, 8, 128])
    Q = P.sb('Q', [64, 8, 64]); XT = P.sb('XT', [64, 8, 64]); X = P.sb('X', [64, 8, 64])
    Wsb = P.sb('Wsb', [64, 8, 64]); Usb = P.sb('Usb', [64, 8, 64])
    KT = P.sb('KT', [64, 8, 64]); BT = P.sb('BT', [64, 8, 64])
    S0T = P.sb('S0T', [64, 8, 64])
    Y1 = P.sb('Y1', [64, 8, 64]); Y2 = P.sb('Y2', [64, 8, 64]); obf = P.sb('obf', [64, 512])
    st8 = P.sb('st8', [64, 8]); st8b = P.sb('st8b', [64, 8]); sbon = P.sb('sbon', [64, 8])
    P.memset(raw[:], 0.0); P.memset(rawg[:], 0.0); P.memset(S0T[:], 0.0)
    r_ = z[:, 0:8, :]; k_ = z[:, 8:16, :]; vT = z[:, 16:24, :]
    Kh = z[:, 8:16, :]
    Bh = z[:, 0:8, :]
    b8 = lambda a: a.unsqueeze(2).broadcast_to([64, 8, 128])
    for tt in range(NT):
        ts_ = slice(tt * 128, (tt + 1) * 128)
        if tt > 0:
            P.copy(raw[:, :, 0:1], raw[:, :, 128:129], eng='act')
            P.copy(rawg[:, :, 0:1], rawg[:, :, 128:129], eng='act')
        for grp, (ps_t, c0) in enumerate([(T01, 0), (T23, 512), (T01, 1024)]):
            for h in range(8):
                for kc in range(8):
                    P.mm(ps_t[0:64, h * 128:(h + 1) * 128], w[:, kc, c0 + h * 64:c0 + (h + 1) * 64], hT[:, kc, ts_],
                         start=(kc == 0), stop=(kc == 7))
            P.copy(raw[:, grp * 8:(grp + 1) * 8, 1:129], v8(ps_t), eng='act' if grp != 1 else 'dve')
        for i in range(2):
            for kc in range(8):
                P.mm(B4[0:64, i * 128:(i + 1) * 128], w[:, kc, 1536 + i * 64:1536 + (i + 1) * 64], hT[:, kc, ts_],
                     start=(kc == 0), stop=(kc == 7))
        for kc in range(8):
            P.mm(B4[:, 256:384], w[:, kc, 1664:1792], hT[:, kc, ts_], start=(kc == 0), stop=(kc == 7))
        for kc in range(8):
            P.mm(B5[0:32, 0:128], w[:, kc, 1792:1824], hT[:, kc, ts_], start=(kc == 0), stop=(kc == 7))
        P.copy(raw[:, 24:26, 1:129], B4[0:64, 0:256].rearrange("p (c t) -> p c t", c=2), eng='act')
        P.copy(rawg[:, 0, 1:129], B4[:, 256:384], eng='act')
        P.copy(rawg[0:32, 1, 1:129], B5[0:32, 0:128], eng='act')
        P.tt(z[:], raw[:, :, 0:128], raw[:, :, 1:129], ALU.subtract)
        P.tt(z[:], z[:], mu[:].unsqueeze(2).broadcast_to([64, 26, 128]), ALU.mult)
        P.tt(z[:], z[:], raw[:, :, 1:129], ALU.add)
        P.tt(zg[:], rawg[:, :, 0:128], rawg[:, :, 1:129], ALU.subtract)
        P.tt(zg[:], zg[:], mug[:].unsqueeze(2).broadcast_to([128, 2, 128]), ALU.mult)
        P.tt(zg[:], zg[:], rawg[:, :, 1:129], ALU.add)
        P.act(txw[:], z[:, 24, :], AF.Tanh)
        P.copy(xab[:], z[:, 25, :], eng='act')
        P.act(sxg[:], zg[:], AF.Sigmoid)
        for h in range(8):
            P.mm(T01[0:64, h * 128:(h + 1) * 128], dup[:, h * 64:(h + 1) * 64], txw[:])
            P.mm(T23[0:64, h * 128:(h + 1) * 128], aup[:, h * 64:(h + 1) * 64], xab[:])
        P.tt(Ta[:], v8(T01), b8(w0), ALU.add)
        P.act(Ta[:], Ta[:], AF.Sigmoid)
        P.ts(Ta[:], Ta[:], -0.6065306597126334, ALU.mult)
        P.tt(Tb[:], v8(T23), b8(a0), ALU.add)
        P.act(Tb[:], Tb[:], AF.Sigmoid)
        P.scan(Tc[:].rearrange("p h t -> p (h t)"), smask[:], Ta[:].rearrange("p h t -> p (h t)"), 0.0, ALU.mult, ALU.add)
        P.act(Td[:], Tc[:], AF.Exp)
        P.act(Te[:], Tc[:], AF.Exp, scale=-1.0)
        P.tt(Ta[:], Tc[:], Ta[:], ALU.subtract)
        P.act(Ta[:], Ta[:], AF.Exp)
        P.copy(gT[:], Td[:].rearrange("p h (c t) -> p h c t", c=2)[:, :, :, 63], eng='act')
        P.tt(Tf[:], k_, b8(kkw), ALU.mult)
        P.act(Tg[:], Tf[:], AF.Square)
        P.mm(T01[0:64, 0:512], self.ones[0:64, 0:64], Tg[:, 0:4, :])
        P.mm(T01[0:64, 512:1024], self.ones[0:64, 0:64], Tg[:, 4:8, :])
        P.act(Tg[:], v8(T01), AF.Sqrt)
        P.ts(Tg[:], Tg[:], 1e-12, ALU.max)
        P.recip(Tg[:], Tg[:])
        P.tt(Tf[:], Tf[:], Tg[:], ALU.mult)
        P.stt(Tg[:], Tb[:], -1.0, b8(kaw), ALU.add, ALU.mult)
        P.stt(Tg[:], Tg[:], 1.0, k_, ALU.add, ALU.mult)
        P.tt(Th[:], r_, Tg[:], ALU.mult)
        P.tt(Th[:], Th[:], b8(rkw), ALU.mult)
        r4 = lambda a: a.rearrange("p h (c t) -> p h c t", c=2)
        P.tt(AR[:, :, :, 1, :], r4(r_), r4(Td[:]), ALU.mult)
        P.stt(AR[:, :, :, 0, :], r4(Tf[:]), -1.0, r4(Ta[:]), ALU.mult, ALU.mult)
        P.tt(Kh, Tg[:], Te[:], ALU.mult)
        P.tt(Tf[:], Tf[:], Tb[:], ALU.mult)
        P.tt(Bh, Tf[:], Te[:], ALU.mult)
        if l == 0:
            P.dma('sp', vfirst[tt], vT, track_dram=False)
        else:
            vf = M2
            vm = M1
            P.dma('sp', vf[:], vfirst[tt], track_dram=False)
            for kc in range(8):
                P.mm(B5[0:32, 0:128], wdown[:, kc, :], hT[:, kc, ts_], start=(kc == 0), stop=(kc == 7))
            P.copy(tb[:], B5[0:32, 0:128], eng='act')
            for h in range(8):
                P.mm(T23[0:64, h * 128:(h + 1) * 128], wup[:, h * 64:(h + 1) * 64], tb[:])
            P.tt(vm[:], v8(T23), b8(v0w), ALU.add)
            P.act(vm[:], vm[:], AF.Sigmoid)
            P.tt(vf[:], vf[:], vT, ALU.subtract)
            P.tt(vf[:], vf[:], vm[:], ALU.mult)
            P.tt(vT, vT, vf[:], ALU.add)
        for Xc in range(2):
            for h in range(8):
                P.tr(T23[0:64, (Xc * 8 + h) * 64:(Xc * 8 + h + 1) * 64], vT[:, h, Xc * 64:(Xc + 1) * 64], I64)
        P.copy(Vc[:].rearrange("p c h i -> p (c h i)"), T23[0:64, :], eng='act')
        for Xc in range(2):
            cs = slice(Xc * 64, (Xc + 1) * 64)
            pS1 = v8(T01); pS2 = v8(T23)
            pQ = v8(B4, 0, 64); pP = v8(B5, 0, 64); pX1 = v8(B6, 0, 64); pX2 = v8(B7, 0, 64)
            for h in range(8):
                ar = AR[:, h, Xc, :, :].rearrange("p a t -> p (a t)")
                P.mm(pS1[:, h, :], Bh[:, h, cs], ar)
                P.mm(pS2[:, h, :], Kh[:, h, cs], ar)
                P.mm(pQ[:, h, :], AR[:, h, Xc, 0, :], Bh[:, h, cs])
            P.tt(M1[:], pS1, m1[:].unsqueeze(1).broadcast_to([64, 8, 128]), ALU.mult)
            P.tt(M2[:], pS2, m1[:].unsqueeze(1).broadcast_to([64, 8, 128]), ALU.mult)
            P.tt(Q[:], pQ, SL[:].unsqueeze(1).broadcast_to([64, 8, 64]), ALU.mult)
            Pm = M1[:, :, 0:64]
            P.tt(XT[:], Pm, I64.unsqueeze(1).broadcast_to([64, 8, 64]), ALU.add)
            P.tt(X[:], Q[:], I64.unsqueeze(1).broadcast_to([64, 8, 64]), ALU.add)
            for kq in range(1, 6):
                last = (kq == 5)
                for h in range(8):
                    P.mm(pP[:, h, :], Q[:, h, :], Pm[:, h, :])
                if not last:
                    for h in range(8):
                        P.mm(pQ[:, h, :], Pm[:, h, :], Q[:, h, :])
                P.copy(Pm, pP, eng='act')
                if not last:
                    P.copy(Q[:], pQ, eng='dve')
                for h in range(8):
                    P.mm(pX1[:, h, :], X[:, h, :], Pm[:, h, :])
                if not last:
                    for h in range(8):
                        P.mm(pX2[:, h, :], Pm[:, h, :], X[:, h, :])
                P.tt(XT[:], XT[:], pX1, ALU.add)
                if not last:
                    P.tt(X[:], X[:], pX2, ALU.add)
            pW = v8(T01, 0, 64); pU = v8(T01, 512, 64); pY = v8(T23, 0, 64); pSt = v8(T23, 512, 64)
            pKT = v8(B4, 0, 64); pBT = v8(B6, 0, 64)
            for h in range(8):
                P.mm(pW[:, h, :], M2[:, h, 0:64], Vc[:, Xc, h, :], start=True, stop=False)
                P.mm(pW[:, h, :], AR[:, h, Xc, 0, :], S0T[:, h, :], start=False, stop=True)
            P.copy(Wsb[:], pW, eng='act')
            for h in range(8):
                P.mm(pU[:, h, :], XT[:, h, :], Wsb[:, h, :])
            P.copy(Usb[:], pU, eng='act')
            for h in range(8):
                P.mm(pY[:, h, :], AR[:, h, Xc, 1, :], S0T[:, h, :], start=True, stop=False)
                P.mm(pY[:, h, :], M2[:, h, 64:128], Vc[:, Xc, h, :], start=False, stop=False)
                P.mm(pY[:, h, :], M1[:, h, 64:128], Usb[:, h, :], start=False, stop=True)
            for h in range(8):
                P.tr(pKT[:, h, :], Kh[:, h, cs], I64)
                P.tr(pBT[:, h, :], Bh[:, h, cs], I64)
            P.copy(KT[:], pKT, eng='act')
            P.copy(BT[:], pBT, eng='dve')
            for h in range(8):
                P.mm(pSt[:, h, :], KT[:, h, :], Vc[:, Xc, h, :], start=True, stop=False)
                P.mm(pSt[:, h, :], BT[:, h, :], Usb[:, h, :], start=False, stop=True)
            P.reduce(st8[:], pY, ALU.add)
            P.ts(st8[:], st8[:], -1.0 / 64, ALU.mult)
            P.tt(Y1[:], pY, st8[:].unsqueeze(2).broadcast_to([64, 8, 64]), ALU.add)
            P.act(Y2[:], Y1[:], AF.Square)
            P.reduce(st8b[:], Y2[:], ALU.add)
            P.act(st8b[:], st8b[:], AF.Sqrt, bias=64e-5, scale=1.0 / 64)
            P.recip(st8b[:], st8b[:])
            P.tt(Y1[:], Y1[:], st8b[:].unsqueeze(2).broadcast_to([64, 8, 64]), ALU.mult)
            Y1f = Y1[:].rearrange("p h i -> p (h i)")
            P.tt(Y1f, Y1f, lnw[:], ALU.mult)
            P.tt(Y1f, Y1f, lnb[:], ALU.add)
            P.tt(S0T[:], S0T[:], pSt, ALU.add)
            P.tt(S0T[:], S0T[:], gT[:, :, Xc:Xc + 1].broadcast_to([64, 8, 64]), ALU.mult)
            for h in range(8):
                P.mm(B5[0:64, h:h + 1], Th[:, h, cs], self.ones[0:64, 0:1])
            P.copy(sbon[:], B5[0:64, 0:8], eng='act')
            P.tt(Y2[:], Vc[:, Xc, :, :], sbon[:].unsqueeze(2).broadcast_to([64, 8, 64]), ALU.mult)
            P.tt(Y1[:], Y1[:], Y2[:], ALU.add)
            P.mm(B7[0:64, :], sxg[:, 0, cs], gu1[:], start=True, stop=False)
            P.mm(B7[0:64, :], sxg[0:32, 1, cs], gu2[:], start=False, stop=True)
            P.tt(obf[:], Y1f, B7[0:64, :], ALU.mult)
            for c in range(4):
                P.tr(B5[:, 256 + c * 64:256 + (c + 1) * 64], obf[:, c * 128:(c + 1) * 128], I64)
            st = self.ostage[self.ostage_i % 2]
            P.copy(st[:, :, cs], B5[:, 256:512].rearrange("p (c t) -> p c t", c=4), eng='act')
            if Xc == 1:
                self.ostage_i += 1
                P.dma('sp', oT[tt], st[:], track_dram=False)
    P.end_phase()


K.rwkv = rwkv


def rwkv2(self, l, hT, oT, vfirst):
    P = self.P
    I = self.inp
    w_in = I['w_in']
    P.begin_phase()
    w = P.sb('w_rwkv', [128, 8, 1824], BF16)
    self.load_w(w[:], w_in[l, :, 0:1824])
    dup = P.sb('dup', [64, 512], BF16); aup = P.sb('aup', [64, 512], BF16)
    P.dma('pool', dup[:], I['rwkv_decay_up'][l]); P.dma('pool', aup[:], I['rwkv_a_up'][l])
    gu1 = P.sb('gu1', [128, 512], BF16); gu2 = P.sb('gu2', [32, 512], BF16)
    P.dma('pool', gu1[:], I['rwkv_gate_up'][l, 0:128, :]); P.dma('pool', gu2[:], I['rwkv_gate_up'][l, 128:160, :])
    mu = P.sb('mu', [64, 26]); mug = P.sb('mug', [128, 2])
    P.dma('sp', mu[:], I['h_mu64'][l]); P.dma('sp', mug[:], I['h_mug'][l])
    rw = P.sb('rw', [64, 6, 8])
    P.dma('sp', rw[:], I['h_rw'][l])
    w0, a0, kkw, kaw, rkw, v0w = [rw[:, i, :] for i in range(6)]
    lnw = P.sb('lnw', [64, 512]); lnb = P.sb('lnb', [64, 512])
    P.dma('sp', lnw[:], I['rwkv_ln_w'][l].partition_broadcast(64)); P.dma('sp', lnb[:], I['rwkv_ln_b'][l].partition_broadcast(64))
    m1 = P.sb('m1', [64, 128]); SL = P.sb('SL', [64, 64]); smask = P.sb('smask', [64, 1024])
    P.dma('sp', m1[:], I['c_m1']); P.dma('sp', SL[:], I['c_sl']); P.dma('sp', smask[:], I['c_scanmask'])
    if l > 0:
        wdown = P.sb('wdown', [128, 8, 32], BF16); wup = P.sb('wup', [32, 512], BF16)
        self.load_w(wdown[:], I['vres_down'][l - 1]); P.dma('pool', wup[:], I['vres_up'][l - 1])
        tb = P.sb('tb', [32, 128], BF16)
    I64 = self.ident[0:64, 0:64]
    T01 = P.ps('T01', [128, 1024]); T23 = P.ps('T23', [128, 1024])
    B4 = P.ps('B4', [128, 512]); B5 = P.ps('B5', [128, 512]); B6 = P.ps('B6', [128, 512]); B7 = P.ps('B7', [128, 512])
    def v8(t, c0=0, n=128):
        return t[0:64, c0:c0 + 8 * n].rearrange("p (h t) -> p h t", h=8)
    raw = P.sb('raw', [64, 26, 129]); rawg = P.sb('rawg', [128, 2, 129])
    z = P.sb('z', [64, 26, 128]); zg = P.sb('zg', [128, 2, 128])
    txw = P.sb('txw', [64, 128], BF16); xab = P.sb('xab', [64, 128], BF16); sxg = P.sb('sxg', [128, 2, 128], BF16)
    Ta = P.sb('Ta', [64, 8, 128]); Tb = P.sb('Tb', [64, 8, 128]); Tc = P.sb('Tc', [64, 8, 128]); Td = P.sb('Td', [64, 8, 128])
    Te = P.sb('Te', [64, 8, 128]); Tf = P.sb('Tf', [64, 8, 128]); Tg = P.sb('Tg', [64, 8, 128]); Th = P.sb('Th', [64, 8, 128])
    AR = P.sb('AR', [64, 8, 2, 2, 64], BF16); gT = P.sb('gT', [64, 8, 2])
    Vc = P.sb('Vc', [64, 2, 8, 64]); Vcb = P.sb('Vcb', [64, 2, 8, 64], BF16)
    Khb = P.sb('Khb', [64, 8, 128], BF16); Bhb = P.sb('Bhb', [64, 8, 128], BF16)
    KTb = P.sb('KTb', [64, 2, 8, 64], BF16); BTb = P.sb('BTb', [64, 2, 8, 64], BF16)
    M1s = [P.sb('M1_%d' % i, [64, 8, 128], BF16) for i in range(2)]
    M2s = [P.sb('M2_%d' % i, [64, 8, 128], BF16) for i in range(2)]
    Qs = [P.sb('Q_%d' % i, [64, 8, 64], BF16) for i in range(2)]
    Xbs = [P.sb('Xb_%d' % i, [64, 8, 64], BF16) for i in range(2)]
    XTs = [P.sb('XT_%d' % i, [64, 8, 64]) for i in range(2)]
    XTb = [P.sb('XTb_%d' % i, [64, 8, 64], BF16) for i in range(2)]
    Wsb = P.sb('Wsb', [64, 8, 64], BF16); Usb = P.sb('Usb', [64, 8, 64], BF16)
    S0T = P.sb('S0T', [64, 8, 64]); S0Tb = P.sb('S0Tb', [64, 8, 64], BF16)
    vtmp = [P.sb('vtmp%d' % i, [64, 8, 128]) for i in range(2)]
    Y1 = P.sb('Y1', [64, 8, 64]); Y2 = P.sb('Y2', [64, 8, 64]); obf = P.sb('obf', [64, 512])
    st8 = P.sb('st8', [64, 8]); st8b = P.sb('st8b', [64, 8]); sbon = P.sb('sbon', [64, 8])
    P.memset(raw[:], 0.0); P.memset(rawg[:], 0.0); P.memset(S0T[:], 0.0); P.memset(S0Tb[:], 0.0)
    r_ = z[:, 0:8, :]; k_ = z[:, 8:16, :]; vT = z[:, 16:24, :]
    Kh = z[:, 8:16, :]
    Bh = z[:, 0:8, :]
    b8 = lambda a: a.unsqueeze(2).broadcast_to([64, 8, 128])
    for tt in range(NT):
        ts_ = slice(tt * 128, (tt + 1) * 128)
        if tt > 0:
            P.copy(raw[:, :, 0:1], raw[:, :, 128:129], eng='act')
            P.copy(rawg[:, :, 0:1], rawg[:, :, 128:129], eng='act')
        for grp, (ps_t, c0) in enumerate([(T01, 0), (T23, 512), (T01, 1024)]):
            for h in range(8):
                for kc in range(8):
                    P.mm(ps_t[0:64, h * 128:(h + 1) * 128], w[:, kc, c0 + h * 64:c0 + (h + 1) * 64], hT[:, kc, ts_],
                         start=(kc == 0), stop=(kc == 7))
            P.copy(raw[:, grp * 8:(grp + 1) * 8, 1:129], v8(ps_t), eng='act' if grp != 1 else 'dve')
        for i in range(2):
            for kc in range(8):
                P.mm(B4[0:64, i * 128:(i + 1) * 128], w[:, kc, 1536 + i * 64:1536 + (i + 1) * 64], hT[:, kc, ts_],
                     start=(kc == 0), stop=(kc == 7))
        for kc in range(8):
            P.mm(B4[:, 256:384], w[:, kc, 1664:1792], hT[:, kc, ts_], start=(kc == 0), stop=(kc == 7))
        for kc in range(8):
            P.mm(B5[0:32, 0:128], w[:, kc, 1792:1824], hT[:, kc, ts_], start=(kc == 0), stop=(kc == 7))
        P.copy(raw[:, 24:26, 1:129], B4[0:64, 0:256].rearrange("p (c t) -> p c t", c=2), eng='act')
        P.copy(rawg[:, 0, 1:129], B4[:, 256:384], eng='act')
        P.copy(rawg[0:32, 1, 1:129], B5[0:32, 0:128], eng='act')
        P.tt(z[:], raw[:, :, 0:128], raw[:, :, 1:129], ALU.subtract)
        P.tt(z[:], z[:], mu[:].unsqueeze(2).broadcast_to([64, 26, 128]), ALU.mult)
        P.tt(z[:], z[:], raw[:, :, 1:129], ALU.add)
        P.tt(zg[:], rawg[:, :, 0:128], rawg[:, :, 1:129], ALU.subtract)
        P.tt(zg[:], zg[:], mug[:].unsqueeze(2).broadcast_to([128, 2, 128]), ALU.mult)
        P.tt(zg[:], zg[:], rawg[:, :, 1:129], ALU.add)
        P.act(txw[:], z[:, 24, :], AF.Tanh)
        P.copy(xab[:], z[:, 25, :], eng='act')
        P.act(sxg[:], zg[:], AF.Sigmoid)
        for h in range(8):
            P.mm(T01[0:64, h * 128:(h + 1) * 128], dup[:, h * 64:(h + 1) * 64], txw[:])
            P.mm(T23[0:64, h * 128:(h + 1) * 128], aup[:, h * 64:(h + 1) * 64], xab[:])
        P.tt(Ta[:], v8(T01), b8(w0), ALU.add)
        P.act(Ta[:], Ta[:], AF.Sigmoid)
        P.ts(Ta[:], Ta[:], -0.6065306597126334, ALU.mult)
        P.tt(Tb[:], v8(T23), b8(a0), ALU.add)
        P.act(Tb[:], Tb[:], AF.Sigmoid)
        P.scan(Tc[:].rearrange("p h t -> p (h t)"), smask[:], Ta[:].rearrange("p h t -> p (h t)"), 0.0, ALU.mult, ALU.add)
        P.act(Td[:], Tc[:], AF.Exp)
        P.act(Te[:], Tc[:], AF.Exp, scale=-1.0)
        P.tt(Ta[:], Tc[:], Ta[:], ALU.subtract)
        P.act(Ta[:], Ta[:], AF.Exp)
        P.copy(gT[:], Td[:].rearrange("p h (c t) -> p h c t", c=2)[:, :, :, 63], eng='act')
        P.tt(Tf[:], k_, b8(kkw), ALU.mult)
        P.act(Tg[:], Tf[:], AF.Square)
        P.mm(T01[0:64, 0:512], self.ones[0:64, 0:64], Tg[:, 0:4, :])
        P.mm(T01[0:64, 512:1024], self.ones[0:64, 0:64], Tg[:, 4:8, :])
        P.act(Tg[:], v8(T01), AF.Sqrt)
        P.ts(Tg[:], Tg[:], 1e-12, ALU.max)
        P.recip(Tg[:], Tg[:])
        P.tt(Tf[:], Tf[:], Tg[:], ALU.mult)
        P.stt(Tg[:], Tb[:], -1.0, b8(kaw), ALU.add, ALU.mult)
        P.stt(Tg[:], Tg[:], 1.0, k_, ALU.add, ALU.mult)
        P.tt(Th[:], r_, Tg[:], ALU.mult)
        P.tt(Th[:], Th[:], b8(rkw), ALU.mult)
        r4 = lambda a: a.rearrange("p h (c t) -> p h c t", c=2)
        P.tt(AR[:, :, :, 1, :], r4(r_), r4(Td[:]), ALU.mult)
        P.stt(AR[:, :, :, 0, :], r4(Tf[:]), -1.0, r4(Ta[:]), ALU.mult, ALU.mult)
        P.tt(Kh, Tg[:], Te[:], ALU.mult)
        P.tt(Tf[:], Tf[:], Tb[:], ALU.mult)
        P.tt(Bh, Tf[:], Te[:], ALU.mult)
        if l == 0:
            P.dma('sp', vfirst[tt], vT, track_dram=False)
        else:
            vf = vtmp[0]
            vm = vtmp[1]
            P.dma('sp', vf[:], vfirst[tt], track_dram=False)
            for kc in range(8):
                P.mm(B5[0:32, 0:128], wdown[:, kc, :], hT[:, kc, ts_], start=(kc == 0), stop=(kc == 7))
            P.copy(tb[:], B5[0:32, 0:128], eng='act')
            for h in range(8):
                P.mm(T23[0:64, h * 128:(h + 1) * 128], wup[:, h * 64:(h + 1) * 64], tb[:])
            P.tt(vm[:], v8(T23), b8(v0w), ALU.add)
            P.act(vm[:], vm[:], AF.Sigmoid)
            P.tt(vf[:], vf[:], vT, ALU.subtract)
            P.tt(vf[:], vf[:], vm[:], ALU.mult)
            P.tt(vT, vT, vf[:], ALU.add)
        for Xc in range(2):
            for h in range(8):
                P.tr(T23[0:64, (Xc * 8 + h) * 64:(Xc * 8 + h + 1) * 64], vT[:, h, Xc * 64:(Xc + 1) * 64], I64)
        P.copy(Vc[:].rearrange("p c h i -> p (c h i)"), T23[0:64, :], eng='act')
        P.copy(Vcb[:].rearrange("p c h i -> p (c h i)"), T23[0:64, :], eng='dve')
        P.copy(Khb[:], Kh, eng='pool')
        P.copy(Bhb[:], Bh, eng='pool')
        for Xc in range(2):
            for h in range(8):
                P.tr(T01[0:64, (Xc * 8 + h) * 64:(Xc * 8 + h + 1) * 64], Kh[:, h, Xc * 64:(Xc + 1) * 64], I64)
        P.copy(KTb[:].rearrange("p c h i -> p (c h i)"), T01[0:64, :], eng='act')
        for Xc in range(2):
            for h in range(8):
                P.tr(T23[0:64, (Xc * 8 + h) * 64:(Xc * 8 + h + 1) * 64], Bh[:, h, Xc * 64:(Xc + 1) * 64], I64)
        P.copy(BTb[:].rearrange("p c h i -> p (c h i)"), T23[0:64, :], eng='dve')
        I8 = I64.unsqueeze(1).broadcast_to([64, 8, 64])
        for Xc in range(2):
            cs = slice(Xc * 64, (Xc + 1) * 64)
            pS1 = v8(T01); pS2 = v8(T23); pQ0 = v8(B4, 0, 64)
            for h in range(8):
                ar = AR[:, h, Xc, :, :].rearrange("p a t -> p (a t)")
                P.mm(pS1[:, h, :], Bhb[:, h, cs], ar)
                P.mm(pS2[:, h, :], Khb[:, h, cs], ar)
                P.mm(pQ0[:, h, :], AR[:, h, Xc, 0, :], Bhb[:, h, cs])
            P.tt(M1s[Xc][:], pS1, m1[:].unsqueeze(1).broadcast_to([64, 8, 128]), ALU.mult)
            P.tt(M2s[Xc][:], pS2, m1[:].unsqueeze(1).broadcast_to([64, 8, 128]), ALU.mult)
            P.tt(Qs[Xc][:], pQ0, SL[:].unsqueeze(1).broadcast_to([64, 8, 64]), ALU.mult)
            P.tt(XTs[Xc][:], M1s[Xc][:, :, 0:64], I8, ALU.add)
            P.tt(Xbs[Xc][:], Qs[Xc][:], I8, ALU.add)
        bankP = [v8(B4, 0, 64), v8(T01, 0, 64)]
        bankQ = [v8(B5, 0, 64), v8(T01, 512, 64)]
        bankX1 = [v8(B6, 0, 64), v8(T23, 0, 64)]
        bankX2 = [v8(B7, 0, 64), v8(T23, 512, 64)]
        for kq in range(1, 6):
            last = (kq == 5)
            for Xc in range(2):
                Pm = M1s[Xc][:, :, 0:64]
                for h in range(8):
                    P.mm(bankP[Xc][:, h, :], Qs[Xc][:, h, :], Pm[:, h, :])
                if not last:
                    for h in range(8):
                        P.mm(bankQ[Xc][:, h, :], Pm[:, h, :], Qs[Xc][:, h, :])
            for Xc in range(2):
                Pm = M1s[Xc][:, :, 0:64]
                P.copy(Pm, bankP[Xc], eng='act')
                if not last:
                    P.copy(Qs[Xc][:], bankQ[Xc], eng='dve')
            for Xc in range(2):
                Pm = M1s[Xc][:, :, 0:64]
                for h in range(8):
                    P.mm(bankX1[Xc][:, h, :], Xbs[Xc][:, h, :], Pm[:, h, :])
                if not last:
                    for h in range(8):
                        P.mm(bankX2[Xc][:, h, :], Pm[:, h, :], Xbs[Xc][:, h, :])
            for Xc in range(2):
                P.tt(XTs[Xc][:], XTs[Xc][:], bankX1[Xc], ALU.add)
                if not last:
                    P.tt(Xbs[Xc][:], Xbs[Xc][:], bankX2[Xc], ALU.add)
        for Xc in range(2):
            P.copy(XTb[Xc][:], XTs[Xc][:], eng='act')
        for Xc in range(2):
            cs = slice(Xc * 64, (Xc + 1) * 64)
            M1 = M1s[Xc]; M2 = M2s[Xc]
            if Xc == 0:
                pW = v8(B4, 0, 64); pU = v8(B5, 0, 64); pY = v8(B6, 0, 64); pSt = v8(B7, 0, 64)
                gbank = B4; obank = B5
            else:
                pW = v8(T01, 0, 64); pU = v8(T01, 512, 64); pY = v8(T23, 0, 64); pSt = v8(T23, 512, 64)
                gbank = T01[:, 0:512]; obank = T01[:, 512:1024]
            for h in range(8):
                P.mm(pW[:, h, :], M2[:, h, 0:64], Vcb[:, Xc, h, :], start=True, stop=False)
                P.mm(pW[:, h, :], AR[:, h, Xc, 0, :], S0Tb[:, h, :], start=False, stop=True)
            P.copy(Wsb[:], pW, eng='act')
            for h in range(8):
                P.mm(pU[:, h, :], XTb[Xc][:, h, :], Wsb[:, h, :])
            P.copy(Usb[:], pU, eng='act')
            for h in range(8):
                P.mm(pY[:, h, :], AR[:, h, Xc, 1, :], S0Tb[:, h, :], start=True, stop=False)
                P.mm(pY[:, h, :], M2[:, h, 64:128], Vcb[:, Xc, h, :], start=False, stop=False)
                P.mm(pY[:, h, :], M1[:, h, 64:128], Usb[:, h, :], start=False, stop=True)
            for h in range(8):
                P.mm(pSt[:, h, :], KTb[:, Xc, h, :], Vcb[:, Xc, h, :], start=True, stop=False)
                P.mm(pSt[:, h, :], BTb[:, Xc, h, :], Usb[:, h, :], start=False, stop=True)
            P.reduce(st8[:], pY, ALU.add)
            P.ts(st8[:], st8[:], -1.0 / 64, ALU.mult)
            P.tt(Y1[:], pY, st8[:].unsqueeze(2).broadcast_to([64, 8, 64]), ALU.add)
            P.act(Y2[:], Y1[:], AF.Square)
            P.reduce(st8b[:], Y2[:], ALU.add)
            P.act(st8b[:], st8b[:], AF.Sqrt, bias=64e-5, scale=1.0 / 64)
            P.recip(st8b[:], st8b[:])
            P.tt(Y1[:], Y1[:], st8b[:].unsqueeze(2).broadcast_to([64, 8, 64]), ALU.mult)
            Y1f = Y1[:].rearrange("p h i -> p (h i)")
            P.tt(Y1f, Y1f, lnw[:], ALU.mult)
            P.tt(Y1f, Y1f, lnb[:], ALU.add)
            P.tt(S0T[:], S0T[:], pSt, ALU.add)
            P.tt(S0T[:], S0T[:], gT[:, :, Xc:Xc + 1].broadcast_to([64, 8, 64]), ALU.mult)
            P.copy(S0Tb[:], S0T[:], eng='act')
            for h in range(8):
                P.mm(obank[0:64, h:h + 1], Th[:, h, cs], self.ones[0:64, 0:1])
            P.copy(sbon[:], obank[0:64, 0:8], eng='act')
            P.tt(Y2[:], Vc[:, Xc, :, :], sbon[:].unsqueeze(2).broadcast_to([64, 8, 64]), ALU.mult)
            P.tt(Y1[:], Y1[:], Y2[:], ALU.add)
            P.mm(gbank[0:64, :], sxg[:, 0, cs], gu1[:], start=True, stop=False)
            P.mm(gbank[0:64, :], sxg[0:32, 1, cs], gu2[:], start=False, stop=True)
            P.tt(obf[:], Y1f, gbank[0:64, :], ALU.mult)
            for c in range(4):
                P.tr(obank[:, 256 + c * 64:256 + (c + 1) * 64], obf[:, c * 128:(c + 1) * 128], I64)
            st = self.ostage[self.ostage_i % 2]
            P.copy(st[:, :, cs], obank[:, 256:512].rearrange("p (c t) -> p c t", c=4), eng='act')
            if Xc == 1:
                self.ostage_i += 1
                P.dma('sp', oT[tt], st[:], track_dram=False)
    P.end_phase()


K.rwkv = rwkv2


def rwkv3(self, l, hT, oT, vfirst):
    P = self.P
    I = self.inp
    w_in = I['w_in']
    P.begin_phase()
    wa = P.sb('wa_rwkv', [128, 8, 1824], BF16)
    wb = P.sb('wb_rwkv', [128, 8, 1824], BF16)
    P.begin_phase()
    w = P.sb('w_rwkv', [128, 8, 1824], BF16)
    self.load_w(w[:], w_in[l, :, 0:1824])
    mub = P.sb('mub', [128, 1824]); omub = P.sb('omub', [128, 1824])
    P.dma('sp', mub[:], I['rwkv_mu'][l].partition_broadcast(128))
    P.ts(omub[:], mub[:], -1.0, ALU.mult, 1.0, ALU.add)
    for kc in range(8):
        P.tt(wa[:, kc, :], w[:, kc, :], omub[:], ALU.mult, eng='dve')
        P.tt(wb[:, kc, :], w[:, kc, :], mub[:], ALU.mult, eng='dve')
    P.end_phase()
    dup = P.sb('dup', [64, 512], BF16); aup = P.sb('aup', [64, 512], BF16)
    P.dma('pool', dup[:], I['rwkv_decay_up'][l]); P.dma('pool', aup[:], I['rwkv_a_up'][l])
    gu1 = P.sb('gu1', [128, 512], BF16); gu2 = P.sb('gu2', [32, 512], BF16)
    P.dma('pool', gu1[:], I['rwkv_gate_up'][l, 0:128, :]); P.dma('pool', gu2[:], I['rwkv_gate_up'][l, 128:160, :])
    rw = P.sb('rw', [64, 6, 8])
    P.dma('sp', rw[:], I['h_rw'][l])
    w0, a0, kkw, kaw, rkw, v0w = [rw[:, i, :] for i in range(6)]
    lnw = P.sb('lnw', [64, 512]); lnb = P.sb('lnb', [64, 512])
    P.dma('sp', lnw[:], I['rwkv_ln_w'][l].partition_broadcast(64)); P.dma('sp', lnb[:], I['rwkv_ln_b'][l].partition_broadcast(64))
    m1 = P.sb('m1', [64, 128]); SL = P.sb('SL', [64, 64]); smask = P.sb('smask', [64, 1024])
    P.dma('sp', m1[:], I['c_m1']); P.dma('sp', SL[:], I['c_sl']); P.dma('sp', smask[:], I['c_scanmask'])
    if l > 0:
        wdown = P.sb('wdown', [128, 8, 32], BF16); wup = P.sb('wup', [32, 512], BF16)
        self.load_w(wdown[:], I['vres_down'][l - 1]); P.dma('pool', wup[:], I['vres_up'][l - 1])
        tb = P.sb('tb', [32, 128], BF16)
    I64 = self.ident[0:64, 0:64]
    T01 = P.ps('T01', [128, 1024]); T23 = P.ps('T23', [128, 1024])
    B4 = P.ps('B4', [128, 512]); B5 = P.ps('B5', [128, 512]); B6 = P.ps('B6', [128, 512]); B7 = P.ps('B7', [128, 512])
    def v8(t, c0=0, n=128):
        return t[0:64, c0:c0 + 8 * n].rearrange("p (h t) -> p h t", h=8)
    z = P.sb('z', [64, 26, 128]); zg = P.sb('zg', [128, 2, 128])
    txw = P.sb('txw', [64, 128], BF16); xab = P.sb('xab', [64, 128], BF16); sxg = P.sb('sxg', [128, 2, 128], BF16)
    Ta = P.sb('Ta', [64, 8, 128]); Tb = P.sb('Tb', [64, 8, 128]); Tc = P.sb('Tc', [64, 8, 128]); Td = P.sb('Td', [64, 8, 128])
    Te = P.sb('Te', [64, 8, 128]); Tf = P.sb('Tf', [64, 8, 128]); Tg = P.sb('Tg', [64, 8, 128]); Th = P.sb('Th', [64, 8, 128])
    AR = P.sb('AR', [64, 8, 2, 2, 64], BF16); gT = P.sb('gT', [64, 8, 2])
    Vc = P.sb('Vc', [64, 2, 8, 64]); Vcb = P.sb('Vcb', [64, 2, 8, 64], BF16)
    Khb = P.sb('Khb', [64, 8, 128], BF16); Bhb = P.sb('Bhb', [64, 8, 128], BF16)
    KTb = P.sb('KTb', [64, 2, 8, 64], BF16); BTb = P.sb('BTb', [64, 2, 8, 64], BF16)
    M1s = [P.sb('M1_%d' % i, [64, 8, 128], BF16) for i in range(2)]
    M2s = [P.sb('M2_%d' % i, [64, 8, 128], BF16) for i in range(2)]
    Qs = [P.sb('Q_%d' % i, [64, 8, 64], BF16) for i in range(2)]
    Xbs = [P.sb('Xb_%d' % i, [64, 8, 64], BF16) for i in range(2)]
    XTs = [P.sb('XT_%d' % i, [64, 8, 64]) for i in range(2)]
    XTb = [P.sb('XTb_%d' % i, [64, 8, 64], BF16) for i in range(2)]
    Wsb = P.sb('Wsb', [64, 8, 64], BF16); Usb = P.sb('Usb', [64, 8, 64], BF16)
    S0T = P.sb('S0T', [64, 8, 64]); S0Tb = P.sb('S0Tb', [64, 8, 64], BF16)
    Y1 = P.sb('Y1', [64, 8, 64]); Y2 = P.sb('Y2', [64, 8, 64]); obf = P.sb('obf', [64, 512])
    st8 = P.sb('st8', [64, 8]); st8b = P.sb('st8b', [64, 8]); sbon = P.sb('sbon', [64, 8])
    P.memset(zg[:], 0.0); P.memset(S0T[:], 0.0); P.memset(S0Tb[:], 0.0)
    r_ = z[:, 0:8, :]; k_ = z[:, 8:16, :]; vT = z[:, 16:24, :]
    Kh = z[:, 8:16, :]
    Bh = z[:, 0:8, :]
    b8 = lambda a: a.unsqueeze(2).broadcast_to([64, 8, 128])
    for tt in range(NT):
        ts_ = slice(tt * 128, (tt + 1) * 128)
        def proj(out, c0, m):
            for kc in range(8):
                P.mm(out, wa[:, kc, c0:c0 + m], hT[:, kc, ts_], start=(kc == 0), stop=False)
            if tt == 0:
                for kc in range(8):
                    P.mm(out[:, 1:128], wb[:, kc, c0:c0 + m], hT[:, kc, 0:127], start=False, stop=(kc == 7))
            else:
                for kc in range(8):
                    P.mm(out, wb[:, kc, c0:c0 + m], hT[:, kc, tt * 128 - 1:(tt + 1) * 128 - 1], start=False, stop=(kc == 7))
        for grp, (ps_t, c0) in enumerate([(T01, 0), (T23, 512), (T01, 1024)]):
            for h in range(8):
                proj(ps_t[0:64, h * 128:(h + 1) * 128], c0 + h * 64, 64)
            P.copy(z[:, grp * 8:(grp + 1) * 8, :], v8(ps_t), eng='act' if grp != 1 else 'dve')
        for i in range(2):
            proj(B4[0:64, i * 128:(i + 1) * 128], 1536 + i * 64, 64)
        proj(B4[:, 256:384], 1664, 128)
        proj(B5[0:32, 0:128], 1792, 32)
        P.copy(z[:, 24:26, :], B4[0:64, 0:256].rearrange("p (c t) -> p c t", c=2), eng='act')
        P.copy(zg[:, 0, :], B4[:, 256:384], eng='act')
        P.copy(zg[0:32, 1, :], B5[0:32, 0:128], eng='act')
        P.act(txw[:], z[:, 24, :], AF.Tanh)
        P.copy(xab[:], z[:, 25, :], eng='act')
        P.act(sxg[:], zg[:], AF.Sigmoid)
        for h in range(8):
            P.mm(T01[0:64, h * 128:(h + 1) * 128], dup[:, h * 64:(h + 1) * 64], txw[:])
            P.mm(T23[0:64, h * 128:(h + 1) * 128], aup[:, h * 64:(h + 1) * 64], xab[:])
        P.tt(Ta[:], v8(T01), b8(w0), ALU.add)
        P.act(Ta[:], Ta[:], AF.Sigmoid)
        P.tt(Tb[:], v8(T23), b8(a0), ALU.add)
        P.act(Tb[:], Tb[:], AF.Sigmoid)
        P.scan(Tc[:].rearrange("p h t -> p (h t)"), smask[:], Ta[:].rearrange("p h t -> p (h t)"), 0.0, ALU.mult, ALU.add)
        P.act(Td[:], Tc[:], AF.Exp, scale=-0.6065306597126334)
        P.act(Te[:], Tc[:], AF.Exp, scale=0.6065306597126334)
        P.tt(Ta[:], Tc[:], Ta[:], ALU.subtract)
        P.act(Ta[:], Ta[:], AF.Exp, scale=-0.6065306597126334)
        P.copy(gT[:], Td[:].rearrange("p h (c t) -> p h c t", c=2)[:, :, :, 63], eng='act')
        P.tt(Tf[:], k_, b8(kkw), ALU.mult)
        P.act(Tg[:], Tf[:], AF.Square)
        P.mm(T01[0:64, 0:512], self.ones[0:64, 0:64], Tg[:, 0:4, :])
        P.mm(T01[0:64, 512:1024], self.ones[0:64, 0:64], Tg[:, 4:8, :])
        P.act(Tg[:], v8(T01), AF.Sqrt)
        P.ts(Tg[:], Tg[:], 1e-12, ALU.max)
        P.recip(Tg[:], Tg[:])
        P.tt(Tf[:], Tf[:], Tg[:], ALU.mult)
        P.stt(Tg[:], Tb[:], -1.0, b8(kaw), ALU.add, ALU.mult)
        P.stt(Tg[:], Tg[:], 1.0, k_, ALU.add, ALU.mult)
        P.tt(Th[:], r_, Tg[:], ALU.mult)
        P.tt(Th[:], Th[:], b8(rkw), ALU.mult)
        r4 = lambda a: a.rearrange("p h (c t) -> p h c t", c=2)
        P.tt(AR[:, :, :, 1, :], r4(r_), r4(Td[:]), ALU.mult)
        P.stt(AR[:, :, :, 0, :], r4(Tf[:]), -1.0, r4(Ta[:]), ALU.mult, ALU.mult)
        P.tt(Kh, Tg[:], Te[:], ALU.mult)
        P.tt(Tf[:], Tf[:], Tb[:], ALU.mult)
        P.tt(Bh, Tf[:], Te[:], ALU.mult)
        if l == 0:
            P.dma('sp', vfirst[tt], vT, track_dram=False)
        else:
            vf = Ta
            vm = Tb
            P.dma('sp', vf[:], vfirst[tt], track_dram=False)
            for kc in range(8):
                P.mm(B5[0:32, 0:128], wdown[:, kc, :], hT[:, kc, ts_], start=(kc == 0), stop=(kc == 7))
            P.copy(tb[:], B5[0:32, 0:128], eng='act')
            for h in range(8):
                P.mm(T23[0:64, h * 128:(h + 1) * 128], wup[:, h * 64:(h + 1) * 64], tb[:])
            P.tt(vm[:], v8(T23), b8(v0w), ALU.add)
            P.act(vm[:], vm[:], AF.Sigmoid)
            P.tt(vf[:], vf[:], vT, ALU.subtract)
            P.tt(vf[:], vf[:], vm[:], ALU.mult)
            P.tt(vT, vT, vf[:], ALU.add)
        for Xc in range(2):
            for h in range(8):
                P.tr(T23[0:64, (Xc * 8 + h) * 64:(Xc * 8 + h + 1) * 64], vT[:, h, Xc * 64:(Xc + 1) * 64], I64)
        P.copy(Vc[:].rearrange("p c h i -> p (c h i)"), T23[0:64, :], eng='act')
        P.copy(Vcb[:].rearrange("p c h i -> p (c h i)"), T23[0:64, :], eng='dve')
        P.copy(Khb[:], Kh, eng='act')
        P.copy(Bhb[:], Bh, eng='act')
        for Xc in range(2):
            for h in range(8):
                P.tr(T01[0:64, (Xc * 8 + h) * 64:(Xc * 8 + h + 1) * 64], Kh[:, h, Xc * 64:(Xc + 1) * 64], I64)
        P.copy(KTb[:].rearrange("p c h i -> p (c h i)"), T01[0:64, :], eng='act')
        for Xc in range(2):
            for h in range(8):
                P.tr(T23[0:64, (Xc * 8 + h) * 64:(Xc * 8 + h + 1) * 64], Bh[:, h, Xc * 64:(Xc + 1) * 64], I64)
        P.copy(BTb[:].rearrange("p c h i -> p (c h i)"), T23[0:64, :], eng='dve')
        I8 = I64.unsqueeze(1).broadcast_to([64, 8, 64])
        for Xc in range(2):
            cs = slice(Xc * 64, (Xc + 1) * 64)
            pS1 = v8(T01); pS2 = v8(T23); pQ0 = v8(B4, 0, 64)
            for h in range(8):
                ar = AR[:, h, Xc, :, :].rearrange("p a t -> p (a t)")
                P.mm(pS1[:, h, :], Bhb[:, h, cs], ar)
                P.mm(pS2[:, h, :], Khb[:, h, cs], ar)
                P.mm(pQ0[:, h, :], AR[:, h, Xc, 0, :], Bhb[:, h, cs])
            P.tt(M1s[Xc][:], pS1, m1[:].unsqueeze(1).broadcast_to([64, 8, 128]), ALU.mult)
            P.tt(M2s[Xc][:], pS2, m1[:].unsqueeze(1).broadcast_to([64, 8, 128]), ALU.mult)
            P.tt(Qs[Xc][:], pQ0, SL[:].unsqueeze(1).broadcast_to([64, 8, 64]), ALU.mult)
            P.tt(XTs[Xc][:], M1s[Xc][:, :, 0:64], I8, ALU.add)
            P.tt(Xbs[Xc][:], Qs[Xc][:], I8, ALU.add)
        bankP = [v8(B4, 0, 64), v8(T01, 0, 64)]
        bankQ = [v8(B5, 0, 64), v8(T01, 512, 64)]
        bankX1 = [v8(B6, 0, 64), v8(T23, 0, 64)]
        bankX2 = [v8(B7, 0, 64), v8(T23, 512, 64)]
        for kq in range(1, 6):
            last = (kq == 5)
            for Xc in range(2):
                Pm = M1s[Xc][:, :, 0:64]
                for h in range(8):
                    P.mm(bankP[Xc][:, h, :], Qs[Xc][:, h, :], Pm[:, h, :])
                if not last:
                    for h in range(8):
                        P.mm(bankQ[Xc][:, h, :], Pm[:, h, :], Qs[Xc][:, h, :])
            for Xc in range(2):
                Pm = M1s[Xc][:, :, 0:64]
                P.copy(Pm, bankP[Xc], eng='act')
                if not last:
                    P.copy(Qs[Xc][:], bankQ[Xc], eng='act')
            for Xc in range(2):
                Pm = M1s[Xc][:, :, 0:64]
                for h in range(8):
                    P.mm(bankX1[Xc][:, h, :], Xbs[Xc][:, h, :], Pm[:, h, :])
                if not last:
                    for h in range(8):
                        P.mm(bankX2[Xc][:, h, :], Pm[:, h, :], Xbs[Xc][:, h, :])
            for Xc in range(2):
                P.tt(XTs[Xc][:], XTs[Xc][:], bankX1[Xc], ALU.add)
                if not last:
                    P.tt(Xbs[Xc][:], Xbs[Xc][:], bankX2[Xc], ALU.add)
        for Xc in range(2):
            P.copy(XTb[Xc][:], XTs[Xc][:], eng='act')
        for Xc in range(2):
            cs = slice(Xc * 64, (Xc + 1) * 64)
            M1 = M1s[Xc]; M2 = M2s[Xc]
            if Xc == 0:
                pW = v8(B4, 0, 64); pU = v8(B5, 0, 64); pY = v8(B6, 0, 64); pSt = v8(B7, 0, 64)
                gbank = B4; obank = B5
            else:
                pW = v8(T01, 0, 64); pU = v8(T01, 512, 64); pY = v8(T23, 0, 64); pSt = v8(T23, 512, 64)
                gbank = T01[:, 0:512]; obank = T01[:, 512:1024]
            for h in range(8):
                P.mm(pW[:, h, :], M2[:, h, 0:64], Vcb[:, Xc, h, :], start=True, stop=False)
                P.mm(pW[:, h, :], AR[:, h, Xc, 0, :], S0Tb[:, h, :], start=False, stop=True)
            P.copy(Wsb[:], pW, eng='act')
            for h in range(8):
                P.mm(pU[:, h, :], XTb[Xc][:, h, :], Wsb[:, h, :])
            P.copy(Usb[:], pU, eng='act')
            for h in range(8):
                P.mm(pY[:, h, :], AR[:, h, Xc, 1, :], S0Tb[:, h, :], start=True, stop=False)
                P.mm(pY[:, h, :], M2[:, h, 64:128], Vcb[:, Xc, h, :], start=False, stop=False)
                P.mm(pY[:, h, :], M1[:, h, 64:128], Usb[:, h, :], start=False, stop=True)
            for h in range(8):
                P.mm(pSt[:, h, :], KTb[:, Xc, h, :], Vcb[:, Xc, h, :], start=True, stop=False)
                P.mm(pSt[:, h, :], BTb[:, Xc, h, :], Usb[:, h, :], start=False, stop=True)
            P.reduce(st8[:], pY, ALU.add)
            P.ts(st8[:], st8[:], -1.0 / 64, ALU.mult)
            P.tt(Y1[:], pY, st8[:].unsqueeze(2).broadcast_to([64, 8, 64]), ALU.add)
            P.act(Y2[:], Y1[:], AF.Square)
            P.reduce(st8b[:], Y2[:], ALU.add)
            P.act(st8b[:], st8b[:], AF.Sqrt, bias=64e-5, scale=1.0 / 64)
            P.recip(st8b[:], st8b[:])
            P.tt(Y1[:], Y1[:], st8b[:].unsqueeze(2).broadcast_to([64, 8, 64]), ALU.mult)
            Y1f = Y1[:].rearrange("p h i -> p (h i)")
            P.tt(Y1f, Y1f, lnw[:], ALU.mult)
            P.tt(Y1f, Y1f, lnb[:], ALU.add)
            P.tt(S0T[:], S0T[:], pSt, ALU.add)
            P.tt(S0T[:], S0T[:], gT[:, :, Xc:Xc + 1].broadcast_to([64, 8, 64]), ALU.mult)
            P.copy(S0Tb[:], S0T[:], eng='act')
            for h in range(8):
                P.mm(obank[0:64, h:h + 1], Th[:, h, cs], self.ones[0:64, 0:1])
            P.copy(sbon[:], obank[0:64, 0:8], eng='act')
            P.tt(Y2[:], Vc[:, Xc, :, :], sbon[:].unsqueeze(2).broadcast_to([64, 8, 64]), ALU.mult)
            P.tt(Y1[:], Y1[:], Y2[:], ALU.add)
            P.mm(gbank[0:64, :], sxg[:, 0, cs], gu1[:], start=True, stop=False)
            P.mm(gbank[0:64, :], sxg[0:32, 1, cs], gu2[:], start=False, stop=True)
            P.tt(obf[:], Y1f, gbank[0:64, :], ALU.mult)
            for c in range(4):
                P.tr(obank[:, 256 + c * 64:256 + (c + 1) * 64], obf[:, c * 128:(c + 1) * 128], I64)
            st = self.ostage[self.ostage_i % 2]
            P.copy(st[:, :, cs], obank[:, 256:512].rearrange("p (c t) -> p c t", c=4), eng='act')
            if Xc == 1:
                self.ostage_i += 1
                P.dma('sp', oT[tt], st[:], track_dram=False)
    P.end_phase()


K.rwkv = rwkv3


def rwkv4(self, l, hT, oT, vfirst):
    P = self.P
    I = self.inp
    w_in = I['w_in']
    P.begin_phase()
    wa = P.sb('wa_rwkv', [128, 8, 1824], BF16)
    wb = P.sb('wb_rwkv', [128, 8, 1824], BF16)
    P.begin_phase()
    w = P.sb('w_rwkv', [128, 8, 1824], BF16)
    self.load_w(w[:], w_in[l, :, 0:1824])
    mub = P.sb('mub', [128, 1824]); omub = P.sb('omub', [128, 1824])
    P.dma('sp', mub[:], I['rwkv_mu'][l].partition_broadcast(128))
    P.ts(omub[:], mub[:], -1.0, ALU.mult, 1.0, ALU.add)
    for kc in range(8):
        P.tt(wa[:, kc, :], w[:, kc, :], omub[:], ALU.mult, eng='dve')
        P.tt(wb[:, kc, :], w[:, kc, :], mub[:], ALU.mult, eng='dve')
    P.end_phase()
    dup = P.sb('dup', [64, 512], BF16); aup = P.sb('aup', [64, 512], BF16)
    P.dma('pool', dup[:], I['rwkv_decay_up'][l]); P.dma('pool', aup[:], I['rwkv_a_up'][l])
    gu1 = P.sb('gu1', [128, 512], BF16); gu2 = P.sb('gu2', [32, 512], BF16)
    P.dma('pool', gu1[:], I['rwkv_gate_up'][l, 0:128, :]); P.dma('pool', gu2[:], I['rwkv_gate_up'][l, 128:160, :])
    rw = P.sb('rw', [64, 6, 8])
    P.dma('sp', rw[:], I['h_rw'][l])
    w0, a0, kkw, kaw, rkw, v0w = [rw[:, i, :] for i in range(6)]
    lnw = P.sb('lnw', [64, 512]); lnb = P.sb('lnb', [64, 512])
    P.dma('sp', lnw[:], I['rwkv_ln_w'][l].partition_broadcast(64)); P.dma('sp', lnb[:], I['rwkv_ln_b'][l].partition_broadcast(64))
    m1 = P.sb('m1', [64, 128]); SL = P.sb('SL', [64, 64]); smask = P.sb('smask', [64, 1024])
    P.dma('sp', m1[:], I['c_m1']); P.dma('sp', SL[:], I['c_sl']); P.dma('sp', smask[:], I['c_scanmask'])
    if l > 0:
        wdown = P.sb('wdown', [128, 8, 32], BF16); wup = P.sb('wup', [32, 512], BF16)
        self.load_w(wdown[:], I['vres_down'][l - 1]); P.dma('pool', wup[:], I['vres_up'][l - 1])
        tb = P.sb('tb', [32, 128], BF16)
    I64 = self.ident[0:64, 0:64]
    T01 = P.ps('T01', [128, 1024]); T23 = P.ps('T23', [128, 1024])
    B4 = P.ps('B4', [128, 512]); B5 = P.ps('B5', [128, 512]); B6 = P.ps('B6', [128, 512]); B7 = P.ps('B7', [128, 512])
    def v8(t, c0=0, n=128):
        return t[0:64, c0:c0 + 8 * n].rearrange("p (h t) -> p h t", h=8)
    z = P.sb('z', [64, 26, 128]); zg = P.sb('zg', [128, 2, 128])
    txw = P.sb('txw', [64, 128], BF16); xab = P.sb('xab', [64, 128], BF16); sxg = P.sb('sxg', [128, 2, 128], BF16)
    Ta = P.sb('Ta', [64, 8, 128]); Tb = P.sb('Tb', [64, 8, 128]); Tc = P.sb('Tc', [64, 8, 128]); Td = P.sb('Td', [64, 8, 128])
    Te = P.sb('Te', [64, 8, 128]); Tf = P.sb('Tf', [64, 8, 128]); Tg = P.sb('Tg', [64, 8, 128]); Th = P.sb('Th', [64, 8, 128])
    AR = P.sb('AR', [64, 8, 2, 2, 64], BF16); gT = P.sb('gT', [64, 8, 2])
    Vc = P.sb('Vc', [64, 2, 8, 64]); Vcb = P.sb('Vcb', [64, 2, 8, 64], BF16)
    Khb = P.sb('Khb', [64, 8, 128], BF16); Bhb = P.sb('Bhb', [64, 8, 128], BF16)
    KTb = P.sb('KTb', [64, 2, 8, 64], BF16); BTb = P.sb('BTb', [64, 2, 8, 64], BF16)
    M1s = [P.sb('M1_%d' % i, [64, 8, 128], BF16) for i in range(2)]
    M2s = [P.sb('M2_%d' % i, [64, 8, 128], BF16) for i in range(2)]
    Qs = [P.sb('Q_%d' % i, [64, 8, 64], BF16) for i in range(2)]
    Xbs = [P.sb('Xb_%d' % i, [64, 8, 64], BF16) for i in range(2)]
    XTs = [P.sb('XT_%d' % i, [64, 8, 64]) for i in range(2)]
    XTb = [P.sb('XTb_%d' % i, [64, 8, 64], BF16) for i in range(2)]
    Wsb = P.sb('Wsb', [64, 8, 64], BF16); Usb = P.sb('Usb', [64, 8, 64], BF16)
    S0T = P.sb('S0T', [64, 8, 64]); S0Tb = P.sb('S0Tb', [64, 8, 64], BF16)
    Y1 = P.sb('Y1', [64, 8, 64]); Y2 = P.sb('Y2', [64, 8, 64]); obf = P.sb('obf', [64, 512])
    st8 = P.sb('st8', [64, 8]); st8b = P.sb('st8b', [64, 8]); sbon = P.sb('sbon', [64, 8])
    P.memset(zg[:], 0.0); P.memset(S0T[:], 0.0); P.memset(S0Tb[:], 0.0)
    r_ = z[:, 0:8, :]; k_ = z[:, 8:16, :]; vT = z[:, 16:24, :]
    Kh = z[:, 8:16, :]
    Bh = z[:, 0:8, :]
    b8 = lambda a: a.unsqueeze(2).broadcast_to([64, 8, 128])
    def v4(t):
        return t[0:64, 0:512].rearrange("p (h t) -> p h t", h=4)

    def emit_proj(tt):
        ts_ = slice(tt * 128, (tt + 1) * 128)

        def proj(out, c0, m):
            for kc in range(8):
                P.mm(out, wa[:, kc, c0:c0 + m], hT[:, kc, ts_], start=(kc == 0), stop=False)
            if tt == 0:
                for kc in range(8):
                    P.mm(out[:, 1:128], wb[:, kc, c0:c0 + m], hT[:, kc, 0:127], start=False, stop=(kc == 7))
            else:
                for kc in range(8):
                    P.mm(out, wb[:, kc, c0:c0 + m], hT[:, kc, tt * 128 - 1:(tt + 1) * 128 - 1], start=False, stop=(kc == 7))
        for grp, (pa_, pb_, c0) in enumerate([(B4, B5, 0), (B6, B7, 512), (B4, B5, 1024)]):
            for h in range(8):
                bank = pa_ if h < 4 else pb_
                proj(bank[0:64, (h % 4) * 128:(h % 4 + 1) * 128], c0 + h * 64, 64)
            P.copy(z[:, grp * 8:grp * 8 + 4, :], v4(pa_), eng='act')
            P.copy(z[:, grp * 8 + 4:grp * 8 + 8, :], v4(pb_), eng='act')
        for i in range(2):
            proj(B6[0:64, i * 128:(i + 1) * 128], 1536 + i * 64, 64)
        proj(B6[:, 256:384], 1664, 128)
        proj(B7[0:32, 0:128], 1792, 32)
        P.copy(z[:, 24:26, :], B6[0:64, 0:256].rearrange("p (c t) -> p c t", c=2), eng='act')
        P.copy(zg[:, 0, :], B6[:, 256:384], eng='act')
        P.copy(zg[0:32, 1, :], B7[0:32, 0:128], eng='act')

    emit_proj(0)
    for tt in range(NT):
        ts_ = slice(tt * 128, (tt + 1) * 128)
        P.act(txw[:], z[:, 24, :], AF.Tanh)
        P.copy(xab[:], z[:, 25, :], eng='act')
        P.act(sxg[:], zg[:], AF.Sigmoid)
        for h in range(8):
            P.mm(T01[0:64, h * 128:(h + 1) * 128], dup[:, h * 64:(h + 1) * 64], txw[:])
            P.mm(T23[0:64, h * 128:(h + 1) * 128], aup[:, h * 64:(h + 1) * 64], xab[:])
        P.tt(Ta[:], v8(T01), b8(w0), ALU.add)
        P.act(Ta[:], Ta[:], AF.Sigmoid)
        P.tt(Tb[:], v8(T23), b8(a0), ALU.add)
        P.act(Tb[:], Tb[:], AF.Sigmoid)
        P.scan(Tc[:].rearrange("p h t -> p (h t)"), smask[:], Ta[:].rearrange("p h t -> p (h t)"), 0.0, ALU.mult, ALU.add)
        P.act(Td[:], Tc[:], AF.Exp, scale=-0.6065306597126334)
        P.act(Te[:], Tc[:], AF.Exp, scale=0.6065306597126334)
        P.tt(Ta[:], Tc[:], Ta[:], ALU.subtract)
        P.act(Ta[:], Ta[:], AF.Exp, scale=-0.6065306597126334)
        P.copy(gT[:], Td[:].rearrange("p h (c t) -> p h c t", c=2)[:, :, :, 63], eng='act')
        P.tt(Tf[:], k_, b8(kkw), ALU.mult)
        P.act(Tg[:], Tf[:], AF.Square)
        P.mm(T01[0:64, 0:512], self.ones[0:64, 0:64], Tg[:, 0:4, :])
        P.mm(T01[0:64, 512:1024], self.ones[0:64, 0:64], Tg[:, 4:8, :])
        P.act(Tg[:], v8(T01), AF.Sqrt)
        P.ts(Tg[:], Tg[:], 1e-12, ALU.max)
        P.recip(Tg[:], Tg[:])
        P.tt(Tf[:], Tf[:], Tg[:], ALU.mult)
        P.stt(Tg[:], Tb[:], -1.0, b8(kaw), ALU.add, ALU.mult)
        P.stt(Tg[:], Tg[:], 1.0, k_, ALU.add, ALU.mult)
        P.tt(Th[:], r_, Tg[:], ALU.mult)
        P.tt(Th[:], Th[:], b8(rkw), ALU.mult)
        r4 = lambda a: a.rearrange("p h (c t) -> p h c t", c=2)
        P.tt(AR[:, :, :, 1, :], r4(r_), r4(Td[:]), ALU.mult)
        P.stt(AR[:, :, :, 0, :], r4(Tf[:]), -1.0, r4(Ta[:]), ALU.mult, ALU.mult)
        P.tt(Kh, Tg[:], Te[:], ALU.mult)
        P.tt(Tf[:], Tf[:], Tb[:], ALU.mult)
        P.tt(Bh, Tf[:], Te[:], ALU.mult)
        if l == 0:
            P.dma('sp', vfirst[tt], vT, track_dram=False)
        else:
            vf = Ta
            vm = Tb
            P.dma('sp', vf[:], vfirst[tt], track_dram=False)
            for kc in range(8):
                P.mm(B5[0:32, 0:128], wdown[:, kc, :], hT[:, kc, ts_], start=(kc == 0), stop=(kc == 7))
            P.copy(tb[:], B5[0:32, 0:128], eng='act')
            for h in range(8):
                P.mm(T23[0:64, h * 128:(h + 1) * 128], wup[:, h * 64:(h + 1) * 64], tb[:])
            P.tt(vm[:], v8(T23), b8(v0w), ALU.add)
            P.act(vm[:], vm[:], AF.Sigmoid)
            P.tt(vf[:], vf[:], vT, ALU.subtract)
            P.tt(vf[:], vf[:], vm[:], ALU.mult)
            P.tt(vT, vT, vf[:], ALU.add)
        for Xc in range(2):
            for h in range(8):
                P.tr(T23[0:64, (Xc * 8 + h) * 64:(Xc * 8 + h + 1) * 64], vT[:, h, Xc * 64:(Xc + 1) * 64], I64)
        P.copy(Vc[:].rearrange("p c h i -> p (c h i)"), T23[0:64, :], eng='act')
        P.copy(Vcb[:].rearrange("p c h i -> p (c h i)"), T23[0:64, :], eng='dve')
        P.copy(Khb[:], Kh, eng='act')
        P.copy(Bhb[:], Bh, eng='act')
        for Xc in range(2):
            for h in range(8):
                P.tr(T01[0:64, (Xc * 8 + h) * 64:(Xc * 8 + h + 1) * 64], Kh[:, h, Xc * 64:(Xc + 1) * 64], I64)
        P.copy(KTb[:].rearrange("p c h i -> p (c h i)"), T01[0:64, :], eng='act')
        for Xc in range(2):
            for h in range(8):
                P.tr(T23[0:64, (Xc * 8 + h) * 64:(Xc * 8 + h + 1) * 64], Bh[:, h, Xc * 64:(Xc + 1) * 64], I64)
        P.copy(BTb[:].rearrange("p c h i -> p (c h i)"), T23[0:64, :], eng='dve')
        I8 = I64.unsqueeze(1).broadcast_to([64, 8, 64])
        for Xc in range(2):
            cs = slice(Xc * 64, (Xc + 1) * 64)
            pS1 = v8(T01); pS2 = v8(T23); pQ0 = v8(B4, 0, 64)
            for h in range(8):
                ar = AR[:, h, Xc, :, :].rearrange("p a t -> p (a t)")
                P.mm(pS1[:, h, :], Bhb[:, h, cs], ar)
                P.mm(pS2[:, h, :], Khb[:, h, cs], ar)
                P.mm(pQ0[:, h, :], AR[:, h, Xc, 0, :], Bhb[:, h, cs])
            P.tt(M1s[Xc][:], pS1, m1[:].unsqueeze(1).broadcast_to([64, 8, 128]), ALU.mult)
            P.tt(M2s[Xc][:], pS2, m1[:].unsqueeze(1).broadcast_to([64, 8, 128]), ALU.mult)
            P.tt(Qs[Xc][:], pQ0, SL[:].unsqueeze(1).broadcast_to([64, 8, 64]), ALU.mult)
            P.tt(XTs[Xc][:], M1s[Xc][:, :, 0:64], I8, ALU.add)
            P.tt(Xbs[Xc][:], Qs[Xc][:], I8, ALU.add)
        bankP = [v8(B4, 0, 64), v8(T01, 0, 64)]
        bankQ = [v8(B5, 0, 64), v8(T01, 512, 64)]
        bankX1 = [v8(B6, 0, 64), v8(T23, 0, 64)]
        bankX2 = [v8(B7, 0, 64), v8(T23, 512, 64)]
        for kq in range(1, 6):
            last = (kq == 5)
            for Xc in range(2):
                Pm = M1s[Xc][:, :, 0:64]
                for h in range(8):
                    P.mm(bankP[Xc][:, h, :], Qs[Xc][:, h, :], Pm[:, h, :])
                if not last:
                    for h in range(8):
                        P.mm(bankQ[Xc][:, h, :], Pm[:, h, :], Qs[Xc][:, h, :])
            for Xc in range(2):
                Pm = M1s[Xc][:, :, 0:64]
                P.copy(Pm, bankP[Xc], eng='act')
                if not last:
                    P.copy(Qs[Xc][:], bankQ[Xc], eng='act')
            for Xc in range(2):
                Pm = M1s[Xc][:, :, 0:64]
                for h in range(8):
                    P.mm(bankX1[Xc][:, h, :], Xbs[Xc][:, h, :], Pm[:, h, :])
                if not last:
                    for h in range(8):
                        P.mm(bankX2[Xc][:, h, :], Pm[:, h, :], Xbs[Xc][:, h, :])
            for Xc in range(2):
                P.tt(XTs[Xc][:], XTs[Xc][:], bankX1[Xc], ALU.add)
                if not last:
                    P.tt(Xbs[Xc][:], Xbs[Xc][:], bankX2[Xc], ALU.add)
        for Xc in range(2):
            P.copy(XTb[Xc][:], XTs[Xc][:], eng='act')
        for Xc in range(2):
            cs = slice(Xc * 64, (Xc + 1) * 64)
            M1 = M1s[Xc]; M2 = M2s[Xc]
            if Xc == 0:
                pW = v8(B4, 0, 64); pU = v8(B5, 0, 64); pY = v8(B6, 0, 64); pSt = v8(B7, 0, 64)
                gbank = B4; obank = B5
            else:
                pW = v8(T01, 0, 64); pU = v8(T01, 512, 64); pY = v8(T23, 0, 64); pSt = v8(T23, 512, 64)
                gbank = T01[:, 0:512]; obank = T01[:, 512:1024]
            for h in range(8):
                P.mm(pW[:, h, :], M2[:, h, 0:64], Vcb[:, Xc, h, :], start=True, stop=False)
                P.mm(pW[:, h, :], AR[:, h, Xc, 0, :], S0Tb[:, h, :], start=False, stop=True)
            P.copy(Wsb[:], pW, eng='act')
            for h in range(8):
                P.mm(pU[:, h, :], XTb[Xc][:, h, :], Wsb[:, h, :])
            P.copy(Usb[:], pU, eng='act')
            for h in range(8):
                P.mm(pY[:, h, :], AR[:, h, Xc, 1, :], S0Tb[:, h, :], start=True, stop=False)
                P.mm(pY[:, h, :], M2[:, h, 64:128], Vcb[:, Xc, h, :], start=False, stop=False)
                P.mm(pY[:, h, :], M1[:, h, 64:128], Usb[:, h, :], start=False, stop=True)
            for h in range(8):
                P.mm(pSt[:, h, :], KTb[:, Xc, h, :], Vcb[:, Xc, h, :], start=True, stop=False)
                P.mm(pSt[:, h, :], BTb[:, Xc, h, :], Usb[:, h, :], start=False, stop=True)
            P.reduce(st8[:], pY, ALU.add)
            P.ts(st8[:], st8[:], -1.0 / 64, ALU.mult)
            P.tt(Y1[:], pY, st8[:].unsqueeze(2).broadcast_to([64, 8, 64]), ALU.add)
            P.act(Y2[:], Y1[:], AF.Square)
            P.reduce(st8b[:], Y2[:], ALU.add)
            P.act(st8b[:], st8b[:], AF.Sqrt, bias=64e-5, scale=1.0 / 64)
            P.recip(st8b[:], st8b[:])
            P.tt(Y1[:], Y1[:], st8b[:].unsqueeze(2).broadcast_to([64, 8, 64]), ALU.mult)
            Y1f = Y1[:].rearrange("p h i -> p (h i)")
            P.tt(Y1f, Y1f, lnw[:], ALU.mult)
            P.tt(Y1f, Y1f, lnb[:], ALU.add)
            P.tt(S0T[:], S0T[:], pSt, ALU.add)
            P.tt(S0T[:], S0T[:], gT[:, :, Xc:Xc + 1].broadcast_to([64, 8, 64]), ALU.mult)
            P.copy(S0Tb[:], S0T[:], eng='act')
            for h in range(8):
                P.mm(obank[0:64, h:h + 1], Th[:, h, cs], self.ones[0:64, 0:1])
            P.copy(sbon[:], obank[0:64, 0:8], eng='act')
            P.tt(Y2[:], Vc[:, Xc, :, :], sbon[:].unsqueeze(2).broadcast_to([64, 8, 64]), ALU.mult)
            P.tt(Y1[:], Y1[:], Y2[:], ALU.add)
            P.mm(gbank[0:64, :], sxg[:, 0, cs], gu1[:], start=True, stop=False)
            P.mm(gbank[0:64, :], sxg[0:32, 1, cs], gu2[:], start=False, stop=True)
            P.tt(obf[:], Y1f, gbank[0:64, :], ALU.mult)
            if Xc == 1 and tt + 1 < NT:
                emit_proj(tt + 1)
            for c in range(4):
                P.tr(obank[:, 256 + c * 64:256 + (c + 1) * 64], obf[:, c * 128:(c + 1) * 128], I64)
            st = self.ostage[self.ostage_i % 2]
            P.copy(st[:, :, cs], obank[:, 256:512].rearrange("p (c t) -> p c t", c=4), eng='act')
            if Xc == 1:
                self.ostage_i += 1
                P.dma('sp', oT[tt], st[:], track_dram=False)
    P.end_phase()


K.rwkv = rwkv4


def rwkv6(self, l, hT, oT, vfirst):
    P = self.P
    I = self.inp
    w_in = I['w_in']
    P.begin_phase()
    wa = P.sb('wa_rwkv', [128, 8, 1824], BF16)
    wb = P.sb('wb_rwkv', [128, 8, 1824], BF16)
    P.begin_phase()
    w = P.sb('w_rwkv', [128, 8, 1824], BF16)
    self.load_w(w[:], w_in[l, :, 0:1824])
    mub = P.sb('mub', [128, 1824]); omub = P.sb('omub', [128, 1824])
    P.dma('sp', mub[:], I['rwkv_mu'][l].partition_broadcast(128))
    P.ts(omub[:], mub[:], -1.0, ALU.mult, 1.0, ALU.add)
    for kc in range(8):
        P.tt(wa[:, kc, :], w[:, kc, :], omub[:], ALU.mult, eng='dve')
        P.tt(wb[:, kc, :], w[:, kc, :], mub[:], ALU.mult, eng='dve')
    P.end_phase()
    dup = P.sb('dup', [64, 512], BF16); aup = P.sb('aup', [64, 512], BF16)
    P.dma('pool', dup[:], I['rwkv_decay_up'][l]); P.dma('pool', aup[:], I['rwkv_a_up'][l])
    gu1 = P.sb('gu1', [128, 512], BF16); gu2 = P.sb('gu2', [32, 512], BF16)
    P.dma('pool', gu1[:], I['rwkv_gate_up'][l, 0:128, :]); P.dma('pool', gu2[:], I['rwkv_gate_up'][l, 128:160, :])
    rw = P.sb('rw', [64, 6, 8])
    P.dma('sp', rw[:], I['h_rw'][l])
    w0, a0, kkw, kaw, rkw, v0w = [rw[:, i, :] for i in range(6)]
    lnw = P.sb('lnw', [64, 512]); lnb = P.sb('lnb', [64, 512])
    P.dma('sp', lnw[:], I['rwkv_ln_w'][l].partition_broadcast(64)); P.dma('sp', lnb[:], I['rwkv_ln_b'][l].partition_broadcast(64))
    m1 = P.sb('m1', [64, 128]); SL = P.sb('SL', [64, 64]); smask = P.sb('smask', [64, 1024])
    P.dma('sp', m1[:], I['c_m1']); P.dma('sp', SL[:], I['c_sl']); P.dma('sp', smask[:], I['c_scanmask'])
    if l > 0:
        wdown = P.sb('wdown', [128, 8, 32], BF16); wup = P.sb('wup', [32, 512], BF16)
        self.load_w(wdown[:], I['vres_down'][l - 1]); P.dma('pool', wup[:], I['vres_up'][l - 1])
        tb = P.sb('tb', [32, 128], BF16)
    I64 = self.ident[0:64, 0:64]
    T01 = P.ps('T01', [128, 1024]); T23 = P.ps('T23', [128, 1024])
    B4 = P.ps('B4', [128, 512]); B5 = P.ps('B5', [128, 512]); B6 = P.ps('B6', [128, 512]); B7 = P.ps('B7', [128, 512])
    def v8(t, c0=0, n=128):
        return t[0:64, c0:c0 + 8 * n].rearrange("p (h t) -> p h t", h=8)
    z = P.sb('z', [64, 26, 128]); zg = P.sb('zg', [128, 2, 128])
    txw = P.sb('txw', [64, 128], BF16); xab = P.sb('xab', [64, 128], BF16); sxg = P.sb('sxg', [128, 2, 128], BF16)
    Ta = P.sb('Ta', [64, 8, 128]); Tb = P.sb('Tb', [64, 8, 128]); Tc = P.sb('Tc', [64, 8, 128]); Td = P.sb('Td', [64, 8, 128])
    Te = P.sb('Te', [64, 8, 128]); Tf = P.sb('Tf', [64, 8, 128]); Tg = P.sb('Tg', [64, 8, 128]); Th = P.sb('Th', [64, 8, 128])
    AR = P.sb('AR', [64, 8, 2, 2, 64], BF16); gT = P.sb('gT', [64, 8, 2])
    Vc = P.sb('Vc', [64, 2, 8, 64]); Vcb = P.sb('Vcb', [64, 2, 8, 64], BF16)
    Khb = P.sb('Khb', [64, 8, 128], BF16); Bhb = P.sb('Bhb', [64, 8, 128], BF16)
    KTb = P.sb('KTb', [64, 2, 8, 64], BF16); BTb = P.sb('BTb', [64, 2, 8, 64], BF16)
    M1s = [P.sb('M1_%d' % i, [64, 8, 128], BF16) for i in range(2)]
    M2s = [P.sb('M2_%d' % i, [64, 8, 128], BF16) for i in range(2)]
    Qs = [P.sb('Q_%d' % i, [64, 8, 64], BF16) for i in range(2)]
    Xbs = [P.sb('Xb_%d' % i, [64, 8, 64], BF16) for i in range(2)]
    XTs = [P.sb('XT_%d' % i, [64, 8, 64]) for i in range(2)]
    XTb = [P.sb('XTb_%d' % i, [64, 8, 64], BF16) for i in range(2)]
    Wsb = P.sb('Wsb', [64, 8, 64], BF16); Usb = P.sb('Usb', [64, 8, 64], BF16)
    S0T = P.sb('S0T', [64, 8, 64]); S0Tb = P.sb('S0Tb', [64, 8, 64], BF16)
    Y1 = P.sb('Y1', [64, 8, 64]); Y2 = P.sb('Y2', [64, 8, 64]); obf = P.sb('obf', [64, 512])
    st8 = P.sb('st8', [64, 8]); st8b = P.sb('st8b', [64, 8]); sbon = P.sb('sbon', [64, 8])
    P.memset(zg[:], 0.0); P.memset(S0T[:], 0.0); P.memset(S0Tb[:], 0.0)
    r_ = z[:, 0:8, :]; k_ = z[:, 8:16, :]; vT = z[:, 16:24, :]
    Kh = z[:, 8:16, :]
    Bh = z[:, 0:8, :]
    b8 = lambda a: a.unsqueeze(2).broadcast_to([64, 8, 128])
    def v4(t):
        return t[0:64, 0:512].rearrange("p (h t) -> p h t", h=4)

    def emit_proj(tt, groups=(0, 1, 2, 3)):
        ts_ = slice(tt * 128, (tt + 1) * 128)

        def proj(out, c0, m):
            for kc in range(8):
                P.mm(out, wa[:, kc, c0:c0 + m], hT[:, kc, ts_], start=(kc == 0), stop=False)
            if tt == 0:
                for kc in range(8):
                    P.mm(out[:, 1:128], wb[:, kc, c0:c0 + m], hT[:, kc, 0:127], start=False, stop=(kc == 7))
            else:
                for kc in range(8):
                    P.mm(out, wb[:, kc, c0:c0 + m], hT[:, kc, tt * 128 - 1:(tt + 1) * 128 - 1], start=False, stop=(kc == 7))
        for grp, (pa_, pb_, c0) in enumerate([(B4, B5, 0), (B6, B7, 512), (B4, B5, 1024)]):
            if grp not in groups:
                continue
            for h in range(8):
                bank = pa_ if h < 4 else pb_
                proj(bank[0:64, (h % 4) * 128:(h % 4 + 1) * 128], c0 + h * 64, 64)
            P.copy(z[:, grp * 8:grp * 8 + 4, :], v4(pa_), eng='act')
            P.copy(z[:, grp * 8 + 4:grp * 8 + 8, :], v4(pb_), eng='act')
        if 3 in groups:
            for i in range(2):
                proj(B6[0:64, i * 128:(i + 1) * 128], 1536 + i * 64, 64)
            proj(B6[:, 256:384], 1664, 128)
            proj(B7[0:32, 0:128], 1792, 32)
            P.copy(z[:, 24:26, :], B6[0:64, 0:256].rearrange("p (c t) -> p c t", c=2), eng='act')
            P.copy(zg[:, 0, :], B6[:, 256:384], eng='act')
            P.copy(zg[0:32, 1, :], B7[0:32, 0:128], eng='act')

    emit_proj(0)
    for tt in range(NT):
        ts_ = slice(tt * 128, (tt + 1) * 128)
        P.act(txw[:], z[:, 24, :], AF.Tanh)
        P.copy(xab[:], z[:, 25, :], eng='act')
        P.act(sxg[:], zg[:], AF.Sigmoid)
        for h in range(8):
            P.mm(T01[0:64, h * 128:(h + 1) * 128], dup[:, h * 64:(h + 1) * 64], txw[:])
            P.mm(T23[0:64, h * 128:(h + 1) * 128], aup[:, h * 64:(h + 1) * 64], xab[:])
        P.tt(Ta[:], v8(T01), b8(w0), ALU.add)
        P.act(Ta[:], Ta[:], AF.Sigmoid)
        P.tt(Tb[:], v8(T23), b8(a0), ALU.add)
        P.act(Tb[:], Tb[:], AF.Sigmoid)
        P.scan(Tc[:].rearrange("p h t -> p (h t)"), smask[:], Ta[:].rearrange("p h t -> p (h t)"), 0.0, ALU.mult, ALU.add)
        P.act(Td[:], Tc[:], AF.Exp, scale=-0.6065306597126334)
        P.act(Te[:], Tc[:], AF.Exp, scale=0.6065306597126334)
        P.tt(Ta[:], Tc[:], Ta[:], ALU.subtract)
        P.act(Ta[:], Ta[:], AF.Exp, scale=-0.6065306597126334)
        P.copy(gT[:], Td[:].rearrange("p h (c t) -> p h c t", c=2)[:, :, :, 63], eng='act')
        P.tt(Tf[:], k_, b8(kkw), ALU.mult)
        P.act(Tg[:], Tf[:], AF.Square)
        P.mm(T01[0:64, 0:512], self.ones[0:64, 0:64], Tg[:, 0:4, :])
        P.mm(T01[0:64, 512:1024], self.ones[0:64, 0:64], Tg[:, 4:8, :])
        P.act(Tg[:], v8(T01), AF.Sqrt)
        P.ts(Tg[:], Tg[:], 1e-12, ALU.max)
        P.recip(Tg[:], Tg[:])
        P.tt(Tf[:], Tf[:], Tg[:], ALU.mult)
        P.stt(Tg[:], Tb[:], -1.0, b8(kaw), ALU.add, ALU.mult)
        P.stt(Tg[:], Tg[:], 1.0, k_, ALU.add, ALU.mult)
        P.tt(Th[:], r_, Tg[:], ALU.mult)
        P.tt(Th[:], Th[:], b8(rkw), ALU.mult)
        r4 = lambda a: a.rearrange("p h (c t) -> p h c t", c=2)
        P.tt(AR[:, :, :, 1, :], r4(r_), r4(Td[:]), ALU.mult)
        P.stt(AR[:, :, :, 0, :], r4(Tf[:]), -1.0, r4(Ta[:]), ALU.mult, ALU.mult)
        P.tt(Kh, Tg[:], Te[:], ALU.mult)
        P.tt(Tf[:], Tf[:], Tb[:], ALU.mult)
        P.tt(Bh, Tf[:], Te[:], ALU.mult)
        if l == 0:
            P.dma('sp', vfirst[tt], vT, track_dram=False)
        else:
            vf = Ta
            vm = Tb
            P.dma('sp', vf[:], vfirst[tt], track_dram=False)
            for kc in range(8):
                P.mm(B5[0:32, 0:128], wdown[:, kc, :], hT[:, kc, ts_], start=(kc == 0), stop=(kc == 7))
            P.copy(tb[:], B5[0:32, 0:128], eng='act')
            for h in range(8):
                P.mm(T23[0:64, h * 128:(h + 1) * 128], wup[:, h * 64:(h + 1) * 64], tb[:])
            P.tt(vm[:], v8(T23), b8(v0w), ALU.add)
            P.act(vm[:], vm[:], AF.Sigmoid)
            P.tt(vf[:], vf[:], vT, ALU.subtract)
            P.tt(vf[:], vf[:], vm[:], ALU.mult)
            P.tt(vT, vT, vf[:], ALU.add)
        for Xc in range(2):
            for h in range(8):
                P.tr(T23[0:64, (Xc * 8 + h) * 64:(Xc * 8 + h + 1) * 64], vT[:, h, Xc * 64:(Xc + 1) * 64], I64)
        P.copy(Vc[:].rearrange("p c h i -> p (c h i)"), T23[0:64, :], eng='act')
        P.copy(Vcb[:].rearrange("p c h i -> p (c h i)"), T23[0:64, :], eng='dve')
        P.copy(Khb[:], Kh, eng='act')
        P.copy(Bhb[:], Bh, eng='act')
        for Xc in range(2):
            for h in range(8):
                P.tr(T01[0:64, (Xc * 8 + h) * 64:(Xc * 8 + h + 1) * 64], Kh[:, h, Xc * 64:(Xc + 1) * 64], I64)
        P.copy(KTb[:].rearrange("p c h i -> p (c h i)"), T01[0:64, :], eng='act')
        for Xc in range(2):
            for h in range(8):
                P.tr(T23[0:64, (Xc * 8 + h) * 64:(Xc * 8 + h + 1) * 64], Bh[:, h, Xc * 64:(Xc + 1) * 64], I64)
        P.copy(BTb[:].rearrange("p c h i -> p (c h i)"), T23[0:64, :], eng='dve')
        I8 = I64.unsqueeze(1).broadcast_to([64, 8, 64])
        for Xc in range(2):
            cs = slice(Xc * 64, (Xc + 1) * 64)
            pS1 = v8(T01); pS2 = v8(T23); pQ0 = v8(B4, 0, 64)
            for h in range(8):
                ar = AR[:, h, Xc, :, :].rearrange("p a t -> p (a t)")
                P.mm(pS1[:, h, :], Bhb[:, h, cs], ar)
                P.mm(pS2[:, h, :], Khb[:, h, cs], ar)
                P.mm(pQ0[:, h, :], AR[:, h, Xc, 0, :], Bhb[:, h, cs])
            P.tt(M1s[Xc][:], pS1, m1[:].unsqueeze(1).broadcast_to([64, 8, 128]), ALU.mult)
            P.tt(M2s[Xc][:], pS2, m1[:].unsqueeze(1).broadcast_to([64, 8, 128]), ALU.mult)
            P.tt(Qs[Xc][:], pQ0, SL[:].unsqueeze(1).broadcast_to([64, 8, 64]), ALU.mult)
            P.tt(XTs[Xc][:], M1s[Xc][:, :, 0:64], I8, ALU.add)
            P.tt(Xbs[Xc][:], Qs[Xc][:], I8, ALU.add)
        bankP = [v8(B4, 0, 64), v8(T01, 0, 64)]
        bankQ = [v8(B5, 0, 64), v8(T01, 512, 64)]
        bankX1 = [v8(B6, 0, 64), v8(T23, 0, 64)]
        bankX2 = [v8(B7, 0, 64), v8(T23, 512, 64)]
        for kq in range(1, 6):
            last = (kq == 5)
            for Xc in range(2):
                Pm = M1s[Xc][:, :, 0:64]
                for h in range(8):
                    P.mm(bankP[Xc][:, h, :], Qs[Xc][:, h, :], Pm[:, h, :])
                if not last:
                    for h in range(8):
                        P.mm(bankQ[Xc][:, h, :], Pm[:, h, :], Qs[Xc][:, h, :])
            for Xc in range(2):
                Pm = M1s[Xc][:, :, 0:64]
                P.copy(Pm, bankP[Xc], eng='act')
                if not last:
                    P.copy(Qs[Xc][:], bankQ[Xc], eng='act')
            for Xc in range(2):
                Pm = M1s[Xc][:, :, 0:64]
                for h in range(8):
                    P.mm(bankX1[Xc][:, h, :], Xbs[Xc][:, h, :], Pm[:, h, :])
                if not last:
                    for h in range(8):
                        P.mm(bankX2[Xc][:, h, :], Pm[:, h, :], Xbs[Xc][:, h, :])
            for Xc in range(2):
                P.tt(XTs[Xc][:], XTs[Xc][:], bankX1[Xc], ALU.add)
                if not last:
                    P.tt(Xbs[Xc][:], Xbs[Xc][:], bankX2[Xc], ALU.add)
        for Xc in range(2):
            P.copy(XTb[Xc][:], XTs[Xc][:], eng='act')
        for Xc in range(2):
            cs = slice(Xc * 64, (Xc + 1) * 64)
            M1 = M1s[Xc]; M2 = M2s[Xc]
            if Xc == 0:
                pW = v8(B4, 0, 64); pU = v8(B5, 0, 64); pY = v8(B6, 0, 64); pSt = v8(B7, 0, 64)
                gbank = B4; obank = B5
            else:
                pW = v8(T01, 0, 64); pU = v8(T01, 512, 64); pY = v8(T23, 0, 64); pSt = v8(T23, 512, 64)
                gbank = T01[:, 0:512]; obank = T01[:, 512:1024]
            for h in range(8):
                P.mm(pW[:, h, :], M2[:, h, 0:64], Vcb[:, Xc, h, :], start=True, stop=False)
                P.mm(pW[:, h, :], AR[:, h, Xc, 0, :], S0Tb[:, h, :], start=False, stop=True)
            P.copy(Wsb[:], pW, eng='act')
            for h in range(8):
                P.mm(pU[:, h, :], XTb[Xc][:, h, :], Wsb[:, h, :])
            P.copy(Usb[:], pU, eng='act')
            for h in range(8):
                P.mm(pY[:, h, :], AR[:, h, Xc, 1, :], S0Tb[:, h, :], start=True, stop=False)
                P.mm(pY[:, h, :], M2[:, h, 64:128], Vcb[:, Xc, h, :], start=False, stop=False)
                P.mm(pY[:, h, :], M1[:, h, 64:128], Usb[:, h, :], start=False, stop=True)
            for h in range(8):
                P.mm(pSt[:, h, :], KTb[:, Xc, h, :], Vcb[:, Xc, h, :], start=True, stop=False)
                P.mm(pSt[:, h, :], BTb[:, Xc, h, :], Usb[:, h, :], start=False, stop=True)
            P.reduce(st8[:], pY, ALU.add)
            P.ts(st8[:], st8[:], -1.0 / 64, ALU.mult)
            P.tt(Y1[:], pY, st8[:].unsqueeze(2).broadcast_to([64, 8, 64]), ALU.add)
            P.act(Y2[:], Y1[:], AF.Square)
            P.reduce(st8b[:], Y2[:], ALU.add)
            P.act(st8b[:], st8b[:], AF.Sqrt, bias=64e-5, scale=1.0 / 64)
            P.recip(st8b[:], st8b[:])
            P.tt(Y1[:], Y1[:], st8b[:].unsqueeze(2).broadcast_to([64, 8, 64]), ALU.mult)
            Y1f = Y1[:].rearrange("p h i -> p (h i)")
            P.tt(Y1f, Y1f, lnw[:], ALU.mult)
            P.tt(Y1f, Y1f, lnb[:], ALU.add)
            P.tt(S0T[:], S0T[:], pSt, ALU.add)
            P.tt(S0T[:], S0T[:], gT[:, :, Xc:Xc + 1].broadcast_to([64, 8, 64]), ALU.mult)
            P.copy(S0Tb[:], S0T[:], eng='act')
            for h in range(8):
                P.mm(obank[0:64, h:h + 1], Th[:, h, cs], self.ones[0:64, 0:1])
            P.copy(sbon[:], obank[0:64, 0:8], eng='act')
            P.tt(Y2[:], Vc[:, Xc, :, :], sbon[:].unsqueeze(2).broadcast_to([64, 8, 64]), ALU.mult)
            P.tt(Y1[:], Y1[:], Y2[:], ALU.add)
            P.mm(gbank[0:64, :], sxg[:, 0, cs], gu1[:], start=True, stop=False)
            P.mm(gbank[0:64, :], sxg[0:32, 1, cs], gu2[:], start=False, stop=True)
            P.tt(obf[:], Y1f, gbank[0:64, :], ALU.mult)
            if tt + 1 < NT:
                emit_proj(tt + 1, groups=(0, 1) if Xc == 0 else (2, 3))
            for c in range(4):
                P.tr(obank[:, 256 + c * 64:256 + (c + 1) * 64], obf[:, c * 128:(c + 1) * 128], I64)
            st = self.ostage[self.ostage_i % 2]
            P.copy(st[:, :, cs], obank[:, 256:512].rearrange("p (c t) -> p c t", c=4), eng='act')
            if Xc == 1:
                self.ostage_i += 1
                P.dma('sp', oT[tt], st[:], track_dram=False)
    P.end_phase()


K.rwkv = rwkv6


def rwkv5(self, l, hT, oT, vfirst):
    P = self.P
    I = self.inp
    w_in = I['w_in']
    P.begin_phase()
    wa = P.sb('wa_rwkv', [128, 8, 1824], BF16)
    wb = P.sb('wb_rwkv', [128, 8, 1824], BF16)
    P.begin_phase()
    w = P.sb('w_rwkv', [128, 8, 1824], BF16)
    self.load_w(w[:], w_in[l, :, 0:1824])
    mub = P.sb('mub', [128, 1824]); omub = P.sb('omub', [128, 1824])
    P.dma('sp', mub[:], I['rwkv_mu'][l].partition_broadcast(128))
    P.ts(omub[:], mub[:], -1.0, ALU.mult, 1.0, ALU.add)
    for kc in range(8):
        P.tt(wa[:, kc, :], w[:, kc, :], omub[:], ALU.mult, eng='dve')
        P.tt(wb[:, kc, :], w[:, kc, :], mub[:], ALU.mult, eng='dve')
    P.end_phase()
    dup = P.sb('dup', [64, 512], BF16); aup = P.sb('aup', [64, 512], BF16)
    P.dma('pool', dup[:], I['rwkv_decay_up'][l]); P.dma('pool', aup[:], I['rwkv_a_up'][l])
    gu1 = P.sb('gu1', [128, 512], BF16); gu2 = P.sb('gu2', [32, 512], BF16)
    P.dma('pool', gu1[:], I['rwkv_gate_up'][l, 0:128, :]); P.dma('pool', gu2[:], I['rwkv_gate_up'][l, 128:160, :])
    rw = P.sb('rw', [64, 6, 8])
    P.dma('sp', rw[:], I['h_rw'][l])
    w0, a0, kkw, kaw, rkw, v0w = [rw[:, i, :] for i in range(6)]
    lnw = P.sb('lnw', [64, 512]); lnb = P.sb('lnb', [64, 512])
    P.dma('sp', lnw[:], I['rwkv_ln_w'][l].partition_broadcast(64)); P.dma('sp', lnb[:], I['rwkv_ln_b'][l].partition_broadcast(64))
    m1 = P.sb('m1', [64, 128]); SL = P.sb('SL', [64, 64]); smask = P.sb('smask', [64, 1024])
    P.dma('sp', m1[:], I['c_m1']); P.dma('sp', SL[:], I['c_sl']); P.dma('sp', smask[:], I['c_scanmask'])
    if l > 0:
        wdown = P.sb('wdown', [128, 8, 32], BF16); wup = P.sb('wup', [32, 512], BF16)
        self.load_w(wdown[:], I['vres_down'][l - 1]); P.dma('pool', wup[:], I['vres_up'][l - 1])
        tb = P.sb('tb', [32, 128], BF16)
    I64 = self.ident[0:64, 0:64]
    T01 = P.ps('T01', [128, 1024]); T23 = P.ps('T23', [128, 1024])
    B4 = P.ps('B4', [128, 512]); B5 = P.ps('B5', [128, 512]); B6 = P.ps('B6', [128, 512]); B7 = P.ps('B7', [128, 512])
    def v8(t, c0=0, n=128):
        return t[0:64, c0:c0 + 8 * n].rearrange("p (h t) -> p h t", h=8)
    zbuf = [P.sb('z%d' % i, [64, 26, 128]) for i in range(2)]
    zgbuf = [P.sb('zg%d' % i, [128, 2, 128]) for i in range(2)]
    txw = P.sb('txw', [64, 128], BF16); xab = P.sb('xab', [64, 128], BF16); sxg = P.sb('sxg', [128, 2, 128], BF16)
    Ta = P.sb('Ta', [64, 8, 128]); Tb = P.sb('Tb', [64, 8, 128]); Tc = P.sb('Tc', [64, 8, 128]); Td = P.sb('Td', [64, 8, 128])
    Te = P.sb('Te', [64, 8, 128]); Tf = P.sb('Tf', [64, 8, 128]); Tg = P.sb('Tg', [64, 8, 128]); Th = P.sb('Th', [64, 8, 128])
    AR = P.sb('AR', [64, 8, 2, 2, 64], BF16); gT = P.sb('gT', [64, 8, 2])
    Vc = P.sb('Vc', [64, 2, 8, 64]); Vcb = P.sb('Vcb', [64, 2, 8, 64], BF16)
    Khb = P.sb('Khb', [64, 8, 128], BF16); Bhb = P.sb('Bhb', [64, 8, 128], BF16)
    KTb = P.sb('KTb', [64, 2, 8, 64], BF16); BTb = P.sb('BTb', [64, 2, 8, 64], BF16)
    M1s = [P.sb('M1_%d' % i, [64, 8, 128], BF16) for i in range(2)]
    M2s = [P.sb('M2_%d' % i, [64, 8, 128], BF16) for i in range(2)]
    Qs = [P.sb('Q_%d' % i, [64, 8, 64], BF16) for i in range(2)]
    Xbs = [P.sb('Xb_%d' % i, [64, 8, 64], BF16) for i in range(2)]
    XTb = [P.sb('XTb_%d' % i, [64, 8, 64], BF16) for i in range(2)]
    Wsb = P.sb('Wsb', [64, 8, 64], BF16); Usb = P.sb('Usb', [64, 8, 64], BF16)
    S0T = P.sb('S0T', [64, 8, 64]); S0Tb = P.sb('S0Tb', [64, 8, 64], BF16)
    _fl = lambda t: t[:].rearrange("p h t -> p (h t)")[:, 0:512]
    XTs = [_fl(Td).rearrange("p (h i) -> p h i", h=8), _fl(Te).rearrange("p (h i) -> p h i", h=8)]
    Y1 = _fl(Ta).rearrange("p (h i) -> p h i", h=8); Y2 = _fl(Tb).rearrange("p (h i) -> p h i", h=8); obf = _fl(Tc)
    st8 = P.sb('st8', [64, 8]); st8b = P.sb('st8b', [64, 8]); sbon = P.sb('sbon', [64, 8])
    P.memset(zgbuf[0][:], 0.0); P.memset(zgbuf[1][:], 0.0); P.memset(S0T[:], 0.0); P.memset(S0Tb[:], 0.0)
    b8 = lambda a: a.unsqueeze(2).broadcast_to([64, 8, 128])
    def v4(t):
        return t[0:64, 0:512].rearrange("p (h t) -> p h t", h=4)

    def emit_proj(tt, part):
        ts_ = slice(tt * 128, (tt + 1) * 128)
        z = zbuf[tt % 2]; zg = zgbuf[tt % 2]

        def proj(out, c0, m):
            for kc in range(8):
                P.mm(out, wa[:, kc, c0:c0 + m], hT[:, kc, ts_], start=(kc == 0), stop=False)
            if tt == 0:
                for kc in range(8):
                    P.mm(out[:, 1:128], wb[:, kc, c0:c0 + m], hT[:, kc, 0:127], start=False, stop=(kc == 7))
            else:
                for kc in range(8):
                    P.mm(out, wb[:, kc, c0:c0 + m], hT[:, kc, tt * 128 - 1:(tt + 1) * 128 - 1], start=False, stop=(kc == 7))
        if part < 3:
            grp = part
            pa_, pb_ = (B4, B5) if grp != 1 else (B6, B7)
            c0 = grp * 512
            for h in range(8):
                bank = pa_ if h < 4 else pb_
                proj(bank[0:64, (h % 4) * 128:(h % 4 + 1) * 128], c0 + h * 64, 64)
            P.copy(z[:, grp * 8:grp * 8 + 4, :], v4(pa_), eng='act')
            P.copy(z[:, grp * 8 + 4:grp * 8 + 8, :], v4(pb_), eng='act')
        else:
            for i in range(2):
                proj(B6[0:64, i * 128:(i + 1) * 128], 1536 + i * 64, 64)
            proj(B6[:, 256:384], 1664, 128)
            proj(B7[0:32, 0:128], 1792, 32)
            P.copy(z[:, 24:26, :], B6[0:64, 0:256].rearrange("p (c t) -> p c t", c=2), eng='act')
            P.copy(zg[:, 0, :], B6[:, 256:384], eng='act')
            P.copy(zg[0:32, 1, :], B7[0:32, 0:128], eng='act')

    for part in range(4):
        emit_proj(0, part)
    for tt in range(NT):
        ts_ = slice(tt * 128, (tt + 1) * 128)
        z = zbuf[tt % 2]; zg = zgbuf[tt % 2]
        r_ = z[:, 0:8, :]; k_ = z[:, 8:16, :]; vT = z[:, 16:24, :]
        Kh = z[:, 8:16, :]; Bh = z[:, 0:8, :]
        nxt = (lambda p: emit_proj(tt + 1, p)) if tt + 1 < NT else (lambda p: None)
        P.act(txw[:], z[:, 24, :], AF.Tanh)
        P.copy(xab[:], z[:, 25, :], eng='act')
        P.act(sxg[:], zg[:], AF.Sigmoid)
        for h in range(8):
            P.mm(T01[0:64, h * 128:(h + 1) * 128], dup[:, h * 64:(h + 1) * 64], txw[:])
            P.mm(T23[0:64, h * 128:(h + 1) * 128], aup[:, h * 64:(h + 1) * 64], xab[:])
        nxt(0)
        P.tt(Ta[:], v8(T01), b8(w0), ALU.add)
        P.act(Ta[:], Ta[:], AF.Sigmoid)
        P.tt(Tb[:], v8(T23), b8(a0), ALU.add)
        P.act(Tb[:], Tb[:], AF.Sigmoid)
        P.scan(Tc[:].rearrange("p h t -> p (h t)"), smask[:], Ta[:].rearrange("p h t -> p (h t)"), 0.0, ALU.mult, ALU.add)
        P.act(Td[:], Tc[:], AF.Exp, scale=-0.6065306597126334)
        P.act(Te[:], Tc[:], AF.Exp, scale=0.6065306597126334)
        P.tt(Ta[:], Tc[:], Ta[:], ALU.subtract)
        P.act(Ta[:], Ta[:], AF.Exp, scale=-0.6065306597126334)
        P.copy(gT[:], Td[:].rearrange("p h (c t) -> p h c t", c=2)[:, :, :, 63], eng='act')
        P.tt(Tf[:], k_, b8(kkw), ALU.mult)
        P.act(Tg[:], Tf[:], AF.Square)
        P.mm(T01[0:64, 0:512], self.ones[0:64, 0:64], Tg[:, 0:4, :])
        P.mm(T01[0:64, 512:1024], self.ones[0:64, 0:64], Tg[:, 4:8, :])
        nxt(1)
        P.act(Tg[:], v8(T01), AF.Sqrt)
        P.ts(Tg[:], Tg[:], 1e-12, ALU.max)
        P.recip(Tg[:], Tg[:])
        P.tt(Tf[:], Tf[:], Tg[:], ALU.mult)
        P.stt(Tg[:], Tb[:], -1.0, b8(kaw), ALU.add, ALU.mult)
        P.stt(Tg[:], Tg[:], 1.0, k_, ALU.add, ALU.mult)
        P.tt(Th[:], r_, Tg[:], ALU.mult)
        P.tt(Th[:], Th[:], b8(rkw), ALU.mult)
        r4 = lambda a: a.rearrange("p h (c t) -> p h c t", c=2)
        P.tt(AR[:, :, :, 1, :], r4(r_), r4(Td[:]), ALU.mult)
        P.stt(AR[:, :, :, 0, :], r4(Tf[:]), -1.0, r4(Ta[:]), ALU.mult, ALU.mult)
        P.tt(Kh, Tg[:], Te[:], ALU.mult)
        P.tt(Tf[:], Tf[:], Tb[:], ALU.mult)
        P.tt(Bh, Tf[:], Te[:], ALU.mult)
        if l == 0:
            P.dma('sp', vfirst[tt], vT, track_dram=False)
        else:
            vf = Ta
            vm = Tb
            P.dma('sp', vf[:], vfirst[tt], track_dram=False)
            for kc in range(8):
                P.mm(T01[0:32, 0:128], wdown[:, kc, :], hT[:, kc, ts_], start=(kc == 0), stop=(kc == 7))
            P.copy(tb[:], T01[0:32, 0:128], eng='act')
            for h in range(8):
                P.mm(T23[0:64, h * 128:(h + 1) * 128], wup[:, h * 64:(h + 1) * 64], tb[:])
            P.tt(vm[:], v8(T23), b8(v0w), ALU.add)
            P.act(vm[:], vm[:], AF.Sigmoid)
            P.tt(vf[:], vf[:], vT, ALU.subtract)
            P.tt(vf[:], vf[:], vm[:], ALU.mult)
            P.tt(vT, vT, vf[:], ALU.add)
        nxt(2)
        for Xc in range(2):
            for h in range(8):
                P.tr(T23[0:64, (Xc * 8 + h) * 64:(Xc * 8 + h + 1) * 64], vT[:, h, Xc * 64:(Xc + 1) * 64], I64)
        P.copy(Vc[:].rearrange("p c h i -> p (c h i)"), T23[0:64, :], eng='act')
        P.copy(Vcb[:].rearrange("p c h i -> p (c h i)"), T23[0:64, :], eng='dve')
        P.copy(Khb[:], Kh, eng='act')
        P.copy(Bhb[:], Bh, eng='act')
        for Xc in range(2):
            for h in range(8):
                P.tr(T01[0:64, (Xc * 8 + h) * 64:(Xc * 8 + h + 1) * 64], Kh[:, h, Xc * 64:(Xc + 1) * 64], I64)
        P.copy(KTb[:].rearrange("p c h i -> p (c h i)"), T01[0:64, :], eng='act')
        for Xc in range(2):
            for h in range(8):
                P.tr(T23[0:64, (Xc * 8 + h) * 64:(Xc * 8 + h + 1) * 64], Bh[:, h, Xc * 64:(Xc + 1) * 64], I64)
        P.copy(BTb[:].rearrange("p c h i -> p (c h i)"), T23[0:64, :], eng='dve')
        nxt(3)
        I8 = I64.unsqueeze(1).broadcast_to([64, 8, 64])
        for Xc in range(2):
            cs = slice(Xc * 64, (Xc + 1) * 64)
            pS1 = v8(T01); pS2 = v8(T23); pQ0 = v8(B4, 0, 64)
            for h in range(8):
                ar = AR[:, h, Xc, :, :].rearrange("p a t -> p (a t)")
                P.mm(pS1[:, h, :], Bhb[:, h, cs], ar)
                P.mm(pS2[:, h, :], Khb[:, h, cs], ar)
                P.mm(pQ0[:, h, :], AR[:, h, Xc, 0, :], Bhb[:, h, cs])
            P.tt(M1s[Xc][:], pS1, m1[:].unsqueeze(1).broadcast_to([64, 8, 128]), ALU.mult)
            P.tt(M2s[Xc][:], pS2, m1[:].unsqueeze(1).broadcast_to([64, 8, 128]), ALU.mult)
            P.tt(Qs[Xc][:], pQ0, SL[:].unsqueeze(1).broadcast_to([64, 8, 64]), ALU.mult)
            P.tt(XTs[Xc], M1s[Xc][:, :, 0:64], I8, ALU.add)
            P.tt(Xbs[Xc][:], Qs[Xc][:], I8, ALU.add)
        bankP = [v8(B4, 0, 64), v8(T01, 0, 64)]
        bankQ = [v8(B5, 0, 64), v8(T01, 512, 64)]
        bankX1 = [v8(B6, 0, 64), v8(T23, 0, 64)]
        bankX2 = [v8(B7, 0, 64), v8(T23, 512, 64)]
        for kq in range(1, 6):
            last = (kq == 5)
            for Xc in range(2):
                Pm = M1s[Xc][:, :, 0:64]
                for h in range(8):
                    P.mm(bankP[Xc][:, h, :], Qs[Xc][:, h, :], Pm[:, h, :])
                if not last:
                    for h in range(8):
                        P.mm(bankQ[Xc][:, h, :], Pm[:, h, :], Qs[Xc][:, h, :])
            for Xc in range(2):
                Pm = M1s[Xc][:, :, 0:64]
                P.copy(Pm, bankP[Xc], eng='act')
                if not last:
                    P.copy(Qs[Xc][:], bankQ[Xc], eng='act')
            for Xc in range(2):
                Pm = M1s[Xc][:, :, 0:64]
                for h in range(8):
                    P.mm(bankX1[Xc][:, h, :], Xbs[Xc][:, h, :], Pm[:, h, :])
                if not last:
                    for h in range(8):
                        P.mm(bankX2[Xc][:, h, :], Pm[:, h, :], Xbs[Xc][:, h, :])
            for Xc in range(2):
                P.tt(XTs[Xc], XTs[Xc], bankX1[Xc], ALU.add)
                if not last:
                    P.tt(Xbs[Xc][:], Xbs[Xc][:], bankX2[Xc], ALU.add)
        for Xc in range(2):
            P.copy(XTb[Xc][:], XTs[Xc], eng='act')
        for Xc in range(2):
            cs = slice(Xc * 64, (Xc + 1) * 64)
            M1 = M1s[Xc]; M2 = M2s[Xc]
            if Xc == 0:
                pW = v8(B4, 0, 64); pU = v8(B5, 0, 64); pY = v8(B6, 0, 64); pSt = v8(B7, 0, 64)
                gbank = B4; obank = B5
            else:
                pW = v8(T01, 0, 64); pU = v8(T01, 512, 64); pY = v8(T23, 0, 64); pSt = v8(T23, 512, 64)
                gbank = T01[:, 0:512]; obank = T01[:, 512:1024]
            for h in range(8):
                P.mm(pW[:, h, :], M2[:, h, 0:64], Vcb[:, Xc, h, :], start=True, stop=False)
                P.mm(pW[:, h, :], AR[:, h, Xc, 0, :], S0Tb[:, h, :], start=False, stop=True)
            P.copy(Wsb[:], pW, eng='act')
            for h in range(8):
                P.mm(pU[:, h, :], XTb[Xc][:, h, :], Wsb[:, h, :])
            P.copy(Usb[:], pU, eng='act')
            for h in range(8):
                P.mm(pY[:, h, :], AR[:, h, Xc, 1, :], S0Tb[:, h, :], start=True, stop=False)
                P.mm(pY[:, h, :], M2[:, h, 64:128], Vcb[:, Xc, h, :], start=False, stop=False)
                P.mm(pY[:, h, :], M1[:, h, 64:128], Usb[:, h, :], start=False, stop=True)
            for h in range(8):
                P.mm(pSt[:, h, :], KTb[:, Xc, h, :], Vcb[:, Xc, h, :], start=True, stop=False)
                P.mm(pSt[:, h, :], BTb[:, Xc, h, :], Usb[:, h, :], start=False, stop=True)
            P.reduce(st8[:], pY, ALU.add)
            P.ts(st8[:], st8[:], -1.0 / 64, ALU.mult)
            P.tt(Y1, pY, st8[:].unsqueeze(2).broadcast_to([64, 8, 64]), ALU.add)
            P.act(Y2, Y1, AF.Square)
            P.reduce(st8b[:], Y2, ALU.add)
            P.act(st8b[:], st8b[:], AF.Sqrt, bias=64e-5, scale=1.0 / 64)
            P.recip(st8b[:], st8b[:])
            P.tt(Y1, Y1, st8b[:].unsqueeze(2).broadcast_to([64, 8, 64]), ALU.mult)
            Y1f = Y1.rearrange("p h i -> p (h i)")
            P.tt(Y1f, Y1f, lnw[:], ALU.mult)
            P.tt(Y1f, Y1f, lnb[:], ALU.add)
            P.tt(S0T[:], S0T[:], pSt, ALU.add)
            P.tt(S0T[:], S0T[:], gT[:, :, Xc:Xc + 1].broadcast_to([64, 8, 64]), ALU.mult)
            P.copy(S0Tb[:], S0T[:], eng='act')
            for h in range(8):
                P.mm(obank[0:64, h:h + 1], Th[:, h, cs], self.ones[0:64, 0:1])
            P.copy(sbon[:], obank[0:64, 0:8], eng='act')
            P.tt(Y2, Vc[:, Xc, :, :], sbon[:].unsqueeze(2).broadcast_to([64, 8, 64]), ALU.mult)
            P.tt(Y1, Y1, Y2, ALU.add)
            P.mm(gbank[0:64, :], sxg[:, 0, cs], gu1[:], start=True, stop=False)
            P.mm(gbank[0:64, :], sxg[0:32, 1, cs], gu2[:], start=False, stop=True)
            P.tt(obf, Y1f, gbank[0:64, :], ALU.mult)
            for c in range(4):
                P.tr(obank[:, 256 + c * 64:256 + (c + 1) * 64], obf[:, c * 128:(c + 1) * 128], I64)
            st = self.ostage[self.ostage_i % 2]
            P.copy(st[:, :, cs], obank[:, 256:512].rearrange("p (c t) -> p c t", c=4), eng='act')
            if Xc == 1:
                self.ostage_i += 1
                P.dma('sp', oT[tt], st[:], track_dram=False)
    P.end_phase()


K.rwkv = rwkv4


def merge2(self, l, hT, oTs, x_in, x_out):
    P = self.P
    I = self.inp
    P.begin_phase()
    mT = P.sb('mT', [128, 8, S], BF16)
    P.begin_phase()
    oS = [P.sb('oS%d' % i, [128, NT, 4, 128], BF16) for i in range(4)]
    for tq in range(4):
        for i in range(4):
            P.dma('sp', oS[i][:, tq * 4:(tq + 1) * 4, :, :], oTs[i][tq * 4:(tq + 1) * 4].rearrange("t p c s -> p t c s"))
    wb = [P.sb('wb%d' % i, [128, 4, 4, 128], BF16) for i in range(2)]
    wg = [P.sb('wg%d' % i, [128, 8, 4, 128], BF16) for i in range(2)]
    pu = [P.ps('pu%d' % i, [128, 512]) for i in range(2)]
    pg = [P.ps('pg%d' % i, [128, 512]) for i in range(2)]
    sg = [P.sb('sg%d' % i, [128, 512]) for i in range(2)]
    macc = P.sb('macc', [128, 512]); tmpm = P.sb('tmpm', [128, 512])
    it = 0
    for j in range(8):
        b = j % 2
        for i in range(4):
            P.dma('pool', wb[b][:, i, :, :], I['w_branch'][l, i, :, j * 128:(j + 1) * 128].rearrange("(k p) d -> p k d", p=128))
            P.dma('pool', wg[b][:, :, i, :],
                  I['w_in'][l, :, C_GATE + i * 1024 + j * 128:C_GATE + i * 1024 + (j + 1) * 128].rearrange("(k p) d -> p k d", p=128))
        for tb in range(4):
            tsl = slice(tb * 512, (tb + 1) * 512)
            for i in range(4):
                q = it % 2
                it += 1
                for kc in range(4):
                    P.mm(pu[q][:], wb[b][:, i, kc, :], oS[i][:, tb * 4:(tb + 1) * 4, kc, :], start=(kc == 0), stop=(kc == 3))
                for kc in range(8):
                    P.mm(pg[q][:], wg[b][:, kc, i, :], hT[:, kc, tsl], start=(kc == 0), stop=(kc == 7))
                P.act(sg[q][:], pg[q][:], AF.Sigmoid)
                if i == 0:
                    P.tt(macc[:], sg[q][:], pu[q][:], ALU.mult)
                elif i < 3:
                    P.tt(tmpm[:], sg[q][:], pu[q][:], ALU.mult)
                    P.tt(macc[:], macc[:], tmpm[:], ALU.add)
                else:
                    P.tt(tmpm[:], sg[q][:], pu[q][:], ALU.mult)
                    P.tt(mT[:, j, tsl], macc[:], tmpm[:], ALU.add)
    P.end_phase()
    P.begin_phase()
    wo = P.sb('wo', [128, 8, D], BF16)
    self.load_w(wo[:], I['w_out'][l])
    po = [P.ps('po%d' % i, [128, 512]) for i in range(4)]
    xt = [P.sb('xt%d' % i, [128, D]) for i in range(6)]
    for tt in range(min(6, NT)):
        P.dma('sp', xt[tt % 6][:], x_in[tt * 128:(tt + 1) * 128, :])
    for tt in range(NT):
        b = tt % 6
        for dh in range(2):
            pp = po[(tt % 2) * 2 + dh]
            for kc in range(8):
                P.mm(pp[:], mT[:, kc, tt * 128:(tt + 1) * 128], wo[:, kc, dh * 512:(dh + 1) * 512], start=(kc == 0), stop=(kc == 7))
            P.tt(xt[b][:, dh * 512:(dh + 1) * 512], xt[b][:, dh * 512:(dh + 1) * 512], pp[:], ALU.add)
        P.dma('sp', x_out[tt * 128:(tt + 1) * 128, :], xt[b][:], track_dram=False)
        if tt + 6 < NT:
            P.dma('sp', xt[b][:], x_in[(tt + 6) * 128:(tt + 7) * 128, :])
    P.end_phase()
    P.end_phase()


K.merge = merge2


def ffn(self, l, hT, x_in, x_out, moe):
    P = self.P
    I = self.inp
    P.begin_phase()
    yacc = P.sb('yacc', [128, NT, D])
    for tt in range(NT):
        P.dma('sp', yacc[:, tt, :], x_in[tt * 128:(tt + 1) * 128, :])
    if moe:
        E, F = 8, 3584
        logits = P.sb('logits', [128, NT, 8])
        self.norm_T(x_in, I['norm_ffn'][l], hT, router=(I['moe_router'][0], logits), x_sb=yacc)
        W1 = [I['moe_w1'][0, e] for e in range(E)]; W3 = [I['moe_w3'][0, e] for e in range(E)]; W2 = [I['moe_w2'][0, e] for e in range(E)]
        gate = P.sb('gate', [128, NT, 8])
        mk1 = P.sb('mk1', [128, NT, 8]); mk2 = P.sb('mk2', [128, NT, 8]); l2 = P.sb('l2', [128, NT, 8])
        m1 = P.sb('m1', [128, NT]); m2 = P.sb('m2', [128, NT]); g1 = P.sb('g1', [128, NT]); g2 = P.sb('g2', [128, NT])
        bc = lambda a: a.unsqueeze(2).broadcast_to([128, NT, 8])
        P.reduce(m1[:], logits[:], ALU.max)
        P.tt(mk1[:], logits[:], bc(m1[:]), ALU.is_equal)
        P.stt(l2[:], mk1[:], -1e30, logits[:], ALU.mult, ALU.add)
        P.reduce(m2[:], l2[:], ALU.max)
        P.tt(mk2[:], l2[:], bc(m2[:]), ALU.is_equal)
        P.tt(g2[:], m2[:], m1[:], ALU.subtract)
        P.act(g2[:], g2[:], AF.Exp)
        P.ts(g1[:], g2[:], 1.0, ALU.add)
        P.recip(g1[:], g1[:])
        P.tt(g2[:], g2[:], g1[:], ALU.mult)
        P.tt(mk1[:], mk1[:], bc(g1[:]), ALU.mult)
        P.tt(mk2[:], mk2[:], bc(g2[:]), ALU.mult)
        P.tt(gate[:], mk1[:], mk2[:], ALU.add)
    else:
        E, F = 1, 2816
        self.norm_T(x_in, I['norm_ffn'][l], hT, x_sb=yacc)
        W1 = [I['ffn_w1'][0]]; W3 = [I['ffn_w3'][0]]; W2 = [I['ffn_w2'][0]]
    w1g = [P.sb('w1g%d' % i, [128, 8, 512], BF16) for i in range(2)]
    w3g = [P.sb('w3g%d' % i, [128, 8, 512], BF16) for i in range(2)]
    w2g = [P.sb('w2g%d' % i, [128, 4, D], BF16) for i in range(2)]
    actT = P.sb('actT', [128, 4, S], BF16)
    s1 = [P.sb('s1_%d' % i, [128, 512]) for i in range(2)]
    p1 = [P.ps('p1_%d' % i, [128, 512]) for i in range(2)]
    p3 = [P.ps('p3_%d' % i, [128, 512]) for i in range(2)]
    py = [P.ps('py_%d' % i, [128, 512]) for i in range(4)]
    groups = []
    for e in range(E):
        f0 = 0
        while f0 < F:
            fw = min(512, F - f0)
            groups.append((e, f0, fw))
            f0 += fw
    first = True
    it = 0
    iy = 0
    for gi, (e, f0, fw) in enumerate(groups):
        b = gi % 2
        nfc = fw // 128
        P.dma('pool', w1g[b][:, :, 0:fw], W1[e][:, f0:f0 + fw].rearrange("(k p) c -> p k c", p=128))
        P.dma('pool', w3g[b][:, :, 0:fw], W3[e][:, f0:f0 + fw].rearrange("(k p) c -> p k c", p=128))
        P.dma('pool', w2g[b][:, 0:nfc, :], W2[e][f0:f0 + fw, :].rearrange("(k p) c -> p k c", p=128))
        for tb in range(4):
            tsl = slice(tb * 512, (tb + 1) * 512)
            for fc in range(nfc):
                q = it % 2
                it += 1
                for kc in range(8):
                    P.mm(p1[q][:], w1g[b][:, kc, fc * 128:(fc + 1) * 128], hT[:, kc, tsl], start=(kc == 0), stop=(kc == 7))
                for kc in range(8):
                    P.mm(p3[q][:], w3g[b][:, kc, fc * 128:(fc + 1) * 128], hT[:, kc, tsl], start=(kc == 0), stop=(kc == 7))
                P.act(s1[q][:], p1[q][:], AF.Silu)
                P.tt(actT[:, fc, tsl], s1[q][:], p3[q][:], ALU.mult)
        for tt in range(NT):
            for dh in range(2):
                pp = py[iy % 4]
                iy += 1
                for fc in range(nfc):
                    P.mm(pp[:], actT[:, fc, tt * 128:(tt + 1) * 128], w2g[b][:, fc, dh * 512:(dh + 1) * 512],
                         start=(fc == 0), stop=(fc == nfc - 1))
                ya = yacc[:, tt, dh * 512:(dh + 1) * 512]
                if moe:
                    P.stt(ya, pp[:], gate[:, tt, e:e + 1], ya, ALU.mult, ALU.add)
                else:
                    P.tt(ya, ya, pp[:], ALU.add)
    for tt in range(NT):
        P.dma('sp', x_out[tt * 128:(tt + 1) * 128, :], yacc[:, tt, :], track_dram=False)
    P.end_phase()


K.ffn = ffn


def build_all(self, nlayers=2, dbg_stop=None):
    P = self.P
    nc = self.nc
    I = self.inp
    self.consts()
    hT = P.sb('hT', [128, 8, S], BF16, persist=True)
    oTs = [nc.dram_tensor('oT%d' % i, [NT, 128, 4, 128], BF16, kind='Internal').ap() for i in range(4)]
    vfirst = nc.dram_tensor('vfirst', [NT, 64, 8, 128], F32, kind='Internal').ap()
    xm = [nc.dram_tensor('xm%d' % i, [S, D], F32, kind='Internal').ap() for i in range(2)]
    xf = nc.dram_tensor('xf0', [S, D], F32, kind='Internal').ap()
    y = self.dout('y', [S, D])
    xcur = I['x']
    for l in range(nlayers):
        self.norm_T(xcur, I['norm_mix'][l], hT)
        self.rwkv(l, hT, oTs[0], vfirst)
        self.mamba(l, hT, oTs[1])
        self.attention(l, hT, oTs[2])
        self.gla(l, hT, oTs[3])
        xo = y if (dbg_stop == ('m', l)) else xm[l]
        self.merge(l, hT, oTs, xcur, xo)
        if dbg_stop == ('m', l):
            break
        xo2 = y if (l == nlayers - 1 or dbg_stop == ('f', l)) else xf
        self.ffn(l, hT, xm[l], xo2, moe=(l % 2 == 1))
        if dbg_stop == ('f', l):
            break
        xcur = xo2
    P.finish()
    P.emit()


K.build_all = build_all


def host_consts(inputs):
    c = {}
    c['c_ident'] = np.eye(128, dtype=np.float32)
    s = np.arange(128)[:, None]
    l = np.arange(128)[None, :]
    same = (s // 64) == (l // 64)
    c['c_blktri'] = (same & (s <= l)).astype(np.float32)
    c['c_negmask'] = np.where(same & (s <= l), 0.0, -1e30).astype(np.float32)
    c['c_blkones'] = same.astype(np.float32)
    rb = np.asarray(inputs['att_rel_bias'], dtype=np.float32)
    j = np.arange(5)[None, :, None]
    kk = np.arange(128)[:, None, None]
    ll = np.arange(128)[None, None, :]
    rel = np.clip((j - 4) * 128 + kk - ll, -128, 128) + 128
    c['c_attbias'] = np.ascontiguousarray(np.transpose(rb[:, rel], (1, 0, 2, 3)))
    dq = 2 * (4 - j) + ll // 64 - kk // 64
    c['c_attmask'] = ((dq >= 0) & (dq <= 8)).astype(np.float32)
    cw = np.asarray(inputs['ssm_conv_w'], dtype=np.float32)
    L = cw.shape[0]
    c['h_convw'] = np.ascontiguousarray(cw.reshape(L, 4, 8, 128).transpose(0, 3, 2, 1))
    cb = np.asarray(inputs['ssm_conv_b'], dtype=np.float32)
    c['h_convb'] = np.ascontiguousarray(cb.reshape(L, 8, 128).transpose(0, 2, 1))
    mu = np.asarray(inputs['rwkv_mu'], dtype=np.float32)
    m64 = np.zeros((L, 64, 26), np.float32)
    m64[:, :, 0:24] = mu[:, 0:1536].reshape(L, 24, 64).transpose(0, 2, 1)
    m64[:, :, 24] = mu[:, 1536:1600]
    m64[:, :, 25] = mu[:, 1600:1664]
    c['h_mu64'] = m64
    mg = np.zeros((L, 128, 2), np.float32)
    mg[:, :, 0] = mu[:, 1664:1792]
    mg[:, 0:32, 1] = mu[:, 1792:1824]
    c['h_mug'] = mg
    rw = np.zeros((L, 64, 6, 8), np.float32)
    for i, nm in enumerate(['rwkv_w0', 'rwkv_a0', 'rwkv_k_k', 'rwkv_k_a', 'rwkv_r_k']):
        rw[:, :, i, :] = np.asarray(inputs[nm], dtype=np.float32).reshape(L, 8, 64).transpose(0, 2, 1)
    v0 = np.asarray(inputs['vres_v0'], dtype=np.float32)
    rw[1:, :, 5, :] = v0.reshape(L - 1, 8, 64).transpose(0, 2, 1)
    c['h_rw'] = rw
    s64 = np.arange(64)[:, None]; t64 = np.arange(64)[None, :]
    SU = (s64 < t64).astype(np.float32); UI = (s64 <= t64).astype(np.float32)
    c['c_m1'] = np.concatenate([SU, UI], axis=1)
    c['c_sl'] = (t64 < s64).astype(np.float32)
    sm = np.ones((64, 1024), np.float32); sm[:, ::64] = 0.0
    c['c_scanmask'] = sm
    return c


_PARAM_NAMES = None


def _build(inputs, hc):
    nc = bass.Bass("TRN2", target_bir_lowering=False)
    k = K(nc)
    k.din('x', [S, D])
    for n, a in inputs.items():
        if n != 'x':
            k.din(n, list(a.shape))
    for n, a in hc.items():
        k.din(n, list(a.shape))
    k.build_all()
    return nc, k


def kernel(**inputs):
    inputs = {n: np.ascontiguousarray(np.asarray(a, dtype=np.float32)) for n, a in inputs.items()}
    hc = host_consts(inputs)
    nc, k = _build(inputs, hc)
    n_cores = 8
    shared = {n: inputs[n] for n in inputs if n != 'x'}
    shared.update(hc)
    in_maps = []
    for b in range(n_cores):
        m = {n: shared[n] for n in k.inp if n != 'x'}
        m['x'] = np.ascontiguousarray(inputs['x'][b])
        in_maps.append(m)
    res = run_bass_kernel_spmd(nc, in_maps, core_ids=list(range(n_cores)))
    out = np.stack([np.asarray(r['y'], dtype=np.float32) for r in res.results], axis=0)
    return out
```
